# Optimizing a Trainium2 kernel written in Bass

```python
import math
import jax, jax.numpy as jnp
from jax import lax
import numpy as np

D_MODEL = 4096
BATCH = 4
SEQ = 2048
DEPTH = 1

CHUNK = 64
Q_BLOCK = 128
EPS = 1e-6

MLA_HEADS = 16
QK_NOPE = 128
QK_ROPE = 64
V_HEAD = 128
Q_RANK = 768
KV_RANK = 512
ROPE_THETA = 10000.0
MLA_WIDTH = MLA_HEADS * V_HEAD

SSM_WIDTH = D_MODEL - MLA_WIDTH
SSM_GROUP_CH = 16
SSM_GROUPS = SSM_WIDTH // SSM_GROUP_CH
SSM_STATE = 64
DT_MIN = 1e-3
DT_MAX = 1e-1

D_MIX = MLA_WIDTH + SSM_WIDTH
IN_COLS = Q_RANK + KV_RANK + QK_ROPE + SSM_WIDTH

N_EGROUPS = 8
EXPERTS_PER_GROUP = 8
N_EXPERTS = N_EGROUPS * EXPERTS_PER_GROUP
TOP_K = 2
D_EXPERT = 512
MOE_BLOCK = 128

kernel_name = "hymba_mla_s5_hier_moe_adaln"


def rms_norm(x, gain):
    xf = x.astype(jnp.float32)
    y = xf * lax.rsqrt(jnp.mean(xf * xf, axis=-1, keepdims=True) + EPS)
    return (y * gain.astype(jnp.float32)).astype(x.dtype)


def modulate(h, shift, scale):
    return h * (1.0 + scale[:, None, :]) + shift[:, None, :]


def rope(x, cos, sin):
    half = x.shape[-1] // 2
    x1, x2 = x[..., :half], x[..., half:]
    return jnp.concatenate([x1 * cos - x2 * sin, x2 * cos + x1 * sin], axis=-1)


def mla_group(q_lat, kv_lat, k_rope_raw, positions, q_lat_gain, w_uq, kv_lat_gain, w_ukv):
    bsz, seq, _ = q_lat.shape
    q = (rms_norm(q_lat, q_lat_gain) @ w_uq).reshape(bsz, seq, MLA_HEADS, QK_NOPE + QK_ROPE)
    kv = (rms_norm(kv_lat, kv_lat_gain) @ w_ukv).reshape(bsz, seq, MLA_HEADS, QK_NOPE + V_HEAD)
    q_nope, q_rope = q[..., :QK_NOPE], q[..., QK_NOPE:]
    k_nope, v = kv[..., :QK_NOPE], kv[..., QK_NOPE:]

    inv_freq = jnp.exp(-math.log(ROPE_THETA) * jnp.arange(0, QK_ROPE, 2, dtype=jnp.float32) / QK_ROPE)
    ang = positions.astype(jnp.float32)[..., None] * inv_freq
    cos = jnp.cos(ang).astype(q.dtype)
    sin = jnp.sin(ang).astype(q.dtype)
    q_rope = rope(q_rope, cos[:, :, None, :], sin[:, :, None, :])
    k_rope = rope(k_rope_raw, cos, sin)

    scale = (QK_NOPE + QK_ROPE) ** -0.5
    outs = []
    for blk in range(seq // Q_BLOCK):
        q0 = blk * Q_BLOCK
        kend = q0 + Q_BLOCK
        s = (jnp.einsum('bqhd,bkhd->bhqk', q_nope[:, q0:kend], k_nope[:, :kend])
             + jnp.einsum('bqhr,bkr->bhqk', q_rope[:, q0:kend], k_rope[:, :kend]))
        s = s.astype(jnp.float32) * scale
        q_chunk = (q0 + jnp.arange(Q_BLOCK)) // CHUNK
        k_chunk = jnp.arange(kend) // CHUNK
        mask = k_chunk[None, :] <= q_chunk[:, None]
        p = jax.nn.softmax(jnp.where(mask, s, -jnp.inf), axis=-1).astype(v.dtype)
        outs.append(jnp.einsum('bhqk,bkhd->bqhd', p, v[:, :kend]))
    return jnp.concatenate(outs, axis=1).reshape(bsz, seq, MLA_WIDTH)


def _complex_affine_combine(left, right):
    ar1, ai1, br1, bi1 = left
    ar2, ai2, br2, bi2 = right
    ar = ar2 * ar1 - ai2 * ai1
    ai = ar2 * ai1 + ai2 * ar1
    ar2b, ai2b = ar2[:, None], ai2[:, None]
    br = ar2b * br1 - ai2b * bi1 + br2
    bi = ar2b * bi1 + ai2b * br1 + bi2
    return ar, ai, br, bi


def s5_group(u, lam_re, lam_im, log_dt, b_re, b_im, c_re, c_im, d_skip, w_glu, b_glu):
    bsz, seq, _ = u.shape
    ug = u.reshape(bsz, seq, SSM_GROUPS, SSM_GROUP_CH)
    dt = jnp.exp(log_dt)[:, None]
    mag = jnp.exp(lam_re * dt)
    abar_re = mag * jnp.cos(lam_im * dt)
    abar_im = mag * jnp.sin(lam_im * dt)
    nr, ni = abar_re - 1.0, abar_im
    den = lam_re * lam_re + lam_im * lam_im
    f_re = (nr * lam_re + ni * lam_im) / den
    f_im = (ni * lam_re - nr * lam_im) / den
    bbar_re = f_re[..., None] * b_re - f_im[..., None] * b_im
    bbar_im = f_re[..., None] * b_im + f_im[..., None] * b_re
    bu_re = jnp.einsum('blgn,gpn->lbgp', ug, bbar_re)
    bu_im = jnp.einsum('blgn,gpn->lbgp', ug, bbar_im)
    a_re = jnp.broadcast_to(abar_re, (seq,) + abar_re.shape)
    a_im = jnp.broadcast_to(abar_im, (seq,) + abar_im.shape)
    _, _, h_re, h_im = lax.associative_scan(_complex_affine_combine, (a_re, a_im, bu_re, bu_im), axis=0)
    y = (jnp.einsum('lbgp,gnp->blgn', h_re, c_re) - jnp.einsum('lbgp,gnp->blgn', h_im, c_im)
         + d_skip.reshape(SSM_GROUPS, SSM_GROUP_CH) * ug)
    g = jax.nn.gelu(y.reshape(bsz, seq, SSM_WIDTH))
    return g * jax.nn.sigmoid(g @ w_glu + b_glu)


def hierarchical_moe(h, w_grp, b_grp, w_erouter, b_erouter, w1, w3, w2):
    bsz, seq, dm = h.shape
    t = bsz * seq
    hf = h.reshape(t, dm)
    g_logits = (hf @ w_grp).astype(jnp.float32) + b_grp.astype(jnp.float32)
    g_prob = jax.nn.softmax(g_logits, axis=-1)
    _, grp = lax.top_k(g_logits, 1)
    p_grp = jnp.take_along_axis(g_prob, grp, axis=1)[:, 0]
    e_logits = ((hf @ w_erouter).astype(jnp.float32) + b_erouter.astype(jnp.float32)
                ).reshape(t, N_EGROUPS, EXPERTS_PER_GROUP)
    e_in = jnp.take_along_axis(e_logits, grp[:, :, None], axis=1)[:, 0]
    top_v, top_i = lax.top_k(e_in, TOP_K)
    weights = p_grp[:, None] * jax.nn.softmax(top_v, axis=-1)
    expert = grp * EXPERTS_PER_GROUP + top_i

    n_assign = t * TOP_K
    e_flat = expert.reshape(n_assign)
    w_flat = weights.reshape(n_assign)
    tok_flat = jnp.repeat(jnp.arange(t, dtype=jnp.int32), TOP_K)
    order = jnp.argsort(e_flat)
    e_sorted, tok_sorted, w_sorted = e_flat[order], tok_flat[order], w_flat[order]
    counts = jnp.bincount(e_flat, length=N_EXPERTS)
    padded = ((counts + MOE_BLOCK - 1) // MOE_BLOCK) * MOE_BLOCK
    starts = jnp.cumsum(counts) - counts
    pends = jnp.cumsum(padded)
    pstarts = pends - padded
    dest = pstarts[e_sorted] + jnp.arange(n_assign) - starts[e_sorted]
    n_blocks = (n_assign + N_EXPERTS * (MOE_BLOCK - 1)) // MOE_BLOCK
    rows = n_blocks * MOE_BLOCK
    row_tok = jnp.full((rows,), t, jnp.int32).at[dest].set(tok_sorted)
    row_w = jnp.zeros((rows,), jnp.float32).at[dest].set(w_sorted)
    block_expert = jnp.minimum(
        jnp.searchsorted(pends, jnp.arange(n_blocks) * MOE_BLOCK, side='right'), N_EXPERTS - 1)
    h_pad = jnp.concatenate([hf, jnp.zeros((1, dm), hf.dtype)], axis=0)

    def run_block(args):
        tok, e = args
        xb = h_pad[tok]
        return (jax.nn.silu(xb @ w1[e]) * (xb @ w3[e])) @ w2[e]

    ys = lax.map(run_block, (row_tok.reshape(n_blocks, MOE_BLOCK), block_expert))
    ys = ys.reshape(rows, dm) * row_w[:, None].astype(ys.dtype)
    out = jnp.zeros((t + 1, dm), ys.dtype).at[row_tok].add(ys)
    return out[:t].reshape(bsz, seq, dm)


def setup_inputs(seed: int = 0) -> dict:
    key = jax.random.key(seed)
    ks = iter(jax.random.split(key, 40))
    f32 = jnp.float32

    def nrm(shape, scale):
        return jax.random.normal(next(ks), shape, f32) * scale

    def gain(shape):
        return 1.0 + nrm(shape, 0.01)

    L = DEPTH
    offsets = jax.random.randint(next(ks), (BATCH, 1), 0, 8192, dtype=jnp.int32)
    positions = offsets + jnp.arange(SEQ, dtype=jnp.int32)[None, :]
    lam_im = (math.pi * jnp.arange(SSM_STATE, dtype=f32))[None, None, :] + nrm((L, SSM_GROUPS, SSM_STATE), 0.01)
    return {
        "x": nrm((BATCH, SEQ, D_MODEL), 1.0),
        "c": nrm((BATCH, D_MODEL), 1.0),
        "positions": positions,
        "w_ada": nrm((L, D_MODEL, 6 * D_MODEL), 0.5 * D_MODEL ** -0.5),
        "b_ada": nrm((L, 6 * D_MODEL), 0.01),
        "norm_mix_gain": gain((L, D_MODEL)),
        "w_in": nrm((L, D_MODEL, IN_COLS), D_MODEL ** -0.5),
        "q_lat_gain": gain((L, Q_RANK)),
        "w_uq": nrm((L, Q_RANK, MLA_HEADS * (QK_NOPE + QK_ROPE)), Q_RANK ** -0.5),
        "kv_lat_gain": gain((L, KV_RANK)),
        "w_ukv": nrm((L, KV_RANK, MLA_HEADS * (QK_NOPE + V_HEAD)), KV_RANK ** -0.5),
        "ssm_lam_re": -0.5 * (1.0 + nrm((L, SSM_GROUPS, SSM_STATE), 0.01)),
        "ssm_lam_im": lam_im,
        "ssm_log_dt": jax.random.uniform(next(ks), (L, SSM_GROUPS), f32, math.log(DT_MIN), math.log(DT_MAX)),
        "ssm_b_re": nrm((L, SSM_GROUPS, SSM_STATE, SSM_GROUP_CH), (2 * SSM_GROUP_CH) ** -0.5),
        "ssm_b_im": nrm((L, SSM_GROUPS, SSM_STATE, SSM_GROUP_CH), (2 * SSM_GROUP_CH) ** -0.5),
        "ssm_c_re": nrm((L, SSM_GROUPS, SSM_GROUP_CH, SSM_STATE), (2 * SSM_STATE) ** -0.5),
        "ssm_c_im": nrm((L, SSM_GROUPS, SSM_GROUP_CH, SSM_STATE), (2 * SSM_STATE) ** -0.5),
        "ssm_d": nrm((L, SSM_WIDTH), 1.0),
        "w_glu": nrm((L, SSM_WIDTH, SSM_WIDTH), SSM_WIDTH ** -0.5),
        "b_glu": nrm((L, SSM_WIDTH), 0.01),
        "mla_out_gain": gain((L, MLA_WIDTH)),
        "ssm_out_gain": gain((L, SSM_WIDTH)),
        "w_out": nrm((L, D_MIX, D_MODEL), D_MIX ** -0.5),
        "norm_ffn_gain": gain((L, D_MODEL)),
        "w_group_router": nrm((L, D_MODEL, N_EGROUPS), D_MODEL ** -0.5),
        "b_group_router": nrm((L, N_EGROUPS), 0.01),
        "w_expert_router": nrm((L, D_MODEL, N_EXPERTS), D_MODEL ** -0.5),
        "b_expert_router": nrm((L, N_EXPERTS), 0.01),
        "w1_experts": nrm((L, N_EXPERTS, D_MODEL, D_EXPERT), D_MODEL ** -0.5),
        "w3_experts": nrm((L, N_EXPERTS, D_MODEL, D_EXPERT), D_MODEL ** -0.5),
        "w2_experts": nrm((L, N_EXPERTS, D_EXPERT, D_MODEL), D_EXPERT ** -0.5),
        "final_gain": gain((D_MODEL,)),
    }


def reference(x, c, positions, w_ada, b_ada, norm_mix_gain, w_in, q_lat_gain, w_uq, kv_lat_gain, w_ukv,
              ssm_lam_re, ssm_lam_im, ssm_log_dt, ssm_b_re, ssm_b_im, ssm_c_re, ssm_c_im, ssm_d,
              w_glu, b_glu, mla_out_gain, ssm_out_gain, w_out, norm_ffn_gain,
              w_group_router, b_group_router, w_expert_router, b_expert_router,
              w1_experts, w3_experts, w2_experts, final_gain):
    c_act = jax.nn.silu(c)
    s0 = Q_RANK
    s1 = s0 + KV_RANK
    s2 = s1 + QK_ROPE
    for i in range(DEPTH):
        mod = c_act @ w_ada[i] + b_ada[i]
        sh_a, sc_a, g_a, sh_f, sc_f, g_f = jnp.split(mod, 6, axis=-1)

        h = modulate(rms_norm(x, norm_mix_gain[i]), sh_a, sc_a)
        z = h @ w_in[i]
        o_mla = mla_group(z[..., :s0], z[..., s0:s1], z[..., s1:s2], positions,
                          q_lat_gain[i], w_uq[i], kv_lat_gain[i], w_ukv[i])
        o_ssm = s5_group(z[..., s2:], ssm_lam_re[i], ssm_lam_im[i], ssm_log_dt[i],
                         ssm_b_re[i], ssm_b_im[i], ssm_c_re[i], ssm_c_im[i], ssm_d[i],
                         w_glu[i], b_glu[i])
        o = jnp.concatenate([rms_norm(o_mla, mla_out_gain[i]), rms_norm(o_ssm, ssm_out_gain[i])], axis=-1)
        x = x + g_a[:, None, :] * (o @ w_out[i])

        h = modulate(rms_norm(x, norm_ffn_gain[i]), sh_f, sc_f)
        x = x + g_f[:, None, :] * hierarchical_moe(h, w_group_router[i], b_group_router[i],
                                                   w_expert_router[i], b_expert_router[i],
                                                   w1_experts[i], w3_experts[i], w2_experts[i])
    return rms_norm(x, final_gain)
```

```python
import math
import numpy as np
import ml_dtypes
import concourse.bass as bass
import concourse.mybir as mybir
from contextlib import ExitStack
from concourse.bass_utils import run_bass_kernel_spmd

F32 = mybir.dt.float32
BF16 = mybir.dt.bfloat16
I32 = mybir.dt.int32
ALU = mybir.AluOpType
AF = mybir.ActivationFunctionType
AX = mybir.AxisListType

ENGS = ("pe", "dve", "act", "pool", "sp")
NDMASEM = 12
SB_BYTES = 200 * 1024


class T:
    def __init__(self, k, apv, name):
        self.k = k
        self.v = apv
        self.name = name
        self.writer = None
        self.readers = []
        self.birth = list(k.birth)

    def __getitem__(self, key):
        return self.v[key]


class Op:
    __slots__ = ("eng", "fn", "deps", "need_inc", "sem", "val", "is_dma")

    def __init__(self, eng, fn, is_dma=False):
        self.eng = eng
        self.fn = fn
        self.deps = []
        self.need_inc = False
        self.sem = None
        self.val = None
        self.is_dma = is_dma


class K:
    def __init__(self, nc):
        self.nc = nc
        self.es = ExitStack()
        self.ops = {e: [] for e in ENGS}
        self.birth = []
        self.last = {e: None for e in ENGS}
        self.dma_last = {}
        self.dma_ctr = {e: 0 for e in ENGS}
        self.big = self.es.enter_context(nc.sbuf_tensor("sbig", [128, SB_BYTES // 4], F32))
        self.bump = 0
        self.scopes = []
        self.nops = 0
        self.regcache = {}

    def sb(self, name, shape, dtype, parts=None):
        shape = list(shape)
        p = shape[0]
        n = int(np.prod(shape[1:]))
        esz = {F32: 4, BF16: 2, I32: 4}[dtype]
        nbytes = (n * esz + 31) // 32 * 32
        off = self.bump
        self.bump += nbytes
        assert self.bump <= SB_BYTES, "SBUF overflow %s %d" % (name, self.bump)
        v = self.big[0:p, off // 4:(off + nbytes) // 4]
        if dtype != F32:
            v = v.bitcast(dtype)
        v = v[:, 0:n]
        if len(shape) == 3:
            v = v.rearrange("p (a b) -> p a b", a=shape[1], b=shape[2])
        elif len(shape) == 4:
            v = v.rearrange("p (a b c) -> p a b c", a=shape[1], b=shape[2], c=shape[3])
        return T(self, v, name)

    def push(self):
        self.scopes.append(self.bump)

    def pop(self):
        self.bump = self.scopes.pop()
        self.barrier()

    def psum(self, name):
        h = self.es.enter_context(self.nc.psum_tensor(name, [128, 512], F32))
        return T(self, h[:], name)

    def dram(self, name, shape, dtype, kind=None):
        if kind is None:
            h = self.nc.dram_tensor(name, list(shape), dtype)
        else:
            h = self.nc.dram_tensor(name, list(shape), dtype, kind=kind)
        return T(self, h.ap(), name)

    def _add(self, op, reads, writes):
        deps = []
        for t in list(reads) + list(writes):
            deps.extend(t.birth)
        for t in reads:
            if t.writer is not None:
                deps.append(t.writer)
        for t in writes:
            if t.writer is not None:
                deps.append(t.writer)
            deps.extend(t.readers)
        seen = set(id(d) for d in op.deps)
        for d in deps:
            if d is op or id(d) in seen:
                continue
            seen.add(id(d))
            if d.eng == "pe" and op.eng == "pe" and not d.is_dma and not op.is_dma:
                continue
            op.deps.append(d)
            d.need_inc = True
        for t in reads:
            if not op.is_dma:
                t.readers = [r for r in t.readers if r.is_dma or r.eng != op.eng]
            t.readers.append(op)
        for t in writes:
            t.writer = op
            t.readers = []
        self.ops[op.eng].append(op)
        if not op.is_dma:
            self.last[op.eng] = op
        self.nops += 1
        return op

    def op(self, eng, fn, reads=(), writes=()):
        return self._add(Op(eng, fn), reads, writes)

    def _dma_op(self, eng, fn, reads, writes):
        op = Op(eng, fn, is_dma=True)
        slot = (eng, self.dma_ctr[eng] % NDMASEM)
        self.dma_ctr[eng] += 1
        prev = self.dma_last.get(slot)
        if prev is not None:
            op.deps.append(prev)
        op.need_inc = True
        op.sem = slot
        self.dma_last[slot] = op
        return self._add(op, reads, writes)

    def dma(self, eng, out, in_, reads=(), writes=(), **kw):
        return self._dma_op(eng, lambda e: e.dma_start(out=out, in_=in_, **kw), reads, writes)

    def scatter(self, out, idx_ap, in_, reads=(), writes=()):
        return self._dma_op("pool", lambda e: e.indirect_dma_start(
            out=out, out_offset=bass.IndirectOffsetOnAxis(ap=idx_ap, axis=0), in_=in_, in_offset=None), reads, writes)

    def gather(self, out, in_, idx_ap, reads=(), writes=(), bound=None):
        def fn(e):
            kw = {}
            if bound is not None:
                if bound not in self.regcache:
                    self.regcache[bound] = e.to_reg(bound)
                kw = dict(bounds_check=self.regcache[bound], oob_is_err=False)
            return e.indirect_dma_start(out=out, out_offset=None, in_=in_,
                                        in_offset=bass.IndirectOffsetOnAxis(ap=idx_ap, axis=0), **kw)
        return self._dma_op("pool", fn, reads, writes)

    def barrier(self):
        b = [o for o in self.last.values() if o is not None]
        b += list(self.dma_last.values())
        for o in b:
            o.need_inc = True
        self.birth = b

    def ve(self, name, reads, writes, eng="dve", **kw):
        return self.op(eng, lambda e: getattr(e, name)(**kw), reads, writes)

    def mm(self, out, lhsT, rhs, start, stop, reads, writes, **kw):
        return self.op("pe", lambda e: e.matmul(out, lhsT, rhs, start=start, stop=stop, **kw), reads, writes)

    def tr(self, out, in_, ident, reads, writes):
        return self.op("pe", lambda e: e.transpose(out, in_, ident), reads, writes)

    def act(self, out, in_, func, reads, writes, **kw):
        return self.op("act", lambda e: e.activation(out=out, in_=in_, func=func, **kw), reads, writes)

    def emit(self):
        nc = self.nc
        fin = Op("sp", None)
        fin.deps = list(self.dma_last.values()) + [o for o in self.last.values() if o is not None]
        for o in fin.deps:
            o.need_inc = True
        self.ops["sp"].append(fin)
        sems = {}
        for e in ENGS:
            sems[e] = self.es.enter_context(nc.semaphore("s_" + e))
            for i in range(NDMASEM):
                sems[(e, i)] = self.es.enter_context(nc.semaphore("d_%s%d" % (e, i)))
        cnt = {}
        for e in ENGS:
            for op in self.ops[e]:
                if op.is_dma:
                    cnt[op.sem] = cnt.get(op.sem, 0) + 16
                    op.val = cnt[op.sem]
                elif op.need_inc:
                    op.sem = e
                    cnt[e] = cnt.get(e, 0) + 1
                    op.val = cnt[e]
        self.sem_max = dict(cnt)
        engobj = {"pe": "tensor", "dve": "vector", "act": "scalar", "pool": "gpsimd", "sp": "sync"}
        block = self.es.enter_context(nc.Block())

        def make(e):
            def body(eng):
                waited = {}
                for op in self.ops[e]:
                    for d in op.deps:
                        if waited.get(d.sem, 0) < d.val:
                            eng.wait_ge(sems[d.sem], d.val)
                            waited[d.sem] = d.val
                    if op.fn is None:
                        continue
                    ins = op.fn(eng)
                    if op.is_dma:
                        ins.then_inc(sems[op.sem], 16)
                    elif op.need_inc:
                        ins.then_inc(sems[op.sem], 1)
            return body

        for e in ENGS:
            getattr(block, engobj[e])(make(e))
        self.es.close()


D = 4096
SEQ = 2048
NB_FULL = 4
QR, KVR, ROPE = 768, 512, 64
NH = 16
SSMW = 2048
NG = 128
NST = 64
IN_COLS = QR + KVR + ROPE + SSMW
TMC = QR + KVR + ROPE
NE = 64
DE = 512
EPS = 1e-6
BLKT = 2
BLK = BLKT * 128
TWO_PI = 2.0 * math.pi
C1 = 6.28125
C2 = float(TWO_PI - 6.28125)
ATT_SCALE = float((128 + 64) ** -0.5)
PI_LO = 3.1415925


class Cx:
    pass


def build(NB=NB_FULL, stages=None, inject=(), dump=()):
    allst = ["mod", "s1", "s2a", "s2b", "s3", "s4", "s5", "s6", "s7", "s8", "s9"]
    stages = set(allst if stages is None else stages)
    NT = NB * 16
    NTOK = NB * SEQ
    NBLK = (NTOK * 2 + NE * (BLK - 1)) // BLK
    nc = bass.Bass("TRN2", target_bir_lowering=False)
    k = K(nc)
    cx = Cx()
    cx.k, cx.NB, cx.NT = k, NB, NT
    cx.inputs = {}

    def inp(name, shape, dtype=F32):
        t = k.dram(name, shape, dtype, kind="ExternalInput")
        cx.inputs[name] = (tuple(shape), dtype)
        return t

    def scratch(name, shape, dtype):
        if name in inject:
            return inp(name, shape, dtype)
        if name in dump:
            return k.dram(name, shape, dtype, kind="ExternalOutput")
        return k.dram(name, shape, dtype)

    MOD = scratch("MOD", [NB, 6 * D], F32)
    HT = scratch("HT", [NB, 16, 128, 32 * 128], BF16)
    QNT = scratch("QNT", [NB, 6, 128, SEQ], BF16)
    KVNT = scratch("KVNT", [NB, 4, 128, SEQ], BF16)
    KRT = scratch("KRT", [NB, 64, SEQ], BF16)
    COST = scratch("COST", [NB, 64, SEQ], F32)
    SINT = scratch("SINT", [NB, 64, SEQ], F32)
    UT = scratch("UT", [NB, 16, 128, SEQ], BF16)
    OMT = scratch("OMT", [NB, 16, 128, SEQ], BF16)
    GT = scratch("GT", [NB, 16, 128, SEQ], BF16)
    RSM = scratch("RSM", [128, NT], F32)
    RSS = scratch("RSS", [128, NT], F32)
    OST = scratch("OST", [NB, 16, 128, SEQ], BF16)
    X1 = scratch("X1", [NTOK, D], F32)
    XE = scratch("XE", [NBLK * BLK, D], BF16)
    YE = scratch("YE", [NBLK * BLK, D], BF16)
    SLOTS = scratch("SLOTS", [128, NT * 2], I32)
    GATES = scratch("GATES", [128, NT * 2], F32)
    H2B = scratch("H2B", [NTOK, D], BF16)
    IWD = scratch("IWD", [128, NBLK], I32)
    OUT = k.dram("out", [NTOK, D], F32, kind="ExternalOutput")

    P = [k.psum("ps%d" % i) for i in range(8)]
    PB = [p_[:].bitcast(BF16) for p_ in P]

    identf = k.sb("identf", [128, 128], F32)
    identb = k.sb("identb", [128, 128], BF16)
    epsT = k.sb("epsT", [128, 1], F32)
    hpiT = k.sb("hpiT", [128, 1], F32)
    IDENT = inp("ident", [128, 128])
    k.dma("sp", identf[:], IDENT[:, :], writes=[identf])
    k.ve("tensor_copy", [identf], [identb], out=identb[:], in_=identf[:])
    k.ve("memset", [], [epsT], eng="pool", ap=epsT[:], constant=EPS)
    k.ve("memset", [], [hpiT], eng="pool", ap=hpiT[:], constant=math.pi / 2)
    cx.alt = 0
    def evac(out, in_, reads, writes):
        cx.alt ^= 1
        if cx.alt:
            k.op("act", lambda e: e.copy(out=out, in_=in_), reads, writes)
        else:
            k.ve("tensor_copy", reads, writes, out=out, in_=in_)

    def rstd_from_ss(rstd, ss, n, reads_extra=()):
        k.act(rstd[:], ss[:], AF.Sqrt, [ss, epsT], [rstd], scale=1.0 / n, bias=epsT[:])
        k.ve("reciprocal", [rstd], [rstd], out=rstd[:], in_=rstd[:])

    def range_reduce(ang, ti, tf, act_cvt=False):
        k.ve("tensor_scalar", [ang], [ti], out=ti[:], in0=ang[:], scalar1=1.0 / TWO_PI, scalar2=None, op0=ALU.mult)
        if act_cvt:
            k.op("act", lambda e: e.copy(out=tf[:], in_=ti[:]), [ti], [tf])
        else:
            k.ve("tensor_copy", [ti], [tf], out=tf[:], in_=ti[:])
        k.ve("scalar_tensor_tensor", [tf, ang], [ang], out=ang[:], in0=tf[:], scalar=-C1, in1=ang[:], op0=ALU.mult, op1=ALU.add)
        k.ve("scalar_tensor_tensor", [tf, ang], [ang], out=ang[:], in0=tf[:], scalar=-C2, in1=ang[:], op0=ALU.mult, op1=ALU.add)
        k.ve("tensor_scalar", [ang], [ang], out=ang[:], in0=ang[:], scalar1=PI_LO, scalar2=-PI_LO, op0=ALU.min, op1=ALU.max)
        k.ve("scalar_tensor_tensor", [ang], [tf], out=tf[:], in0=ang[:], scalar=-1.0, in1=ang[:], op0=ALU.mult, op1=ALU.max)

    def bcast_row(dst, src_row_ap, eng="sp"):
        k.dma(eng, dst[:], src_row_ap.to_broadcast([128, src_row_ap.shape[-1]]), writes=[dst])

    if "mod" in stages:
        C_IN = inp("c", [NB, D])
        W_ADA = inp("w_ada", [D, 6 * D])
        B_ADA = inp("b_ada", [1, 6 * D])
        k.push()
        c4 = k.sb("c4", [NB, D], F32)
        k.dma("sp", c4[:], C_IN[:, :], writes=[c4])
        k.act(c4[:], c4[:], AF.Silu, [c4], [c4])
        for kc in range(32):
            k.tr(P[0][:, kc * NB:(kc + 1) * NB], c4[:, kc * 128:(kc + 1) * 128], identf[0:NB, 0:NB], [c4, identf], [P[0]])
        cT = k.sb("cT", [128, 32, NB], BF16)
        k.ve("tensor_copy", [P[0]], [cT], out=cT[:], in_=P[0][:, 0:32 * NB].rearrange("p (k b) -> p k b", b=NB))
        ones1 = k.sb("ones1", [1, NB], F32)
        k.ve("memset", [], [ones1], eng="pool", ap=ones1[:], constant=1.0)
        wq = [[k.sb("wa%d_%d" % (i, q), [128, 8, 512], BF16) for q in range(4)] for i in range(2)]
        bch = [k.sb("bch%d" % i, [1, 512], F32) for i in range(2)]
        mo = [k.sb("mo%d" % i, [NB, 512], F32) for i in range(2)]
        for ch in range(48):
            w = wq[ch % 2]
            for q in range(4):
                k.dma("pool", w[q][:], W_ADA[q * 1024:(q + 1) * 1024, ch * 512:(ch + 1) * 512].rearrange("(k p) n -> p k n", p=128), writes=[w[q]])
            bb = bch[ch % 2]
            k.dma("sp", bb[:], B_ADA[0:1, ch * 512:(ch + 1) * 512], writes=[bb])
            ps = P[1 + ch % 2]
            for kc in range(32):
                k.mm(ps[0:NB, :], cT[:, kc, :], w[kc // 8][:, kc % 8, :], kc == 0, False, [cT, w[kc // 8]], [ps])
            k.mm(ps[0:NB, :], ones1[:, :], bb[:, :], False, True, [ones1, bb], [ps])
            m = mo[ch % 2]
            evac(m[:], ps[0:NB, :], [ps], [m])
            k.dma("sp", MOD[:, ch * 512:(ch + 1) * 512], m[:], reads=[m], writes=[MOD])
        k.pop()

    if "s7" in stages:
        zt = k.sb("zt", [128, 2048], BF16)
        k.ve("memset", [], [zt], eng="pool", ap=zt[:], constant=0.0)
        cx.xez = []
        for j in range(NBLK * BLKT):
            for hf in range(2):
                tz = T(k, XE.v, "xez")
                cx.xez.append(tz)
                k.dma("pool", XE[j * 128:(j + 1) * 128, hf * 2048:(hf + 1) * 2048], zt[:], reads=[zt], writes=[tz])

    X_IN = inp("x", [NTOK, D])

    def load_mod_vec(dst, b, idx, eng="sp"):
        bcast_row(dst, MOD[b:b + 1, idx * D:(idx + 1) * D], eng)

    def norm_mod_tile(xt, A, SH, out_t, junk, ss, rstd):
        k.act(junk[:], xt[:], AF.Square, [xt], [junk, ss], accum_out=ss[:])
        rstd_from_ss(rstd, ss, D)
        k.ve("scalar_tensor_tensor", [xt, rstd, A], [xt], out=xt[:], in0=xt[:], scalar=rstd[:, 0:1], in1=A[:], op0=ALU.mult, op1=ALU.mult)
        k.ve("tensor_tensor", [xt, SH], [out_t], out=out_t[:], in0=xt[:], in1=SH[:], op=ALU.add)

    def make_A(A, gtmp, grow_ap):
        bcast_row(gtmp, grow_ap)
        k.ve("scalar_tensor_tensor", [A, gtmp], [A], out=A[:], in0=A[:], scalar=1.0, in1=gtmp[:], op0=ALU.add, op1=ALU.mult)

    if "s1" in stages:
        G_MIX = inp("g_mix", [1, D])
        for b in range(NB):
            k.push()
            Aa = k.sb("Aa", [128, D], F32)
            SHa = k.sb("SHa", [128, D], F32)
            gtmp = k.sb("gtmp", [128, D], F32)
            load_mod_vec(Aa, b, 1)
            load_mod_vec(SHa, b, 0)
            make_A(Aa, gtmp, G_MIX[0:1, :])
            xts = [k.sb("xt%d" % i, [128, D], F32) for i in range(2)]
            hbs = [k.sb("hb%d" % i, [128, D], BF16) for i in range(2)]
            junk = k.sb("junk", [128, D], BF16)
            ss = k.sb("ss", [128, 1], F32)
            rstd = k.sb("rstd", [128, 1], F32)
            hTq = [[k.sb("hT%d_%d" % (i, q), [128, 8, 128], BF16) for q in range(4)] for i in range(2)]
            for t in range(16):
                xt, hb, hT = xts[t % 2], hbs[t % 2], hTq[t % 2]
                r0 = b * SEQ + t * 128
                k.dma("sp", xt[:], X_IN[r0:r0 + 128, :], writes=[xt])
                norm_mod_tile(xt, Aa, SHa, hb, junk, ss, rstd)
                for q in range(4):
                    for j in range(8):
                        kc = q * 8 + j
                        k.tr(PB[q][:, j * 128:(j + 1) * 128], hb[:, kc * 128:(kc + 1) * 128], identb[:], [hb, identb], [P[q]])
                    evac(hT[q][:], PB[q][:, 0:1024].rearrange("p (j n) -> p j n", n=128), [P[q]], [hT[q]])
                    k.dma("sp", HT[b, t, :, q * 1024:(q + 1) * 1024], hT[q][:].rearrange("p k n -> p (k n)"), reads=[hT[q]], writes=[HT])
            k.pop()

    if "s2a" in stages:
        W_IN = inp("w_in", [D, IN_COLS])
        G_Q = inp("g_q", [1, QR])
        G_KV = inp("g_kv", [1, KVR])
        INVF = inp("invf_bc", [128, 32])
        POST = inp("posT", [128, NT], I32)
        k.push()
        Wtm = [k.sb("Wtm%d" % q, [128, 8, TMC], BF16) for q in range(4)]
        for q in range(4):
            k.dma("pool", Wtm[q][:], W_IN[q * 1024:(q + 1) * 1024, 0:TMC].rearrange("(k p) n -> p k n", p=128), writes=[Wtm[q]])
        gq = k.sb("gq", [128, QR], F32)
        gkv = k.sb("gkv", [128, KVR], F32)
        bcast_row(gq, G_Q[0:1, :])
        bcast_row(gkv, G_KV[0:1, :])
        invf = k.sb("invf", [128, 32], F32)
        k.dma("sp", invf[:], INVF[:, :], writes=[invf])
        posi = k.sb("posi", [128, NT], I32)
        posf = k.sb("posf", [128, NT], F32)
        k.dma("sp", posi[:], POST[:, :], writes=[posi])
        k.ve("tensor_copy", [posi], [posf], out=posf[:], in_=posi[:])
        hTs = [k.sb("hTt%d" % i, [128, 32, 128], BF16) for i in range(2)]
        zs = [k.sb("z%d" % i, [128, TMC], F32) for i in range(2)]
        junk = k.sb("junk2", [128, QR], BF16)
        ss = k.sb("ss2", [128, 2], F32)
        rs = k.sb("rs2", [128, 2], F32)
        qn = k.sb("qn", [128, QR], BF16)
        kvn = k.sb("kvn", [128, KVR], BF16)
        qnT = [k.sb("qnT%d" % i, [128, 6, 128], BF16) for i in range(2)]
        kvnT = [k.sb("kvnT%d" % i, [128, 4, 128], BF16) for i in range(2)]
        ang = k.sb("ang", [128, 32], F32)
        ti = k.sb("ti", [128, 32], I32)
        tf = k.sb("tf", [128, 32], F32)
        sn = k.sb("sn", [128, 32], F32)
        cs = k.sb("cs", [128, 32], F32)
        t1 = k.sb("t1", [128, 32], F32)
        t2 = k.sb("t2", [128, 32], F32)
        kr = k.sb("kr", [128, 64], BF16)
        krT = [k.sb("krT%d" % i, [64, 128], BF16) for i in range(2)]
        tabc = k.sb("tabc", [128, 64], F32)
        tabs = k.sb("tabs", [128, 64], F32)
        tabT = [k.sb("tabT%d" % i, [64, 256], F32) for i in range(2)]
        ssq, ssk = ss[:, 0:1], ss[:, 1:2]
        for b in range(NB):
            for t in range(16):
                g = b * 16 + t
                hT, z = hTs[t % 2], zs[t % 2]
                k.dma("sp", hT[:], HT[b, t].rearrange("p (k n) -> p k n", n=128), reads=[HT], writes=[hT])
                for bank, c0, c1 in ((0, 0, 512), (1, 512, 1024), (2, 1024, TMC)):
                    for kc in range(32):
                        k.mm(P[bank][:, 0:c1 - c0], hT[:, kc, :], Wtm[kc // 8][:, kc % 8, c0:c1], kc == 0, kc == 31, [hT, Wtm[kc // 8]], [P[bank]])
                    evac(z[:, c0:c1], P[bank][:, 0:c1 - c0], [P[bank]], [z])
                k.act(junk[:], z[:, 0:QR], AF.Square, [z], [junk, ss], accum_out=ssq)
                k.act(junk[:, 0:KVR], z[:, QR:QR + KVR], AF.Square, [z, junk], [junk, ss], accum_out=ssk)
                k.act(rs[:, 0:1], ssq, AF.Sqrt, [ss, epsT], [rs], scale=1.0 / QR, bias=epsT[:])
                k.act(rs[:, 1:2], ssk, AF.Sqrt, [ss, epsT, rs], [rs], scale=1.0 / KVR, bias=epsT[:])
                k.ve("reciprocal", [rs], [rs], out=rs[:], in_=rs[:])
                k.ve("scalar_tensor_tensor", [z, rs, gq], [qn], out=qn[:], in0=z[:, 0:QR], scalar=rs[:, 0:1], in1=gq[:], op0=ALU.mult, op1=ALU.mult)
                k.ve("scalar_tensor_tensor", [z, rs, gkv], [kvn], out=kvn[:], in0=z[:, QR:QR + KVR], scalar=rs[:, 1:2], in1=gkv[:], op0=ALU.mult, op1=ALU.mult)
                qT, kvT = qnT[t % 2], kvnT[t % 2]
                for j in range(6):
                    k.tr(PB[3][:, j * 128:(j + 1) * 128], qn[:, j * 128:(j + 1) * 128], identb[:], [qn, identb], [P[3]])
                evac(qT[:], PB[3][:, 0:768].rearrange("p (j n) -> p j n", n=128), [P[3]], [qT])
                k.dma("sp", QNT[b, :, :, t * 128:(t + 1) * 128].rearrange("k p n -> p k n"), qT[:], reads=[qT], writes=[QNT])
                for j in range(4):
                    k.tr(PB[4][:, j * 128:(j + 1) * 128], kvn[:, j * 128:(j + 1) * 128], identb[:], [kvn, identb], [P[4]])
                evac(kvT[:], PB[4][:, 0:512].rearrange("p (j n) -> p j n", n=128), [P[4]], [kvT])
                k.dma("sp", KVNT[b, :, :, t * 128:(t + 1) * 128].rearrange("k p n -> p k n"), kvT[:], reads=[kvT], writes=[KVNT])
                k.ve("tensor_scalar", [invf, posf], [ang], out=ang[:], in0=invf[:], scalar1=posf[:, g:g + 1], scalar2=None, op0=ALU.mult)
                range_reduce(ang, ti, tf)
                k.act(sn[:], ang[:], AF.Sin, [ang], [sn])
                k.act(cs[:], tf[:], AF.Sin, [tf, hpiT], [cs], scale=-1.0, bias=hpiT[:])
                x1, x2 = z[:, QR + KVR:QR + KVR + 32], z[:, QR + KVR + 32:TMC]
                k.ve("tensor_tensor", [z, cs], [t1], out=t1[:], in0=x1, in1=cs[:], op=ALU.mult)
                k.ve("tensor_tensor", [z, sn], [t2], out=t2[:], in0=x2, in1=sn[:], op=ALU.mult)
                k.ve("tensor_tensor", [t1, t2], [kr], out=kr[:, 0:32], in0=t1[:], in1=t2[:], op=ALU.subtract)
                k.ve("tensor_tensor", [z, cs], [t1], out=t1[:], in0=x2, in1=cs[:], op=ALU.mult)
                k.ve("tensor_tensor", [z, sn], [t2], out=t2[:], in0=x1, in1=sn[:], op=ALU.mult)
                k.ve("tensor_tensor", [t1, t2], [kr], out=kr[:, 32:64], in0=t1[:], in1=t2[:], op=ALU.add)
                kT = krT[t % 2]
                k.tr(PB[5][0:64, 0:128], kr[:, :], identb[:], [kr, identb], [P[5]])
                evac(kT[:], PB[5][0:64, 0:128], [P[5]], [kT])
                k.dma("sp", KRT[b, :, t * 128:(t + 1) * 128], kT[:], reads=[kT], writes=[KRT])
                k.ve("tensor_copy", [cs], [tabc], out=tabc[:, 0:32], in_=cs[:])
                k.ve("tensor_copy", [cs], [tabc], out=tabc[:, 32:64], in_=cs[:])
                k.ve("tensor_scalar", [sn], [tabs], out=tabs[:, 0:32], in0=sn[:], scalar1=-1.0, scalar2=None, op0=ALU.mult)
                k.ve("tensor_copy", [sn], [tabs], out=tabs[:, 32:64], in_=sn[:])
                tT = tabT[t % 2]
                k.tr(P[6][0:64, 0:128], tabc[:, :], identf[:], [tabc, identf], [P[6]])
                k.tr(P[6][0:64, 128:256], tabs[:, :], identf[:], [tabs, identf], [P[6]])
                evac(tT[:], P[6][0:64, 0:256], [P[6]], [tT])
                k.dma("sp", COST[b, :, t * 128:(t + 1) * 128], tT[:, 0:128], reads=[tT], writes=[COST])
                k.dma("sp", SINT[b, :, t * 128:(t + 1) * 128], tT[:, 128:256], reads=[tT], writes=[SINT])
        k.pop()

    if "s2b" in stages:
        W_IN2 = inp("w_in", [D, IN_COLS]) if "w_in" not in cx.inputs else W_IN
        k.push()
        hTp = [k.sb("hTp%d" % i, [128, 32, 512], BF16) for i in range(2)]
        hpq = [[T(k, hTp[i][:, :, jj * 128:(jj + 1) * 128], "hpq") for jj in range(4)] for i in range(2)]
        Wc = [[k.sb("Wc%d_%d" % (i, q), [128, 8, 512], BF16) for q in range(4)] for i in range(2)]
        uts = [k.sb("ut%d" % i, [128, 512], BF16) for i in range(4)]
        it = 0
        for b in range(NB):
            for pc in range(4):
                hp = hTp[(b * 4 + pc) % 2]
                for jj in range(4):
                    k.dma("sp", hp[:, :, jj * 128:(jj + 1) * 128], HT[b, pc * 4 + jj].rearrange("p (k n) -> p k n", n=128), reads=[HT], writes=[hpq[(b * 4 + pc) % 2][jj]])
                for ch in range(4):
                    w = Wc[it % 2]
                    it += 1
                    c0 = TMC + ch * 512
                    for q in range(4):
                        k.dma("pool", w[q][:], W_IN2[q * 1024:(q + 1) * 1024, c0:c0 + 512].rearrange("(k p) n -> p k n", p=128), writes=[w[q]])
                    for sub in range(4):
                        blk = ch * 4 + sub
                        ps = P[blk % 4]
                        for kc in range(32):
                            k.mm(ps[:, :], w[kc // 8][:, kc % 8, sub * 128:(sub + 1) * 128], hp[:, kc, :], kc == 0, kc == 31, [w[kc // 8]] + hpq[(b * 4 + pc) % 2], [ps])
                        ut = uts[blk % 4]
                        evac(ut[:], ps[:, :], [ps], [ut])
                        k.dma("sp", UT[b, blk, :, pc * 512:(pc + 1) * 512], ut[:], reads=[ut], writes=[UT])
        k.pop()

    if "s3" in stages:
        W_UQX = inp("w_uqx", [QR, NH * 256])
        W_UKV = inp("w_ukv", [KVR, NH * 256])
        GMLA = inp("gmla_L", [128, 16])
        k.push()
        gmla = k.sb("gmla", [128, 16], F32)
        k.dma("sp", gmla[:], GMLA[:, :], writes=[gmla])
        rsm = k.sb("rsm", [128, NT], F32)
        ssm_ = k.sb("ssm_", [128, NT], F32)
        for b in range(NB):
            k.push()
            qnT = k.sb("qnT", [128, 6, SEQ], BF16)
            kvnT = k.sb("kvnT", [128, 4, SEQ], BF16)
            krT = k.sb("krT", [64, SEQ], BF16)
            cosT = k.sb("cosT", [64, SEQ], F32)
            sinT = k.sb("sinT", [64, SEQ], F32)
            k.dma("sp", qnT[:], QNT[b].rearrange("k p n -> p k n"), reads=[QNT], writes=[qnT])
            k.dma("sp", kvnT[:], KVNT[b].rearrange("k p n -> p k n"), reads=[KVNT], writes=[kvnT])
            k.dma("sp", krT[:], KRT[b], reads=[KRT], writes=[krT])
            k.dma("sp", cosT[:], COST[b], reads=[COST], writes=[cosT])
            k.dma("sp", sinT[:], SINT[b], reads=[SINT], writes=[sinT])
            Om = [k.sb("Om%d" % t, [128, NH * 128], BF16) for t in range(16)]
            wqs = [k.sb("wq%d" % i, [128, 6, 256], BF16) for i in range(2)]
            wkvs = [k.sb("wkv%d" % i, [128, 4, 256], BF16) for i in range(2)]
            qTn = k.sb("qTn", [128, SEQ], BF16)
            qTr = k.sb("qTr", [64, SEQ], BF16)
            kTn = k.sb("kTn", [128, SEQ], BF16)
            vhs = [k.sb("vh%d" % i, [128, 16, 129], BF16) for i in range(2)]
            for v_ in vhs:
                k.ve("memset", [], [v_], eng="pool", ap=v_[:, :, 128:129], constant=1.0)
            PTs = [k.sb("PT%d" % i, [128, 512], BF16) for i in range(4)]
            rt1 = k.sb("rt1", [64, 512], F32)
            rt2 = k.sb("rt2", [64, 512], F32)
            rinv = k.sb("rinv", [128, 4], F32)
            pti = 0
            for h in range(NH):
                wq, wkv, vh = wqs[h % 2], wkvs[h % 2], vhs[h % 2]
                k.dma("pool", wq[:], W_UQX[:, h * 256:(h + 1) * 256].rearrange("(k p) n -> p k n", p=128), writes=[wq])
                k.dma("pool", wkv[:], W_UKV[:, h * 256:(h + 1) * 256].rearrange("(k p) n -> p k n", p=128), writes=[wkv])
                for pc in range(4):
                    sl = slice(pc * 512, (pc + 1) * 512)
                    for kc in range(6):
                        k.mm(P[0][:, :], wq[:, kc, 0:128], qnT[:, kc, sl], kc == 0, kc == 5, [wq, qnT], [P[0]])
                    for kc in range(6):
                        k.mm(P[1][0:64, :], wq[:, kc, 128:192], qnT[:, kc, sl], kc == 0, kc == 5, [wq, qnT], [P[1]])
                    for kc in range(6):
                        k.mm(P[2][0:64, :], wq[:, kc, 192:256], qnT[:, kc, sl], kc == 0, kc == 5, [wq, qnT], [P[2]])
                    k.op("act", lambda e, o=qTn[:, sl], i=P[0][:, :]: e.copy(out=o, in_=i), [P[0]], [qTn])
                    k.ve("tensor_tensor", [P[1], cosT], [rt1], out=rt1[:], in0=P[1][0:64, :], in1=cosT[:, sl], op=ALU.mult)
                    k.ve("tensor_tensor", [P[2], sinT], [rt2], out=rt2[:], in0=P[2][0:64, :], in1=sinT[:, sl], op=ALU.mult)
                    k.ve("tensor_tensor", [rt1, rt2], [qTr], out=qTr[:, sl], in0=rt1[:], in1=rt2[:], op=ALU.add)
                    for kc in range(4):
                        k.mm(P[3][:, :], wkv[:, kc, 0:128], kvnT[:, kc, sl], kc == 0, kc == 3, [wkv, kvnT], [P[3]])
                    k.op("act", lambda e, o=kTn[:, sl], i=P[3][:, :]: e.copy(out=o, in_=i), [P[3]], [kTn])
                for tg in range(4):
                    ps = P[tg % 2]
                    for j in range(4):
                        t = tg * 4 + j
                        for kc in range(4):
                            k.mm(ps[:, j * 128:(j + 1) * 128], kvnT[:, kc, t * 128:(t + 1) * 128], wkv[:, kc, 128:256], kc == 0, kc == 3, [kvnT, wkv], [ps])
                    evac(vh[:, tg * 4:(tg + 1) * 4, 0:128], ps[:, :].rearrange("p (j d) -> p j d", d=128), [ps], [vh])
                for p4 in range(4):
                    nkc = 4 * p4 + 4
                    O = [P[4 + i] for i in range(4)]
                    for kc in range(nkc):
                        j = kc - 4 * p4
                        jj = max(j, 0)
                        q0 = jj * 128
                        S = P[kc % 2]
                        qs = slice(p4 * 512 + q0, (p4 + 1) * 512)
                        k.mm(S[:, q0:512], kTn[:, kc * 128:(kc + 1) * 128], qTn[:, qs], True, False, [kTn, qTn], [S])
                        k.mm(S[:, q0:512], krT[:, kc * 128:(kc + 1) * 128], qTr[:, qs], False, True, [krT, qTr], [S])
                        PT = PTs[pti % 4]
                        pti += 1
                        k.act(PT[:, q0:512], S[:, q0:512], AF.Exp, [S], [PT], scale=ATT_SCALE)
                        if j >= 0:
                            k.ve("memset", [], [PT], eng="pool", ap=PT[64:128, q0:q0 + 64], constant=0.0)
                        for i in range(jj, 4):
                            k.mm(O[i][:, 0:129], PT[:, i * 128:(i + 1) * 128], vh[:, kc, :], kc == 0, kc == 4 * p4 + i, [PT, vh], [O[i]])
                    for i in range(4):
                        gi = 4 * p4 + i
                        k.ve("reciprocal", [O[i]], [rinv], out=rinv[:, i:i + 1], in_=O[i][:, 128:129])
                        k.ve("tensor_scalar", [O[i], rinv], [Om[gi]], out=Om[gi][:, h * 128:(h + 1) * 128], in0=O[i][:, 0:128], scalar1=rinv[:, i:i + 1], scalar2=None, op0=ALU.mult)
            junk = k.sb("junk3", [128, NH * 128], BF16)
            omT = [k.sb("omT%d" % i, [128, 1024], BF16) for i in range(2)]
            for t in range(16):
                g = b * 16 + t
                k.act(junk[:], Om[t][:], AF.Square, [Om[t]], [junk, ssm_], accum_out=ssm_[:, g:g + 1])
            k.act(rsm[:, b * 16:(b + 1) * 16], ssm_[:, b * 16:(b + 1) * 16], AF.Sqrt, [ssm_, epsT], [rsm], scale=1.0 / 2048, bias=epsT[:])
            k.ve("reciprocal", [rsm], [rsm], out=rsm[:, b * 16:(b + 1) * 16], in_=rsm[:, b * 16:(b + 1) * 16])
            it = 0
            for kc in range(16):
                for tg in range(2):
                    bank = it % 4
                    o_ = omT[it % 2]
                    it += 1
                    for j in range(8):
                        t = tg * 8 + j
                        k.tr(PB[bank][:, j * 128:(j + 1) * 128], Om[t][:, kc * 128:(kc + 1) * 128], identb[:], [Om[t], identb], [P[bank]])
                    k.ve("tensor_scalar", [P[bank], gmla], [o_], out=o_[:], in0=PB[bank][:, 0:1024], scalar1=gmla[:, kc:kc + 1], scalar2=None, op0=ALU.mult)
                    k.dma("sp", OMT[b, kc, :, tg * 1024:(tg + 1) * 1024], o_[:], reads=[o_], writes=[OMT])
            k.pop()
        k.dma("sp", RSM[:, :], rsm[:], reads=[rsm], writes=[RSM])
        k.pop()

    def bc3(ap2, n):
        return ap2.unsqueeze(2).to_broadcast([ap2.shape[0], ap2.shape[1], n])

    if "s4" in stages:
        k.push()
        LRE1 = inp("lamre_T2", [128, NG]); LIM1 = inp("lamim_T2", [128, NG]); LDT1 = inp("logdt_bc", [128, NG])
        BRE2 = inp("bre_L2", [128, 16, NST]); BIM2 = inp("bim_L2", [128, 16, NST])
        LRE2 = inp("lamre_L2", [128, 16, NST]); LIM2 = inp("lamim_L2", [128, 16, NST]); LDT2 = inp("logdt_L2", [128, 16])
        CL3 = inp("c_L3", [128, NG * 16]); DL = inp("d_L", [128, 16])
        MASK8 = inp("mask8", [128, 8]); PSWI = inp("psw", [128, 128]); IOTA = inp("iota_t", [128, SEQ])
        TH = k.sb("TH", [128, NG], F32); RR = k.sb("RR", [128, NG], F32)
        BT = k.sb("BT", [128, 16, 256], BF16)
        CS = k.sb("CS", [128, NG * 16], BF16)
        mask8 = k.sb("mask8", [128, 8], F32); psw = k.sb("psw", [128, 128], F32)
        iot = k.sb("iot", [128, SEQ], F32); dL = k.sb("dL", [128, 16], F32)
        k.dma("sp", mask8[:], MASK8[:, :], writes=[mask8]); k.dma("sp", psw[:], PSWI[:, :], writes=[psw])
        k.dma("sp", iot[:], IOTA[:, :], writes=[iot]); k.dma("sp", dL[:], DL[:, :], writes=[dL])
        k.push()
        a1 = k.sb("a1", [128, NG], F32); a2 = k.sb("a2", [128, NG], F32); a3 = k.sb("a3", [128, NG], F32)
        k.dma("sp", a1[:], LDT1[:, :], writes=[a1]); k.dma("sp", a2[:], LIM1[:, :], writes=[a2]); k.dma("sp", a3[:], LRE1[:, :], writes=[a3])
        k.act(a1[:], a1[:], AF.Exp, [a1], [a1])
        k.ve("tensor_tensor", [a2, a1], [TH], out=TH[:], in0=a2[:], in1=a1[:], op=ALU.mult)
        k.ve("tensor_tensor", [a3, a1], [a3], out=a3[:], in0=a3[:], in1=a1[:], op=ALU.mult)
        k.act(RR[:], a3[:], AF.Exp, [a3], [RR])
        sh3 = [128, 16, NST]
        lre = k.sb("lre", sh3, F32); lim = k.sb("lim", sh3, F32); bre = k.sb("bre", sh3, F32); bim = k.sb("bim", sh3, F32)
        dt2 = k.sb("dt2", [128, 16], F32)
        for t_, src in ((lre, LRE2), (lim, LIM2), (bre, BRE2), (bim, BIM2)):
            k.dma("sp", t_[:], src[:, :, :], writes=[t_])
        k.dma("sp", dt2[:], LDT2[:, :], writes=[dt2])
        k.act(dt2[:], dt2[:], AF.Exp, [dt2], [dt2])
        mag = k.sb("mag", sh3, F32); th = k.sb("th", sh3, F32); ti2 = k.sb("ti2", sh3, I32); tf2 = k.sb("tf2", sh3, F32)
        sn2 = k.sb("sn2", sh3, F32); cs2 = k.sb("cs2", sh3, F32)
        k.ve("tensor_tensor", [lre, dt2], [mag], out=mag[:], in0=lre[:], in1=bc3(dt2[:, :], NST), op=ALU.mult)
        k.act(mag[:], mag[:], AF.Exp, [mag], [mag])
        k.ve("tensor_tensor", [lim, dt2], [th], out=th[:], in0=lim[:], in1=bc3(dt2[:, :], NST), op=ALU.mult)
        range_reduce(th, ti2, tf2)
        k.act(sn2[:], th[:], AF.Sin, [th], [sn2])
        k.act(cs2[:], tf2[:], AF.Sin, [tf2, hpiT], [cs2], scale=-1.0, bias=hpiT[:])
        nr = k.sb("nr", sh3, F32); ni = k.sb("ni", sh3, F32); den = k.sb("den", sh3, F32); u1 = k.sb("u1", sh3, F32)
        fre = k.sb("fre", sh3, F32); fim = k.sb("fim", sh3, F32)
        TT = lambda o, a, b_, op: k.ve("tensor_tensor", [a, b_], [o], out=o[:], in0=a[:], in1=b_[:], op=op)
        TT(nr, mag, cs2, ALU.mult)
        k.ve("tensor_scalar", [nr], [nr], out=nr[:], in0=nr[:], scalar1=-1.0, scalar2=None, op0=ALU.add)
        TT(ni, mag, sn2, ALU.mult)
        TT(den, lre, lre, ALU.mult); TT(u1, lim, lim, ALU.mult); TT(den, den, u1, ALU.add)
        k.ve("reciprocal", [den], [den], out=den[:], in_=den[:])
        TT(fre, nr, lre, ALU.mult); TT(u1, ni, lim, ALU.mult); TT(fre, fre, u1, ALU.add); TT(fre, fre, den, ALU.mult)
        TT(fim, ni, lre, ALU.mult); TT(u1, nr, lim, ALU.mult); TT(fim, fim, u1, ALU.subtract); TT(fim, fim, den, ALU.mult)
        TT(nr, fre, bre, ALU.mult); TT(u1, fim, bim, ALU.mult)
        k.ve("tensor_tensor", [nr, u1], [BT], out=BT[:, :, 0:64], in0=nr[:], in1=u1[:], op=ALU.subtract)
        k.ve("tensor_tensor", [u1, nr], [BT], out=BT[:, :, 192:256], in0=u1[:], in1=nr[:], op=ALU.subtract)
        TT(ni, fre, bim, ALU.mult); TT(u1, fim, bre, ALU.mult)
        k.ve("tensor_tensor", [ni, u1], [BT], out=BT[:, :, 64:128], in0=ni[:], in1=u1[:], op=ALU.add)
        k.ve("tensor_tensor", [ni, u1], [BT], out=BT[:, :, 128:192], in0=ni[:], in1=u1[:], op=ALU.add)
        ctmp = k.sb("ctmp", [128, NG * 16], F32)
        k.dma("sp", ctmp[:], CL3[:, :], writes=[ctmp])
        k.ve("tensor_copy", [ctmp], [CS], out=CS[0:64, :], in_=ctmp[0:64, :])
        k.ve("tensor_scalar", [ctmp], [CS], out=CS[64:128, :], in0=ctmp[64:128, :], scalar1=-1.0, scalar2=None, op0=ALU.mult)
        k.pop()
        LBs = [k.sb("LB%d" % i, [128, 8, 256], BF16) for i in range(2)]
        LCs = [k.sb("LC%d" % i, [128, 8, 128], BF16) for i in range(2)]
        for l_ in LCs:
            k.ve("memset", [], [l_], eng="pool", ap=l_[:], constant=0.0)
        COSs = [k.sb("COS%d" % i, [128, SEQ], F32) for i in range(2)]
        SINs = [k.sb("SIN%d" % i, [128, SEQ], F32) for i in range(2)]
        ang = k.sb("angS", [128, SEQ], F32); tiS = k.sb("tiS", [128, SEQ], I32); tfS = k.sb("tfS", [128, SEQ], F32)
        uTs = [[k.sb("uT%d_%d" % (i, b), [128, SEQ], BF16) for b in range(NB)] for i in range(2)]
        yacc = [k.sb("yacc%d" % b, [128, SEQ], BF16) for b in range(NB)]
        ring = lambda nm, dt_, n=2: [k.sb("%s%d" % (nm, i), [128, 512], dt_) for i in range(n)]
        t1s, t2s, t3s, t4s, hhs = ring("t1_", BF16, 4), ring("t2_", BF16, 4), ring("t3_", BF16, 4), ring("t4_", BF16, 4), ring("hh", F32)
        yts = ring("yt", BF16, 4)
        hsws = ring("hsw", BF16, 4)
        gy = k.sb("gy", [128, 1024], F32); gy2 = k.sb("gy2", [128, 1024], F32); gin_ = k.sb("gin", [128, 1024], F32)
        gts = [k.sb("gt%d" % i, [128, 1024], BF16) for i in range(2)]
        yaccs = [yacc, [k.sb("yaccB%d" % b, [128, SEQ], BF16) for b in range(NB)]]
        gti = [0]

        def prep_block(gb):
            LB, LC, uT = LBs[gb % 2], LCs[gb % 2], uTs[gb % 2]
            k.ve("tensor_tensor", [BT, mask8], [LB], out=LB[:], in0=BT[:, gb, :].unsqueeze(1).to_broadcast([128, 8, 256]),
                 in1=bc3(mask8[:, :], 256), op=ALU.mult)
            for gl in range(8):
                g = gb * 8 + gl
                k.ve("tensor_copy", [CS], [LC], eng="pool", out=LC[:, gl, gl * 16:(gl + 1) * 16], in_=CS[:, g * 16:(g + 1) * 16])
            for b in range(NB):
                k.dma("sp", uT[b][:], UT[b, gb], reads=[UT], writes=[uT[b]])

        def table_steps(g):
            COS, SIN = COSs[g % 2], SINs[g % 2]
            st = []
            st.append(lambda: k.act(ang[:], iot[:], AF.Copy, [iot, TH], [ang], scale=TH[:, g:g + 1]))
            st.append(lambda: k.ve("tensor_scalar", [ang], [tiS], out=tiS[:], in0=ang[:], scalar1=1.0 / TWO_PI, scalar2=None, op0=ALU.mult))
            st.append(lambda: k.op("act", lambda e: e.copy(out=tfS[:], in_=tiS[:]), [tiS], [tfS]))
            st.append(lambda: k.ve("scalar_tensor_tensor", [tfS, ang], [ang], out=ang[:], in0=tfS[:], scalar=-C1, in1=ang[:], op0=ALU.mult, op1=ALU.add))
            st.append(lambda: k.ve("scalar_tensor_tensor", [tfS, ang], [ang], out=ang[:], in0=tfS[:], scalar=-C2, in1=ang[:], op0=ALU.mult, op1=ALU.add))
            st.append(lambda: k.act(tfS[:], ang[:], AF.Abs, [ang], [tfS]))
            st.append(lambda: k.act(SIN[:], ang[:], AF.Sin, [ang], [SIN], scale=0.999))
            st.append(lambda: k.act(COS[:], tfS[:], AF.Sin, [tfS, hpiT], [COS], scale=-0.999, bias=hpiT[:]))
            return st

        iters = [(gb, gl, b, pc) for gb in range(16) for gl in range(8) for b in range(NB) for pc in range(4)]
        NI = len(iters)

        def ctx_of(n):
            gb, gl, b, pc = iters[n]
            g = gb * 8 + gl
            r_ = n % 4
            return dict(gb=gb, gl=gl, b=b, pc=pc, g=g, r=r_, pa=P[n % 3], pb=P[3], ya_ps=P[4 + pc], sl=slice(pc * 512, (pc + 1) * 512),
                        LB=LBs[gb % 2], LC=LCs[gb % 2], uT=uTs[gb % 2][b], COS=COSs[g % 2], SIN=SINs[g % 2], hh=hhs[pc % 2],
                        yacc=yaccs[gb % 2][b])

        def stA(n):
            c = ctx_of(n)
            t1, t2 = t1s[c["r"]], t2s[c["r"]]
            k.mm(c["pa"][:, :], c["LB"][:, c["gl"], 0:128], c["uT"][:, c["sl"]], True, True, [c["LB"], c["uT"]], [c["pa"]])
            k.mm(c["pb"][:, :], c["LB"][:, c["gl"], 128:256], c["uT"][:, c["sl"]], True, True, [c["LB"], c["uT"]], [c["pb"]])
            k.ve("tensor_tensor", [c["pa"], c["COS"]], [t1], out=t1[:], in0=c["pa"][:, :], in1=c["COS"][:, c["sl"]], op=ALU.mult)
            k.ve("tensor_tensor", [c["pb"], c["SIN"]], [t2], out=t2[:], in0=c["pb"][:, :], in1=c["SIN"][:, c["sl"]], op=ALU.mult)
            k.mm(c["pa"][:, :], identb[:, :], t1[:, :], True, False, [identb, t1], [c["pa"]])
            k.mm(c["pa"][:, :], identb[:, :], t2[:, :], False, True, [identb, t2], [c["pa"]])

        def stB(n):
            c = ctx_of(n)
            t3, hh, pc = t3s[c["r"]], c["hh"], c["pc"]
            init = 0.0 if pc == 0 else hhs[(pc - 1) % 2][:, 511:512]
            rd = [RR, c["pa"]] + ([] if pc == 0 else [hhs[(pc - 1) % 2]])
            k.ve("tensor_tensor_scan", rd, [hh], out=hh[:], data0=RR[:, c["g"]:c["g"] + 1].to_broadcast([128, 512]), data1=c["pa"][:, :],
                 initial=init, op0=ALU.mult, op1=ALU.add)
            k.mm(c["pa"][:, :], psw[:, :], hh[:, :], True, True, [psw, hh], [c["pa"]])
            k.ve("tensor_tensor", [hh, c["COS"]], [t3], eng="pool", out=t3[:], in0=hh[:], in1=c["COS"][:, c["sl"]], op=ALU.mult)

        def stC(n):
            c = ctx_of(n)
            t3, t4 = t3s[c["r"]], t4s[c["r"]]
            hs_ = hsws[c["r"]]
            k.op("act", lambda e, o=hs_[:], i=c["pa"][:, :]: e.copy(out=o, in_=i), [c["pa"]], [hs_])
            k.ve("tensor_tensor", [hs_, c["SIN"]], [t4], eng="pool", out=t4[:], in0=hs_[:], in1=c["SIN"][:, c["sl"]], op=ALU.mult)
            k.mm(c["ya_ps"][:, :], c["LC"][:, c["gl"], :], t3[:, :], c["gl"] == 0, False, [c["LC"], t3], [c["ya_ps"]])
            k.mm(c["ya_ps"][:, :], c["LC"][:, c["gl"], :], t4[:, :], False, c["gl"] == 7, [c["LC"], t4], [c["ya_ps"]])
            if c["gl"] == 7:
                finish_piece(c["gb"], c["b"], c["pc"])

        def finish_piece(gb, b, pc):
            assert NB == 1
            uT = uTs[gb % 2][b]
            sl = slice(pc * 512, (pc + 1) * 512)
            hs = slice(0, 512)
            gt = gts[gti[0] % 2]
            gti[0] += 1
            k.ve("scalar_tensor_tensor", [uT, dL, P[4 + pc]], [gy], out=gy[:, hs], in0=uT[:, sl], scalar=dL[:, gb:gb + 1], in1=P[4 + pc][:, :], op0=ALU.mult, op1=ALU.add)
            k.ve("tensor_tensor", [gy], [gy2], out=gy2[:, hs], in0=gy[:, hs], in1=gy[:, hs], op=ALU.mult)
            k.ve("tensor_scalar", [gy2], [gy2], out=gy2[:, hs], in0=gy2[:, hs], scalar1=0.044715, scalar2=1.0, op0=ALU.mult, op1=ALU.add)
            k.ve("tensor_tensor", [gy2, gy], [gin_], eng="pool", out=gin_[:, hs], in0=gy2[:, hs], in1=gy[:, hs], op=ALU.mult)
            k.act(gin_[:, hs], gin_[:, hs], AF.Sigmoid, [gin_], [gin_], scale=1.5957691216057308)
            k.ve("tensor_tensor", [gin_, gy], [gt], eng="pool", out=gt[:, hs], in0=gin_[:, hs], in1=gy[:, hs], op=ALU.mult)
            k.dma("sp", GT[b, gb, :, sl], gt[:, hs], reads=[gt], writes=[GT])

        prep_block(0)
        for f in table_steps(0):
            f()
        per_grp = NB * 4
        pending = []
        for n in range(NI + 2):
            if n < NI:
                gb, gl, b, pc = iters[n]
                g = gb * 8 + gl
                if b == 0 and pc == 0:
                    if gl == 4 and gb + 1 < 16:
                        prep_block(gb + 1)
                    pending = table_steps(g + 1) if g + 1 < NG else []
                stA(n)
            if 0 <= n - 1 < NI:
                stB(n - 1)
            if 0 <= n - 2 < NI:
                stC(n - 2)
            if n < NI:
                pos = (iters[n][2] * 4 + iters[n][3])
                lo = len(pending) * pos // per_grp if pending else 0
                if pos == 0:
                    cx.tsteps = pending
                K_ = len(cx.tsteps)
                for i_ in range(K_ * pos // per_grp, K_ * (pos + 1) // per_grp):
                    cx.tsteps[i_]()
        k.pop()

    if "s5" in stages:
        W_GLU = inp("w_glu", [SSMW, SSMW])
        BGLU = inp("bglu_L", [128, 16]); GSSM = inp("gssm_L", [128, 16])
        k.push()
        bglu = k.sb("bglu", [128, 16], F32); gssm = k.sb("gssm", [128, 16], F32)
        k.dma("sp", bglu[:], BGLU[:, :], writes=[bglu]); k.dma("sp", gssm[:], GSSM[:, :], writes=[gssm])
        onesb = k.sb("onesb", [128, 1], BF16)
        k.ve("memset", [], [onesb], eng="pool", ap=onesb[:], constant=1.0)
        rss = k.sb("rss", [128, NT], F32)
        gTk = [k.sb("gTk%d" % i, [128, SEQ], BF16) for i in range(16)]
        wgs = [k.sb("wg%d" % i, [128, 16, 128], BF16) for i in range(2)]
        sgs = [k.sb("sg%d" % i, [128, 512], F32) for i in range(2)]
        ors = [k.sb("or%d" % i, [128, 512], F32) for i in range(2)]
        sqs = [k.sb("sq%d" % i, [128, 512], BF16) for i in range(2)]
        osts = [k.sb("ost%d" % i, [128, 512], BF16) for i in range(2)]
        for b in range(NB):
            for kc in range(16):
                k.dma("sp", gTk[kc][:], GT[b, kc], reads=[GT], writes=[gTk[kc]])
            it = 0
            for cb in range(16):
                wg = wgs[cb % 2]
                k.dma("pool", wg[:], W_GLU[:, cb * 128:(cb + 1) * 128].rearrange("(k p) n -> p k n", p=128), writes=[wg])
                for pc in range(4):
                    sl = slice(pc * 512, (pc + 1) * 512)
                    r_ = it % 2
                    it += 1
                    ps = P[r_]
                    for kc in range(16):
                        k.mm(ps[:, :], wg[:, kc, :], gTk[kc][:, sl], kc == 0, kc == 15, [wg, gTk[kc]], [ps])
                    sg, orr, sq, ost = sgs[r_], ors[r_], sqs[r_], osts[r_]
                    k.act(sg[:], ps[:, :], AF.Sigmoid, [ps, bglu], [sg], bias=bglu[:, cb:cb + 1])
                    k.ve("tensor_tensor", [sg, gTk[cb]], [orr], out=orr[:], in0=sg[:], in1=gTk[cb][:, sl], op=ALU.mult)
                    k.act(sq[:], orr[:], AF.Square, [orr], [sq])
                    for j in range(4):
                        t = pc * 4 + j
                        first = (cb == 0 and pc == 0 and j == 0)
                        k.mm(P[7][:, t:t + 1], sq[:, j * 128:(j + 1) * 128], onesb[:, :], first, cb == 15, [sq, onesb], [P[7]], skip_group_check=True)
                    k.ve("tensor_scalar", [orr, gssm], [ost], out=ost[:], in0=orr[:], scalar1=gssm[:, cb:cb + 1], scalar2=None, op0=ALU.mult)
                    k.dma("sp", OST[b, cb, :, sl], ost[:], reads=[ost], writes=[OST])
            k.act(rss[:, b * 16:(b + 1) * 16], P[7][:, 0:16], AF.Sqrt, [P[7], epsT], [rss], scale=1.0 / SSMW, bias=epsT[:])
            k.ve("reciprocal", [rss], [rss], out=rss[:, b * 16:(b + 1) * 16], in_=rss[:, b * 16:(b + 1) * 16])
        k.dma("sp", RSS[:, :], rss[:], reads=[rss], writes=[RSS])
        k.pop()

    if "s6" in stages:
        W_OUT = inp("w_out", [D, D])
        k.push()
        rsm6 = k.sb("rsm6", [128, NT], F32); rss6 = k.sb("rss6", [128, NT], F32)
        k.dma("sp", rsm6[:], RSM[:, :], reads=[RSM], writes=[rsm6]); k.dma("sp", rss6[:], RSS[:, :], reads=[RSS], writes=[rss6])
        GA = k.sb("GA", [128, D], F32)
        oTm = [k.sb("oTm%d" % i, [128, 16, 512], BF16) for i in range(2)]
        oTs = [k.sb("oTs%d" % i, [128, 16, 512], BF16) for i in range(2)]
        Wo = [[k.sb("Wo%d_%d" % (i, q), [128, 8, 512], BF16) for q in range(4)] for i in range(2)]
        xins = [k.sb("xin%d" % i, [128, 512], F32) for i in range(2)]
        tts = [k.sb("tt%d" % i, [128, 512], F32) for i in range(2)]
        it = 0
        wi = 0
        for b in range(NB):
            load_mod_vec(GA, b, 2)
            for tg in range(4):
                om, os_ = oTm[(b * 4 + tg) % 2], oTs[(b * 4 + tg) % 2]
                tsl = slice(tg * 512, (tg + 1) * 512)
                k.dma("sp", om[:], OMT[b, :, :, tsl].rearrange("k p n -> p k n"), reads=[OMT], writes=[om])
                k.dma("sp", os_[:], OST[b, :, :, tsl].rearrange("k p n -> p k n"), reads=[OST], writes=[os_])
                for cc in range(8):
                    w = Wo[wi % 2]
                    wi += 1
                    csl = slice(cc * 512, (cc + 1) * 512)
                    for q in range(4):
                        k.dma("pool", w[q][:], W_OUT[q * 1024:(q + 1) * 1024, csl].rearrange("(k p) n -> p k n", p=128), writes=[w[q]])
                    for j in range(4):
                        tile = tg * 4 + j
                        g = b * 16 + tile
                        r0 = b * SEQ + tile * 128
                        r_ = it % 2
                        it += 1
                        pa, pb_ = P[2 * r_], P[2 * r_ + 1]
                        for kc in range(16):
                            k.mm(pa[:, :], om[:, kc, j * 128:(j + 1) * 128], w[kc // 8][:, kc % 8, :], kc == 0, kc == 15, [om, w[kc // 8]], [pa])
                        for kc in range(16):
                            k.mm(pb_[:, :], os_[:, kc, j * 128:(j + 1) * 128], w[2 + kc // 8][:, kc % 8, :], kc == 0, kc == 15, [os_, w[2 + kc // 8]], [pb_])
                        xin, tt = xins[r_], tts[r_]
                        k.dma("sp", xin[:], X_IN[r0:r0 + 128, csl], writes=[xin])
                        k.ve("tensor_scalar", [pa, rsm6], [tt], out=tt[:], in0=pa[:, :], scalar1=rsm6[:, g:g + 1], scalar2=None, op0=ALU.mult)
                        k.ve("scalar_tensor_tensor", [pb_, rss6, tt], [tt], out=tt[:], in0=pb_[:, :], scalar=rss6[:, g:g + 1], in1=tt[:], op0=ALU.mult, op1=ALU.add)
                        k.ve("tensor_tensor", [tt, GA], [tt], out=tt[:], in0=tt[:], in1=GA[:, csl], op=ALU.mult)
                        k.ve("tensor_tensor", [tt, xin], [tt], out=tt[:], in0=tt[:], in1=xin[:], op=ALU.add)
                        k.dma("sp", X1[r0:r0 + 128, csl], tt[:], reads=[tt], writes=[X1])
        k.pop()

    if "s7" in stages:
        G_FFN = inp("g_ffn", [1, D])
        W_R = inp("w_r", [D, 72]); B_R = inp("b_r", [1, 72])
        TRI = inp("tri", [128, 128]); JB = inp("jb", [128, NBLK]); PIDX = inp("pidx", [128, 1])
        k.push()
        sel12 = k.sb("sel12", [128, NT, 2, 64], BF16)
        gates = k.sb("gates", [128, NT * 2], F32)
        slots = k.sb("slots", [128, NT * 2], I32)
        WR = k.sb("WR", [128, 32, 72], F32)
        k.dma("sp", WR[:], W_R[:, :].rearrange("(k p) n -> p k n", p=128), writes=[WR])
        br = k.sb("br", [1, 72], F32); ones1f = k.sb("ones1f", [1, 128], F32)
        k.dma("sp", br[:], B_R[:, :], writes=[br])
        k.ve("memset", [], [ones1f], eng="pool", ap=ones1f[:], constant=1.0)
        onesB = k.sb("onesB", [128, 128], BF16); trib = k.sb("trib", [128, 128], BF16); trif = k.sb("trif", [128, 128], F32)
        k.ve("memset", [], [onesB], eng="pool", ap=onesB[:], constant=1.0)
        k.dma("sp", trif[:], TRI[:, :], writes=[trif])
        k.ve("tensor_copy", [trif], [trib], out=trib[:], in_=trif[:])
        k.push()
        Af = k.sb("Af", [128, D], F32); SHf = k.sb("SHf", [128, D], F32); gtmp = k.sb("gtmp7", [128, D], F32)
        xts = [k.sb("x1t%d" % i, [128, D], F32) for i in range(2)]
        junk = k.sb("junk7", [128, D], BF16)
        h2bs = [k.sb("h2b%d" % i, [128, D], BF16) for i in range(2)]
        h2T = k.sb("h2T", [128, 32, 128], F32)
        ss = k.sb("ss7", [128, 1], F32); rstd = k.sb("rstd7", [128, 1], F32)
        lg = k.sb("lg", [128, 72], F32); m8 = k.sb("m8", [128, 8], F32); oh = k.sb("oh", [128, 8], F32)
        ngm = k.sb("ngm", [128, 1], F32); ex = k.sb("ex", [128, 8], F32); se = k.sb("se", [128, 1], F32)
        pen = k.sb("pen", [128, 8], F32); em = k.sb("em", [128, 8, 8], F32); t8 = k.sb("t8", [128, 8], F32)
        dl = k.sb("dl", [128, 1], F32); wv = k.sb("wv", [128, 2], F32); ssum = k.sb("ssum", [128, 64], BF16)
        for b in range(NB):
            load_mod_vec(Af, b, 4)
            load_mod_vec(SHf, b, 3)
            make_A(Af, gtmp, G_FFN[0:1, :])
            for t in range(16):
                g = b * 16 + t
                r0 = g * 128
                xt, h2b = xts[g % 2], h2bs[g % 2]
                k.dma("sp", xt[:], X1[r0:r0 + 128, :], reads=[X1], writes=[xt])
                norm_mod_tile(xt, Af, SHf, xt, junk, ss, rstd)
                k.op("act", lambda e, o=h2b[:], i=xt[:]: e.copy(out=o, in_=i), [xt], [h2b])
                k.dma("sp", H2B[r0:r0 + 128, :], h2b[:], reads=[h2b], writes=[H2B])
                for rnd in range(2):
                    for q in range(4):
                        for j in range(4):
                            kc = rnd * 16 + q * 4 + j
                            k.tr(P[q][:, j * 128:(j + 1) * 128], xt[:, kc * 128:(kc + 1) * 128], identf[:], [xt, identf], [P[q]])
                        kc0 = rnd * 16 + q * 4
                        evac(h2T[:, kc0:kc0 + 4, :], P[q][:, :].rearrange("p (j n) -> p j n", n=128), [P[q]], [h2T])
                for kc in range(32):
                    k.mm(P[4][:, 0:72], h2T[:, kc, :], WR[:, kc, :], kc == 0, False, [h2T, WR], [P[4]])
                k.mm(P[4][:, 0:72], ones1f[:, :], br[:, :], False, True, [ones1f, br], [P[4]])
                k.ve("tensor_copy", [P[4]], [lg], out=lg[:], in_=P[4][:, 0:72])
                k.ve("max", [lg], [m8], out=m8[:], in_=lg[:, 0:8])
                k.ve("tensor_scalar", [lg, m8], [oh], out=oh[:], in0=lg[:, 0:8], scalar1=m8[:, 0:1], scalar2=None, op0=ALU.is_equal)
                k.ve("tensor_scalar", [m8], [ngm], out=ngm[:], in0=m8[:, 0:1], scalar1=-1.0, scalar2=None, op0=ALU.mult)
                k.act(ex[:], lg[:, 0:8], AF.Exp, [lg, ngm], [ex, se], bias=ngm[:], accum_out=se[:])
                k.ve("reciprocal", [se], [se], out=se[:], in_=se[:])
                k.ve("tensor_scalar", [oh], [pen], out=pen[:], in0=oh[:], scalar1=1e30, scalar2=-1e30, op0=ALU.mult, op1=ALU.add)
                k.ve("tensor_tensor", [lg, pen], [em], out=em[:], in0=lg[:, 8:72].rearrange("p (a b) -> p a b", b=8), in1=bc3(pen[:, :], 8), op=ALU.add)
                emf = em[:].rearrange("p a b -> p (a b)")
                k.ve("max", [em], [t8], out=t8[:], in_=emf)
                k.ve("tensor_scalar", [em, t8], [sel12], out=sel12[:, g, 0, :], in0=emf, scalar1=t8[:, 0:1], scalar2=None, op0=ALU.is_equal)
                k.ve("tensor_scalar", [em, t8], [sel12], out=sel12[:, g, 1, :], in0=emf, scalar1=t8[:, 1:2], scalar2=None, op0=ALU.is_equal)
                k.ve("tensor_tensor", [t8], [dl], out=dl[:], in0=t8[:, 0:1], in1=t8[:, 1:2], op=ALU.subtract)
                k.act(wv[:, 0:1], dl[:], AF.Sigmoid, [dl], [wv])
                k.act(wv[:, 1:2], dl[:], AF.Sigmoid, [dl, wv], [wv], scale=-1.0)
                k.ve("tensor_scalar", [wv, se], [gates], out=gates[:, 2 * g:2 * g + 2], in0=wv[:], scalar1=se[:, 0:1], scalar2=None, op0=ALU.mult)
                k.ve("tensor_tensor", [sel12], [ssum], out=ssum[:], in0=sel12[:, g, 0, :], in1=sel12[:, g, 1, :], op=ALU.add)
                k.mm(P[7][:, 0:64], onesB[:, :], ssum[:, :], g == 0, g == NT - 1, [onesB, ssum], [P[7]])
        k.pop()
        k.push()
        cnt = k.sb("cnt", [128, 64], F32); pad = k.sb("pad", [128, 64], F32); padi = k.sb("padi", [128, 64], I32)
        pends = k.sb("pends", [128, 64], F32); base = k.sb("base", [128, 64], F32); one64 = k.sb("one64", [128, 64], F32)
        k.ve("memset", [], [one64], eng="pool", ap=one64[:], constant=1.0)
        k.ve("tensor_copy", [P[7]], [cnt], out=cnt[:], in_=P[7][:, 0:64])
        k.ve("tensor_scalar", [cnt], [padi], out=padi[:], in0=cnt[:], scalar1=1.0 / BLK, scalar2=(BLK - 1.0) / BLK - 0.5 + 0.5 / BLK, op0=ALU.mult, op1=ALU.add)
        k.ve("tensor_copy", [padi], [pad], out=pad[:], in_=padi[:])
        k.ve("tensor_scalar", [pad], [pad], out=pad[:], in0=pad[:], scalar1=float(BLK), scalar2=None, op0=ALU.mult)
        k.ve("tensor_tensor_scan", [one64, pad], [pends], out=pends[:], data0=one64[:], data1=pad[:], initial=0.0, op0=ALU.mult, op1=ALU.add)
        k.ve("tensor_tensor", [pends, pad], [base], out=base[:], in0=pends[:], in1=pad[:], op=ALU.subtract)
        jb = k.sb("jb", [128, NBLK], F32); pidx = k.sb("pidx", [128, 1], F32)
        k.dma("sp", jb[:], JB[:, :], writes=[jb]); k.dma("sp", pidx[:], PIDX[:, :], writes=[pidx])
        bexp = k.sb("bexp", [128, NBLK], F32); iw = k.sb("iw", [128, NBLK], I32)
        CH = 48
        cmp_ = k.sb("cmp", [128, CH, 64], F32)
        for c0 in range(0, NBLK, CH):
            n = min(CH, NBLK - c0)
            k.ve("tensor_tensor", [pends, jb], [cmp_], out=cmp_[:, 0:n, :], in0=pends[:, :].unsqueeze(1).to_broadcast([128, n, 64]),
                 in1=bc3(jb[:, c0:c0 + n], 64), op=ALU.is_le)
            k.ve("tensor_reduce", [cmp_], [bexp], out=bexp[:, c0:c0 + n], in_=cmp_[:, 0:n, :], axis=AX.X, op=ALU.add)
        k.ve("tensor_scalar", [bexp], [bexp], out=bexp[:], in0=bexp[:], scalar1=63.0, scalar2=128.0, op0=ALU.min, op1=ALU.mult)
        tl = k.sb("tl", [128, NBLK], F32)
        k.ve("tensor_scalar", [jb, pends], [tl], out=tl[:], in0=jb[:], scalar1=pends[:, 63:64], scalar2=1.0e7, op0=ALU.is_ge, op1=ALU.mult)
        k.ve("tensor_tensor", [bexp, tl], [bexp], out=bexp[:], in0=bexp[:], in1=tl[:], op=ALU.add)
        k.ve("tensor_scalar", [bexp, pidx], [iw], out=iw[:], in0=bexp[:], scalar1=pidx[:, 0:1], scalar2=None, op0=ALU.add)
        k.dma("sp", IWD[:, :], iw[:], reads=[iw], writes=[IWD])
        h2bs = [k.sb("h2c%d" % i, [128, D], BF16) for i in range(2)]
        ssum = k.sb("ssumB", [128, 64], BF16); vv = k.sb("vv", [128, 64], F32); tmp = k.sb("tmpB", [128, 64], F32)
        df = k.sb("df", [128, 2], F32)
        for g in range(NT):
            h2b = h2bs[g % 2]
            r0 = g * 128
            k.dma("sp", h2b[:], H2B[r0:r0 + 128, :], reads=[H2B], writes=[h2b])
            k.ve("tensor_tensor", [sel12], [ssum], out=ssum[:], in0=sel12[:, g, 0, :], in1=sel12[:, g, 1, :], op=ALU.add)
            k.mm(P[5][:, 0:64], trib[:, :], ssum[:, :], True, True, [trib, ssum], [P[5]])
            k.mm(P[6][:, 0:64], onesB[:, :], ssum[:, :], True, True, [onesB, ssum], [P[6]])
            k.ve("tensor_tensor", [P[5], base], [vv], out=vv[:], in0=P[5][:, 0:64], in1=base[:], op=ALU.add)
            k.ve("tensor_tensor", [P[6], base], [base], out=base[:], in0=P[6][:, 0:64], in1=base[:], op=ALU.add)
            for s_ in range(2):
                k.ve("tensor_tensor", [sel12, vv], [tmp], out=tmp[:], in0=sel12[:, g, s_, :], in1=vv[:], op=ALU.mult)
                k.ve("tensor_reduce", [tmp], [df], out=df[:, s_:s_ + 1], in_=tmp[:], axis=AX.X, op=ALU.add)
            k.ve("tensor_copy", [df], [slots], out=slots[:, 2 * g:2 * g + 2], in_=df[:])
            for s_ in range(2):
                k.scatter(XE[:, :], slots[:, 2 * g + s_:2 * g + s_ + 1], h2b[:], reads=[h2b, slots] + (cx.xez if (g == 0 and s_ == 0) else []), writes=[XE])
        k.dma("sp", SLOTS[:, :], slots[:], reads=[slots], writes=[SLOTS])
        k.dma("sp", GATES[:, :], gates[:], reads=[gates], writes=[GATES])
        k.pop()
        k.pop()

    if "s8" in stages:
        W1P = inp("w1p", [NE * 128 * 8, 2048]); W3P = inp("w3p", [NE * 128 * 8, 2048]); W2P = inp("w2p", [NE * 128 * 8, 2048])
        k.push()
        iw0 = k.sb("iw0", [128, NBLK], I32); iwf = k.sb("iwf", [128, NBLK], F32); iwg = k.sb("iwg", [128, NBLK], F32)
        k.dma("sp", iw0[:], IWD[:, :], reads=[IWD], writes=[iw0])
        k.ve("tensor_copy", [iw0], [iwf], out=iwf[:], in_=iw0[:])
        iwc = [k.sb("iwc%d" % c, [128, NBLK], I32) for c in range(8)]
        for c in range(8):
            k.ve("tensor_scalar", [iwf], [iwg], out=iwg[:], in0=iwf[:], scalar1=8.0, scalar2=float(c), op0=ALU.mult, op1=ALU.add)
            k.ve("tensor_copy", [iwg], [iwc[c]], out=iwc[c][:], in_=iwg[:])
        w1c = [[k.sb("w1c%d_%d" % (i, c), [128, 2048], BF16) for c in range(8)] for i in range(2)]
        w3c = [[k.sb("w3c%d_%d" % (i, c), [128, 2048], BF16) for c in range(8)] for i in range(2)]
        w2c = [k.sb("w2c%d" % c, [128, 2048], BF16) for c in range(8)]
        xe = k.sb("xe", [128, D], BF16)
        xT = [k.sb("xT%d" % q, [128, 8, 128], BF16) for q in range(4)]
        a_sb = k.sb("a_sb", [128, 512], F32); act_ = k.sb("act_", [128, 512], BF16); aT = k.sb("aT", [128, 4, 128], BF16)
        yes = [k.sb("ye%d" % i, [128, 2048], BF16) for i in range(2)]
        for j in range(NBLK):
            r_ = j % 2
            for c in range(8):
                k.gather(w1c[r_][c][:], W1P[:, :], iwc[c][:, j:j + 1], reads=[iwc[c], w1c[r_][c]], writes=[w1c[r_][c]], bound=NE * 128 * 8 - 1)
            for c in range(8):
                k.gather(w3c[r_][c][:], W3P[:, :], iwc[c][:, j:j + 1], reads=[iwc[c], w3c[r_][c]], writes=[w3c[r_][c]], bound=NE * 128 * 8 - 1)
            for c in range(8):
                k.gather(w2c[c][:], W2P[:, :], iwc[c][:, j:j + 1], reads=[iwc[c], w2c[c]], writes=[w2c[c]], bound=NE * 128 * 8 - 1)
            for sub in range(BLKT):
                rr0 = (j * BLKT + sub) * 128
                k.dma("sp", xe[:], XE[rr0:rr0 + 128, :], reads=[XE], writes=[xe])
                for q in range(4):
                    for jj in range(8):
                        kc = q * 8 + jj
                        k.tr(PB[q][:, jj * 128:(jj + 1) * 128], xe[:, kc * 128:(kc + 1) * 128], identb[:], [xe, identb], [P[q]])
                    evac(xT[q][:], PB[q][:, 0:1024].rearrange("p (j n) -> p j n", n=128), [P[q]], [xT[q]])
                for kc in range(32):
                    k.mm(P[4][:, :], xT[kc // 8][:, kc % 8, :], w1c[r_][kc // 4][:, (kc % 4) * 512:(kc % 4 + 1) * 512], kc == 0, kc == 31, [xT[kc // 8], w1c[r_][kc // 4]], [P[4]])
                for kc in range(32):
                    k.mm(P[5][:, :], xT[kc // 8][:, kc % 8, :], w3c[r_][kc // 4][:, (kc % 4) * 512:(kc % 4 + 1) * 512], kc == 0, kc == 31, [xT[kc // 8], w3c[r_][kc // 4]], [P[5]])
                k.act(a_sb[:], P[4][:, :], AF.Silu, [P[4]], [a_sb])
                k.ve("tensor_tensor", [a_sb, P[5]], [act_], out=act_[:], in0=a_sb[:], in1=P[5][:, :], op=ALU.mult)
                for mc in range(4):
                    k.tr(PB[6][:, mc * 128:(mc + 1) * 128], act_[:, mc * 128:(mc + 1) * 128], identb[:], [act_, identb], [P[6]])
                evac(aT[:], PB[6][:, 0:512].rearrange("p (j n) -> p j n", n=128), [P[6]], [aT])
                for cc in range(8):
                    ps = P[cc % 4]
                    for kc in range(4):
                        k.mm(ps[:, :], aT[:, kc, :], w2c[kc * 2 + cc // 4][:, (cc % 4) * 512:(cc % 4 + 1) * 512], kc == 0, kc == 3, [aT, w2c[kc * 2 + cc // 4]], [ps])
                    ye = yes[cc // 4]
                    evac(ye[:, (cc % 4) * 512:(cc % 4 + 1) * 512], ps[:, :], [ps], [ye])
                    if cc % 4 == 3:
                        hf = cc // 4
                        k.dma("sp", YE[rr0:rr0 + 128, hf * 2048:(hf + 1) * 2048], ye[:], reads=[ye], writes=[YE])
        k.pop()

    if "s9" in stages:
        G_FIN = inp("g_fin", [1, D])
        k.push()
        slots9 = k.sb("slots9", [128, NT * 2], I32); gates9 = k.sb("gates9", [128, NT * 2], F32)
        k.dma("sp", slots9[:], SLOTS[:, :], reads=[SLOTS], writes=[slots9])
        k.dma("sp", gates9[:], GATES[:, :], reads=[GATES], writes=[gates9])
        GF = k.sb("GF", [128, D], F32); FG = k.sb("FG", [128, D], F32)
        bcast_row(FG, G_FIN[0:1, :])
        y1s = [k.sb("y1_%d" % i, [128, D], BF16) for i in range(2)]
        y2s = [k.sb("y2_%d" % i, [128, D], BF16) for i in range(2)]
        xts = [k.sb("x9_%d" % i, [128, D], F32) for i in range(2)]
        tts = [k.sb("t9_%d" % i, [128, D], F32) for i in range(2)]
        junk = k.sb("junk9", [128, D], BF16)
        ss = k.sb("ss9", [128, 1], F32); rstd = k.sb("rstd9", [128, 1], F32)
        for b in range(NB):
            load_mod_vec(GF, b, 5)
            for t in range(16):
                g = b * 16 + t
                r0 = g * 128
                y1, y2, xt, tt = y1s[g % 2], y2s[g % 2], xts[g % 2], tts[g % 2]
                k.gather(y1[:], YE[:, :], slots9[:, 2 * g:2 * g + 1], reads=[YE, slots9], writes=[y1])
                k.gather(y2[:], YE[:, :], slots9[:, 2 * g + 1:2 * g + 2], reads=[YE, slots9], writes=[y2])
                k.dma("sp", xt[:], X1[r0:r0 + 128, :], reads=[X1], writes=[xt])
                k.ve("tensor_scalar", [y1, gates9], [tt], out=tt[:], in0=y1[:], scalar1=gates9[:, 2 * g:2 * g + 1], scalar2=None, op0=ALU.mult)
                k.ve("scalar_tensor_tensor", [y2, gates9, tt], [tt], out=tt[:], in0=y2[:], scalar=gates9[:, 2 * g + 1:2 * g + 2], in1=tt[:], op0=ALU.mult, op1=ALU.add)
                k.ve("tensor_tensor", [tt, GF], [tt], out=tt[:], in0=tt[:], in1=GF[:], op=ALU.mult)
                k.ve("tensor_tensor", [tt, xt], [xt], out=xt[:], in0=tt[:], in1=xt[:], op=ALU.add)
                k.act(junk[:], xt[:], AF.Square, [xt], [junk, ss], accum_out=ss[:])
                rstd_from_ss(rstd, ss, D)
                k.ve("scalar_tensor_tensor", [xt, rstd, FG], [tt], out=tt[:], in0=xt[:], scalar=rstd[:, 0:1], in1=FG[:], op0=ALU.mult, op1=ALU.mult)
                k.dma("sp", OUT[r0:r0 + 128, :], tt[:], reads=[tt], writes=[OUT])
        k.pop()

    k.emit()
    cx.nc = nc
    return cx


def host_layouts(I, NB=NB_FULL):
    f = np.float32
    NT = NB * 16
    o = {}
    o["x"] = np.ascontiguousarray(I["x"][:NB].reshape(NB * SEQ, D))
    o["c"] = np.ascontiguousarray(I["c"][:NB])
    o["posT"] = np.ascontiguousarray(I["positions"][:NB].reshape(NT, 128).T.astype(np.int32))
    o["w_ada"] = I["w_ada"][0]
    o["b_ada"] = I["b_ada"][0].reshape(1, -1)
    o["g_mix"] = I["norm_mix_gain"][0].reshape(1, -1)
    o["g_ffn"] = I["norm_ffn_gain"][0].reshape(1, -1)
    o["g_fin"] = I["final_gain"].reshape(1, -1)
    o["w_in"] = I["w_in"][0]
    o["g_q"] = I["q_lat_gain"][0].reshape(1, -1)
    o["g_kv"] = I["kv_lat_gain"][0].reshape(1, -1)
    wq = I["w_uq"][0].reshape(QR, NH, 192)
    o["w_uqx"] = np.ascontiguousarray(np.concatenate(
        [wq[:, :, 0:192], wq[:, :, 160:192], wq[:, :, 128:160]], axis=2).reshape(QR, NH * 256))
    o["w_ukv"] = I["w_ukv"][0]
    lre, lim, ldt = I["ssm_lam_re"][0], I["ssm_lam_im"][0], I["ssm_log_dt"][0]
    o["lamre_T2"] = np.ascontiguousarray(np.concatenate([lre.T, lre.T], 0))
    o["lamim_T2"] = np.ascontiguousarray(np.concatenate([lim.T, lim.T], 0))
    o["logdt_bc"] = np.ascontiguousarray(np.broadcast_to(ldt[None, :], (128, NG)))

    def L2(a):
        return np.ascontiguousarray(a.reshape(16, 8, NST, 16).transpose(1, 3, 0, 2).reshape(128, 16, NST))
    o["bre_L2"] = L2(I["ssm_b_re"][0])
    o["bim_L2"] = L2(I["ssm_b_im"][0])
    o["lamre_L2"] = L2(np.broadcast_to(lre[:, :, None], (NG, NST, 16)))
    o["lamim_L2"] = L2(np.broadcast_to(lim[:, :, None], (NG, NST, 16)))
    o["logdt_L2"] = np.ascontiguousarray(np.broadcast_to(ldt.reshape(16, 8)[:, :, None], (16, 8, 16)).transpose(1, 2, 0).reshape(128, 16))
    cre, cim = I["ssm_c_re"][0], I["ssm_c_im"][0]
    o["c_L3"] = np.ascontiguousarray(np.concatenate(
        [cre.transpose(2, 0, 1).reshape(NST, NG * 16), cim.transpose(2, 0, 1).reshape(NST, NG * 16)], 0))
    o["d_L"] = np.ascontiguousarray(I["ssm_d"][0].reshape(16, 128).T)
    o["w_glu"] = I["w_glu"][0]
    o["bglu_L"] = np.ascontiguousarray(I["b_glu"][0].reshape(16, 128).T)
    o["gmla_L"] = np.ascontiguousarray(I["mla_out_gain"][0].reshape(16, 128).T)
    o["gssm_L"] = np.ascontiguousarray(I["ssm_out_gain"][0].reshape(16, 128).T)
    o["w_out"] = I["w_out"][0]
    o["w_r"] = np.ascontiguousarray(np.concatenate([I["w_group_router"][0], I["w_expert_router"][0]], 1))
    o["b_r"] = np.concatenate([I["b_group_router"][0], I["b_expert_router"][0]]).reshape(1, -1)
    o["w1p"] = lambda: np.ascontiguousarray(I["w1_experts"][0].reshape(NE, 32, 128, DE).transpose(0, 2, 1, 3)).reshape(NE * 128 * 8, 2048)
    o["w3p"] = lambda: np.ascontiguousarray(I["w3_experts"][0].reshape(NE, 32, 128, DE).transpose(0, 2, 1, 3)).reshape(NE * 128 * 8, 2048)
    o["w2p"] = lambda: np.ascontiguousarray(I["w2_experts"][0].reshape(NE, 4, 128, D).transpose(0, 2, 1, 3)).reshape(NE * 128 * 8, 2048)
    o["ident"] = np.eye(128, dtype=f)
    invf = np.exp(-math.log(10000.0) * np.arange(0, 64, 2, dtype=f) / 64).astype(f)
    o["invf_bc"] = np.ascontiguousarray(np.broadcast_to(invf[None, :], (128, 32)))
    m8 = np.zeros((128, 8), f)
    for gl in range(8):
        m8[gl * 16:(gl + 1) * 16, gl] = 1
    o["mask8"] = m8
    psw = np.zeros((128, 128), f)
    for p in range(64):
        psw[p + 64, p] = -1.0
        psw[p, p + 64] = 1.0
    o["psw"] = psw
    o["tri"] = np.triu(np.ones((128, 128), f), 1)
    NBLK = (NB * SEQ * 2 + NE * (BLK - 1)) // BLK
    o["jb"] = np.ascontiguousarray(np.broadcast_to((np.arange(NBLK, dtype=f) * BLK)[None, :], (128, NBLK)))
    o["pidx"] = np.arange(128, dtype=f).reshape(128, 1)
    o["iota_t"] = np.ascontiguousarray(np.broadcast_to(np.arange(SEQ, dtype=f)[None, :], (128, SEQ)))
    return o


_NPDT = {F32: np.float32, BF16: ml_dtypes.bfloat16, I32: np.int32}


N_CORES = 4


def kernel(**inputs):
    I = {k_: np.asarray(v) for k_, v in inputs.items()}
    cx = build(NB=1)
    shared = None
    in_maps = []
    for b in range(N_CORES):
        Ib = dict(I)
        Ib["x"] = I["x"][b:b + 1]
        Ib["c"] = I["c"][b:b + 1]
        Ib["positions"] = I["positions"][b:b + 1]
        if shared is None:
            H = host_layouts(Ib, 1)
            shared = {}
            for name, (shape, dt) in cx.inputs.items():
                if name in ("x", "c", "posT"):
                    continue
                v = H[name]() if callable(H[name]) else H[name]
                shared[name] = np.ascontiguousarray(np.asarray(v).astype(_NPDT[dt], copy=False)).reshape(shape)
        m = dict(shared)
        m["x"] = np.ascontiguousarray(Ib["x"].reshape(SEQ, D))
        m["c"] = np.ascontiguousarray(Ib["c"])
        m["posT"] = np.ascontiguousarray(Ib["positions"].reshape(16, 128).T.astype(np.int32))
        in_maps.append(m)
    res = run_bass_kernel_spmd(cx.nc, in_maps, core_ids=list(range(N_CORES)))
    out = np.stack([np.asarray(res.results[b]["out"], dtype=np.float32).reshape(SEQ, D) for b in range(N_CORES)], 0)
    return out
```

```python
import math
import numpy as np
import ml_dtypes
import concourse.bass as bass
import concourse.mybir as mybir
from contextlib import ExitStack
from concourse.bass_utils import run_bass_kernel_spmd

F32 = mybir.dt.float32
BF16 = mybir.dt.bfloat16
I32 = mybir.dt.int32
ALU = mybir.AluOpType
AF = mybir.ActivationFunctionType
AX = mybir.AxisListType

ENGS = ("pe", "dve", "act", "pool", "sp")
NDMASEM = 12
SB_BYTES = 200 * 1024


class T:
    def __init__(self, k, apv, name):
        self.k = k
        self.v = apv
        self.name = name
        self.writer = None
        self.readers = []
        self.birth = list(k.birth)

    def __getitem__(self, key):
        return self.v[key]


class Op:
    __slots__ = ("eng", "fn", "deps", "need_inc", "sem", "val", "is_dma")

    def __init__(self, eng, fn, is_dma=False):
        self.eng = eng
        self.fn = fn
        self.deps = []
        self.need_inc = False
        self.sem = None
        self.val = None
        self.is_dma = is_dma


class K:
    def __init__(self, nc):
        self.nc = nc
        self.es = ExitStack()
        self.ops = {e: [] for e in ENGS}
        self.birth = []
        self.last = {e: None for e in ENGS}
        self.dma_last = {}
        self.dma_ctr = {e: 0 for e in ENGS}
        self.big = self.es.enter_context(nc.sbuf_tensor("sbig", [128, SB_BYTES // 4], F32))
        self.bump = 0
        self.scopes = []
        self.nops = 0
        self.regcache = {}

    def sb(self, name, shape, dtype, parts=None):
        shape = list(shape)
        p = shape[0]
        n = int(np.prod(shape[1:]))
        esz = {F32: 4, BF16: 2, I32: 4}[dtype]
        nbytes = (n * esz + 31) // 32 * 32
        off = self.bump
        self.bump += nbytes
        assert self.bump <= SB_BYTES, "SBUF overflow %s %d" % (name, self.bump)
        v = self.big[0:p, off // 4:(off + nbytes) // 4]
        if dtype != F32:
            v = v.bitcast(dtype)
        v = v[:, 0:n]
        if len(shape) == 3:
            v = v.rearrange("p (a b) -> p a b", a=shape[1], b=shape[2])
        elif len(shape) == 4:
            v = v.rearrange("p (a b c) -> p a b c", a=shape[1], b=shape[2], c=shape[3])
        return T(self, v, name)

    def push(self):
        self.scopes.append(self.bump)

    def pop(self):
        self.bump = self.scopes.pop()
        self.barrier()

    def psum(self, name):
        h = self.es.enter_context(self.nc.psum_tensor(name, [128, 512], F32))
        return T(self, h[:], name)

    def dram(self, name, shape, dtype, kind=None):
        if kind is None:
            h = self.nc.dram_tensor(name, list(shape), dtype)
        else:
            h = self.nc.dram_tensor(name, list(shape), dtype, kind=kind)
        return T(self, h.ap(), name)

    def _add(self, op, reads, writes):
        deps = []
        for t in list(reads) + list(writes):
            deps.extend(t.birth)
        for t in reads:
            if t.writer is not None:
                deps.append(t.writer)
        for t in writes:
            if t.writer is not None:
                deps.append(t.writer)
            deps.extend(t.readers)
        seen = set(id(d) for d in op.deps)
        for d in deps:
            if d is op or id(d) in seen:
                continue
            seen.add(id(d))
            if d.eng == "pe" and op.eng == "pe" and not d.is_dma and not op.is_dma:
                continue
            op.deps.append(d)
            d.need_inc = True
        for t in reads:
            if not op.is_dma:
                t.readers = [r for r in t.readers if r.is_dma or r.eng != op.eng]
            t.readers.append(op)
        for t in writes:
            t.writer = op
            t.readers = []
        self.ops[op.eng].append(op)
        if not op.is_dma:
            self.last[op.eng] = op
        self.nops += 1
        return op

    def op(self, eng, fn, reads=(), writes=()):
        return self._add(Op(eng, fn), reads, writes)

    def _dma_op(self, eng, fn, reads, writes):
        op = Op(eng, fn, is_dma=True)
        slot = (eng, self.dma_ctr[eng] % NDMASEM)
        self.dma_ctr[eng] += 1
        prev = self.dma_last.get(slot)
        if prev is not None:
            op.deps.append(prev)
        op.need_inc = True
        op.sem = slot
        self.dma_last[slot] = op
        return self._add(op, reads, writes)

    def dma(self, eng, out, in_, reads=(), writes=(), **kw):
        return self._dma_op(eng, lambda e: e.dma_start(out=out, in_=in_, **kw), reads, writes)

    def scatter(self, out, idx_ap, in_, reads=(), writes=()):
        return self._dma_op("pool", lambda e: e.indirect_dma_start(
            out=out, out_offset=bass.IndirectOffsetOnAxis(ap=idx_ap, axis=0), in_=in_, in_offset=None), reads, writes)

    def gather(self, out, in_, idx_ap, reads=(), writes=(), bound=None):
        def fn(e):
            kw = {}
            if bound is not None:
                if bound not in self.regcache:
                    self.regcache[bound] = e.to_reg(bound)
                kw = dict(bounds_check=self.regcache[bound], oob_is_err=False)
            return e.indirect_dma_start(out=out, out_offset=None, in_=in_,
                                        in_offset=bass.IndirectOffsetOnAxis(ap=idx_ap, axis=0), **kw)
        return self._dma_op("pool", fn, reads, writes)

    def barrier(self):
        b = [o for o in self.last.values() if o is not None]
        b += list(self.dma_last.values())
        for o in b:
            o.need_inc = True
        self.birth = b

    def ve(self, name, reads, writes, eng="dve", **kw):
        return self.op(eng, lambda e: getattr(e, name)(**kw), reads, writes)

    def mm(self, out, lhsT, rhs, start, stop, reads, writes, **kw):
        return self.op("pe", lambda e: e.matmul(out, lhsT, rhs, start=start, stop=stop, **kw), reads, writes)

    def tr(self, out, in_, ident, reads, writes):
        return self.op("pe", lambda e: e.transpose(out, in_, ident), reads, writes)

    def act(self, out, in_, func, reads, writes, **kw):
        return self.op("act", lambda e: e.activation(out=out, in_=in_, func=func, **kw), reads, writes)

    def emit(self):
        nc = self.nc
        fin = Op("sp", None)
        fin.deps = list(self.dma_last.values()) + [o for o in self.last.values() if o is not None]
        for o in fin.deps:
            o.need_inc = True
        self.ops["sp"].append(fin)
        sems = {}
        for e in ENGS:
            sems[e] = self.es.enter_context(nc.semaphore("s_" + e))
            for i in range(NDMASEM):
                sems[(e, i)] = self.es.enter_context(nc.semaphore("d_%s%d" % (e, i)))
        cnt = {}
        for e in ENGS:
            for op in self.ops[e]:
                if op.is_dma:
                    cnt[op.sem] = cnt.get(op.sem, 0) + 16
                    op.val = cnt[op.sem]
                elif op.need_inc:
                    op.sem = e
                    cnt[e] = cnt.get(e, 0) + 1
                    op.val = cnt[e]
        self.sem_max = dict(cnt)
        engobj = {"pe": "tensor", "dve": "vector", "act": "scalar", "pool": "gpsimd", "sp": "sync"}
        block = self.es.enter_context(nc.Block())

        def make(e):
            def body(eng):
                waited = {}
                for op in self.ops[e]:
                    for d in op.deps:
                        if waited.get(d.sem, 0) < d.val:
                            eng.wait_ge(sems[d.sem], d.val)
                            waited[d.sem] = d.val
                    if op.fn is None:
                        continue
                    ins = op.fn(eng)
                    if op.is_dma:
                        ins.then_inc(sems[op.sem], 16)
                    elif op.need_inc:
                        ins.then_inc(sems[op.sem], 1)
            return body

        for e in ENGS:
            getattr(block, engobj[e])(make(e))
        self.es.close()


D = 4096
SEQ = 2048
NB_FULL = 4
QR, KVR, ROPE = 768, 512, 64
NH = 16
SSMW = 2048
NG = 128
NST = 64
IN_COLS = QR + KVR + ROPE + SSMW
TMC = QR + KVR + ROPE
NE = 64
DE = 512
EPS = 1e-6
BLKT = 2
BLK = BLKT * 128
TWO_PI = 2.0 * math.pi
C1 = 6.28125
C2 = float(TWO_PI - 6.28125)
ATT_SCALE = float((128 + 64) ** -0.5)
PI_LO = 3.1415925


class Cx:
    pass


def build(NB=NB_FULL, stages=None, inject=(), dump=()):
    allst = ["mod", "s1", "s2a", "s2b", "s3", "s4", "s5", "s6", "s7", "s8", "s9"]
    stages = set(allst if stages is None else stages)
    NT = NB * 16
    NTOK = NB * SEQ
    NBLK = (NTOK * 2 + NE * (BLK - 1)) // BLK
    nc = bass.Bass("TRN2", target_bir_lowering=False)
    k = K(nc)
    cx = Cx()
    cx.k, cx.NB, cx.NT = k, NB, NT
    cx.inputs = {}

    def inp(name, shape, dtype=F32):
        t = k.dram(name, shape, dtype, kind="ExternalInput")
        cx.inputs[name] = (tuple(shape), dtype)
        return t

    def scratch(name, shape, dtype):
        if name in inject:
            return inp(name, shape, dtype)
        if name in dump:
            return k.dram(name, shape, dtype, kind="ExternalOutput")
        return k.dram(name, shape, dtype)

    MOD = scratch("MOD", [NB, 6 * D], F32)
    HT = scratch("HT", [NB, 16, 128, 32 * 128], BF16)
    QNT = scratch("QNT", [NB, 6, 128, SEQ], BF16)
    KVNT = scratch("KVNT", [NB, 4, 128, SEQ], BF16)
    KRT = scratch("KRT", [NB, 64, SEQ], BF16)
    COST = scratch("COST", [NB, 64, SEQ], F32)
    SINT = scratch("SINT", [NB, 64, SEQ], F32)
    UT = scratch("UT", [NB, 16, 128, SEQ], BF16)
    OMT = scratch("OMT", [NB, 16, 128, SEQ], BF16)
    GT = scratch("GT", [NB, 16, 128, SEQ], BF16)
    RSM = scratch("RSM", [128, NT], F32)
    RSS = scratch("RSS", [128, NT], F32)
    OST = scratch("OST", [NB, 16, 128, SEQ], BF16)
    X1 = scratch("X1", [NTOK, D], F32)
    XE = scratch("XE", [NBLK * BLK, D], BF16)
    YE = scratch("YE", [NBLK * BLK, D], BF16)
    SLOTS = scratch("SLOTS", [128, NT * 2], I32)
    GATES = scratch("GATES", [128, NT * 2], F32)
    H2B = scratch("H2B", [NTOK, D], BF16)
    IWD = scratch("IWD", [128, NBLK], I32)
    OUT = k.dram("out", [NTOK, D], F32, kind="ExternalOutput")

    P = [k.psum("ps%d" % i) for i in range(8)]
    PB = [p_[:].bitcast(BF16) for p_ in P]

    identf = k.sb("identf", [128, 128], F32)
    identb = k.sb("identb", [128, 128], BF16)
    epsT = k.sb("epsT", [128, 1], F32)
    hpiT = k.sb("hpiT", [128, 1], F32)
    IDENT = inp("ident", [128, 128])
    k.dma("sp", identf[:], IDENT[:, :], writes=[identf])
    k.ve("tensor_copy", [identf], [identb], out=identb[:], in_=identf[:])
    k.ve("memset", [], [epsT], eng="pool", ap=epsT[:], constant=EPS)
    k.ve("memset", [], [hpiT], eng="pool", ap=hpiT[:], constant=math.pi / 2)
    cx.alt = 0
    cx.zf_done = False
    def emit_zero_fill():
        zt = k.sb("zt", [128, 2048], BF16)
        k.ve("memset", [], [zt], eng="pool", ap=zt[:], constant=0.0)
        cx.xez = []
        cx.zf_done = True
        for j in range(NBLK * BLKT):
            for hf in range(2):
                tz = T(k, XE.v, "xez")
                cx.xez.append(tz)
                k.dma("act", XE[j * 128:(j + 1) * 128, hf * 2048:(hf + 1) * 2048], zt[:], reads=[zt], writes=[tz])


    def evac(out, in_, reads, writes):
        cx.alt ^= 1
        if cx.alt:
            k.op("act", lambda e: e.copy(out=out, in_=in_), reads, writes)
        else:
            k.ve("tensor_copy", reads, writes, out=out, in_=in_)

    def rstd_from_ss(rstd, ss, n, reads_extra=()):
        k.act(rstd[:], ss[:], AF.Sqrt, [ss, epsT], [rstd], scale=1.0 / n, bias=epsT[:])
        k.ve("reciprocal", [rstd], [rstd], out=rstd[:], in_=rstd[:])

    def range_reduce(ang, ti, tf, act_cvt=False):
        k.ve("tensor_scalar", [ang], [ti], out=ti[:], in0=ang[:], scalar1=1.0 / TWO_PI, scalar2=None, op0=ALU.mult)
        if act_cvt:
            k.op("act", lambda e: e.copy(out=tf[:], in_=ti[:]), [ti], [tf])
        else:
            k.ve("tensor_copy", [ti], [tf], out=tf[:], in_=ti[:])
        k.ve("scalar_tensor_tensor", [tf, ang], [ang], out=ang[:], in0=tf[:], scalar=-C1, in1=ang[:], op0=ALU.mult, op1=ALU.add)
        k.ve("scalar_tensor_tensor", [tf, ang], [ang], out=ang[:], in0=tf[:], scalar=-C2, in1=ang[:], op0=ALU.mult, op1=ALU.add)
        k.ve("tensor_scalar", [ang], [ang], out=ang[:], in0=ang[:], scalar1=PI_LO, scalar2=-PI_LO, op0=ALU.min, op1=ALU.max)
        k.ve("scalar_tensor_tensor", [ang], [tf], out=tf[:], in0=ang[:], scalar=-1.0, in1=ang[:], op0=ALU.mult, op1=ALU.max)

    def bcast_row(dst, src_row_ap, eng="sp"):
        k.dma(eng, dst[:], src_row_ap.to_broadcast([128, src_row_ap.shape[-1]]), writes=[dst])

    if "mod" in stages:
        C_IN = inp("c", [NB, D])
        W_ADA = inp("w_ada", [D, 6 * D])
        B_ADA = inp("b_ada", [1, 6 * D])
        k.push()
        c4 = k.sb("c4", [NB, D], F32)
        k.dma("sp", c4[:], C_IN[:, :], writes=[c4])
        k.act(c4[:], c4[:], AF.Silu, [c4], [c4])
        for kc in range(32):
            k.tr(P[0][:, kc * NB:(kc + 1) * NB], c4[:, kc * 128:(kc + 1) * 128], identf[0:NB, 0:NB], [c4, identf], [P[0]])
        cT = k.sb("cT", [128, 32, NB], BF16)
        k.ve("tensor_copy", [P[0]], [cT], out=cT[:], in_=P[0][:, 0:32 * NB].rearrange("p (k b) -> p k b", b=NB))
        if "s7" in stages:
            emit_zero_fill()
        ones1 = k.sb("ones1", [1, NB], F32)
        k.ve("memset", [], [ones1], eng="pool", ap=ones1[:], constant=1.0)
        wq = [[k.sb("wa%d_%d" % (i, q), [128, 8, 512], BF16) for q in range(4)] for i in range(2)]
        bch = [k.sb("bch%d" % i, [1, 512], F32) for i in range(2)]
        mo = [k.sb("mo%d" % i, [NB, 512], F32) for i in range(2)]
        for ch in range(48):
            w = wq[ch % 2]
            for q in range(4):
                k.dma("pool", w[q][:], W_ADA[q * 1024:(q + 1) * 1024, ch * 512:(ch + 1) * 512].rearrange("(k p) n -> p k n", p=128), writes=[w[q]])
            bb = bch[ch % 2]
            k.dma("sp", bb[:], B_ADA[0:1, ch * 512:(ch + 1) * 512], writes=[bb])
            ps = P[1 + ch % 2]
            for kc in range(32):
                k.mm(ps[0:NB, :], cT[:, kc, :], w[kc // 8][:, kc % 8, :], kc == 0, False, [cT, w[kc // 8]], [ps])
            k.mm(ps[0:NB, :], ones1[:, :], bb[:, :], False, True, [ones1, bb], [ps])
            m = mo[ch % 2]
            k.ve("tensor_copy", [ps], [m], out=m[:], in_=ps[0:NB, :])
            k.dma("sp", MOD[:, ch * 512:(ch + 1) * 512], m[:], reads=[m], writes=[MOD])
        k.pop()

    if "s7" in stages and not cx.zf_done:
        emit_zero_fill()
    X_IN = inp("x", [NTOK, D])

    def load_mod_vec(dst, b, idx, eng="sp"):
        bcast_row(dst, MOD[b:b + 1, idx * D:(idx + 1) * D], eng)

    def norm_mod_tile(xt, A, SH, out_t, junk, ss, rstd):
        k.act(junk[:], xt[:], AF.Square, [xt], [junk, ss], accum_out=ss[:])
        rstd_from_ss(rstd, ss, D)
        k.ve("scalar_tensor_tensor", [xt, rstd, A], [xt], out=xt[:], in0=xt[:], scalar=rstd[:, 0:1], in1=A[:], op0=ALU.mult, op1=ALU.mult)
        k.ve("tensor_tensor", [xt, SH], [out_t], out=out_t[:], in0=xt[:], in1=SH[:], op=ALU.add)

    def make_A(A, gtmp, grow_ap):
        bcast_row(gtmp, grow_ap)
        k.ve("scalar_tensor_tensor", [A, gtmp], [A], out=A[:], in0=A[:], scalar=1.0, in1=gtmp[:], op0=ALU.add, op1=ALU.mult)

    if "s1" in stages:
        G_MIX = inp("g_mix", [1, D])
        for b in range(NB):
            k.push()
            Aa = k.sb("Aa", [128, D], F32)
            SHa = k.sb("SHa", [128, D], F32)
            gtmp = k.sb("gtmp", [128, D], F32)
            load_mod_vec(Aa, b, 1)
            load_mod_vec(SHa, b, 0)
            make_A(Aa, gtmp, G_MIX[0:1, :])
            xts = [k.sb("xt%d" % i, [128, D], F32) for i in range(2)]
            hbs = [k.sb("hb%d" % i, [128, D], BF16) for i in range(2)]
            junk = k.sb("junk", [128, D], BF16)
            ss = k.sb("ss", [128, 1], F32)
            rstd = k.sb("rstd", [128, 1], F32)
            hTq = [[k.sb("hT%d_%d" % (i, q), [128, 8, 128], BF16) for q in range(4)] for i in range(2)]
            for t in range(16):
                xt, hb, hT = xts[t % 2], hbs[t % 2], hTq[t % 2]
                r0 = b * SEQ + t * 128
                k.dma("sp", xt[:], X_IN[r0:r0 + 128, :], writes=[xt])
                norm_mod_tile(xt, Aa, SHa, hb, junk, ss, rstd)
                for q in range(4):
                    for j in range(8):
                        kc = q * 8 + j
                        k.tr(PB[q][:, j * 128:(j + 1) * 128], hb[:, kc * 128:(kc + 1) * 128], identb[:], [hb, identb], [P[q]])
                    evac(hT[q][:], PB[q][:, 0:1024].rearrange("p (j n) -> p j n", n=128), [P[q]], [hT[q]])
                    k.dma("sp", HT[b, t, :, q * 1024:(q + 1) * 1024], hT[q][:].rearrange("p k n -> p (k n)"), reads=[hT[q]], writes=[HT])
            k.pop()

    if "s2a" in stages:
        W_IN = inp("w_in", [D, IN_COLS])
        G_Q = inp("g_q", [1, QR])
        G_KV = inp("g_kv", [1, KVR])
        INVF = inp("invf_bc", [128, 32])
        POST = inp("posT", [128, NT], I32)
        k.push()
        Wtm = [k.sb("Wtm%d" % q, [128, 8, TMC], BF16) for q in range(4)]
        for q in range(4):
            k.dma("pool", Wtm[q][:], W_IN[q * 1024:(q + 1) * 1024, 0:TMC].rearrange("(k p) n -> p k n", p=128), writes=[Wtm[q]])
        gq = k.sb("gq", [128, QR], F32)
        gkv = k.sb("gkv", [128, KVR], F32)
        bcast_row(gq, G_Q[0:1, :])
        bcast_row(gkv, G_KV[0:1, :])
        invf = k.sb("invf", [128, 32], F32)
        k.dma("sp", invf[:], INVF[:, :], writes=[invf])
        posi = k.sb("posi", [128, NT], I32)
        posf = k.sb("posf", [128, NT], F32)
        k.dma("sp", posi[:], POST[:, :], writes=[posi])
        k.ve("tensor_copy", [posi], [posf], out=posf[:], in_=posi[:])
        hTs = [k.sb("hTt%d" % i, [128, 32, 128], BF16) for i in range(2)]
        zs = [k.sb("z%d" % i, [128, TMC], F32) for i in range(2)]
        junk = k.sb("junk2", [128, QR], BF16)
        ss = k.sb("ss2", [128, 2], F32)
        rs = k.sb("rs2", [128, 2], F32)
        qn = k.sb("qn", [128, QR], BF16)
        kvn = k.sb("kvn", [128, KVR], BF16)
        qnT = [k.sb("qnT%d" % i, [128, 6, 128], BF16) for i in range(2)]
        kvnT = [k.sb("kvnT%d" % i, [128, 4, 128], BF16) for i in range(2)]
        ang = k.sb("ang", [128, 32], F32)
        ti = k.sb("ti", [128, 32], I32)
        tf = k.sb("tf", [128, 32], F32)
        sn = k.sb("sn", [128, 32], F32)
        cs = k.sb("cs", [128, 32], F32)
        t1 = k.sb("t1", [128, 32], F32)
        t2 = k.sb("t2", [128, 32], F32)
        kr = k.sb("kr", [128, 64], BF16)
        krT = [k.sb("krT%d" % i, [64, 128], BF16) for i in range(2)]
        tabc = k.sb("tabc", [128, 64], F32)
        tabs = k.sb("tabs", [128, 64], F32)
        tabT = [k.sb("tabT%d" % i, [64, 256], F32) for i in range(2)]
        ssq, ssk = ss[:, 0:1], ss[:, 1:2]
        for b in range(NB):
            for t in range(16):
                g = b * 16 + t
                hT, z = hTs[t % 2], zs[t % 2]
                k.dma("sp", hT[:], HT[b, t].rearrange("p (k n) -> p k n", n=128), reads=[HT], writes=[hT])
                for bank, c0, c1 in ((0, 0, 512), (1, 512, 1024), (2, 1024, TMC)):
                    for kc in range(32):
                        k.mm(P[bank][:, 0:c1 - c0], hT[:, kc, :], Wtm[kc // 8][:, kc % 8, c0:c1], kc == 0, kc == 31, [hT, Wtm[kc // 8]], [P[bank]])
                    evac(z[:, c0:c1], P[bank][:, 0:c1 - c0], [P[bank]], [z])
                k.act(junk[:], z[:, 0:QR], AF.Square, [z], [junk, ss], accum_out=ssq)
                k.act(junk[:, 0:KVR], z[:, QR:QR + KVR], AF.Square, [z, junk], [junk, ss], accum_out=ssk)
                k.act(rs[:, 0:1], ssq, AF.Sqrt, [ss, epsT], [rs], scale=1.0 / QR, bias=epsT[:])
                k.act(rs[:, 1:2], ssk, AF.Sqrt, [ss, epsT, rs], [rs], scale=1.0 / KVR, bias=epsT[:])
                k.ve("reciprocal", [rs], [rs], out=rs[:], in_=rs[:])
                k.ve("scalar_tensor_tensor", [z, rs, gq], [qn], out=qn[:], in0=z[:, 0:QR], scalar=rs[:, 0:1], in1=gq[:], op0=ALU.mult, op1=ALU.mult)
                k.ve("scalar_tensor_tensor", [z, rs, gkv], [kvn], out=kvn[:], in0=z[:, QR:QR + KVR], scalar=rs[:, 1:2], in1=gkv[:], op0=ALU.mult, op1=ALU.mult)
                qT, kvT = qnT[t % 2], kvnT[t % 2]
                for j in range(6):
                    k.tr(PB[3][:, j * 128:(j + 1) * 128], qn[:, j * 128:(j + 1) * 128], identb[:], [qn, identb], [P[3]])
                evac(qT[:], PB[3][:, 0:768].rearrange("p (j n) -> p j n", n=128), [P[3]], [qT])
                k.dma("sp", QNT[b, :, :, t * 128:(t + 1) * 128].rearrange("k p n -> p k n"), qT[:], reads=[qT], writes=[QNT])
                for j in range(4):
                    k.tr(PB[4][:, j * 128:(j + 1) * 128], kvn[:, j * 128:(j + 1) * 128], identb[:], [kvn, identb], [P[4]])
                evac(kvT[:], PB[4][:, 0:512].rearrange("p (j n) -> p j n", n=128), [P[4]], [kvT])
                k.dma("sp", KVNT[b, :, :, t * 128:(t + 1) * 128].rearrange("k p n -> p k n"), kvT[:], reads=[kvT], writes=[KVNT])
                k.ve("tensor_scalar", [invf, posf], [ang], out=ang[:], in0=invf[:], scalar1=posf[:, g:g + 1], scalar2=None, op0=ALU.mult)
                range_reduce(ang, ti, tf)
                k.act(sn[:], ang[:], AF.Sin, [ang], [sn])
                k.act(cs[:], tf[:], AF.Sin, [tf, hpiT], [cs], scale=-1.0, bias=hpiT[:])
                x1, x2 = z[:, QR + KVR:QR + KVR + 32], z[:, QR + KVR + 32:TMC]
                k.ve("tensor_tensor", [z, cs], [t1], out=t1[:], in0=x1, in1=cs[:], op=ALU.mult)
                k.ve("tensor_tensor", [z, sn], [t2], out=t2[:], in0=x2, in1=sn[:], op=ALU.mult)
                k.ve("tensor_tensor", [t1, t2], [kr], out=kr[:, 0:32], in0=t1[:], in1=t2[:], op=ALU.subtract)
                k.ve("tensor_tensor", [z, cs], [t1], out=t1[:], in0=x2, in1=cs[:], op=ALU.mult)
                k.ve("tensor_tensor", [z, sn], [t2], out=t2[:], in0=x1, in1=sn[:], op=ALU.mult)
                k.ve("tensor_tensor", [t1, t2], [kr], out=kr[:, 32:64], in0=t1[:], in1=t2[:], op=ALU.add)
                kT = krT[t % 2]
                k.tr(PB[5][0:64, 0:128], kr[:, :], identb[:], [kr, identb], [P[5]])
                evac(kT[:], PB[5][0:64, 0:128], [P[5]], [kT])
                k.dma("sp", KRT[b, :, t * 128:(t + 1) * 128], kT[:], reads=[kT], writes=[KRT])
                k.ve("tensor_copy", [cs], [tabc], out=tabc[:, 0:32], in_=cs[:])
                k.ve("tensor_copy", [cs], [tabc], out=tabc[:, 32:64], in_=cs[:])
                k.ve("tensor_scalar", [sn], [tabs], out=tabs[:, 0:32], in0=sn[:], scalar1=-1.0, scalar2=None, op0=ALU.mult)
                k.ve("tensor_copy", [sn], [tabs], out=tabs[:, 32:64], in_=sn[:])
                tT = tabT[t % 2]
                k.tr(P[6][0:64, 0:128], tabc[:, :], identf[:], [tabc, identf], [P[6]])
                k.tr(P[6][0:64, 128:256], tabs[:, :], identf[:], [tabs, identf], [P[6]])
                evac(tT[:], P[6][0:64, 0:256], [P[6]], [tT])
                k.dma("sp", COST[b, :, t * 128:(t + 1) * 128], tT[:, 0:128], reads=[tT], writes=[COST])
                k.dma("sp", SINT[b, :, t * 128:(t + 1) * 128], tT[:, 128:256], reads=[tT], writes=[SINT])
        k.pop()

    if "s2b" in stages:
        W_IN2 = inp("w_in", [D, IN_COLS]) if "w_in" not in cx.inputs else W_IN
        k.push()
        hTp = [k.sb("hTp%d" % i, [128, 32, 512], BF16) for i in range(2)]
        hpq = [[T(k, hTp[i][:, :, jj * 128:(jj + 1) * 128], "hpq") for jj in range(4)] for i in range(2)]
        Wc = [[k.sb("Wc%d_%d" % (i, q), [128, 8, 512], BF16) for q in range(4)] for i in range(2)]
        uts = [k.sb("ut%d" % i, [128, 512], BF16) for i in range(4)]
        it = 0
        for b in range(NB):
            for pc in range(4):
                hp = hTp[(b * 4 + pc) % 2]
                for jj in range(4):
                    k.dma("sp", hp[:, :, jj * 128:(jj + 1) * 128], HT[b, pc * 4 + jj].rearrange("p (k n) -> p k n", n=128), reads=[HT], writes=[hpq[(b * 4 + pc) % 2][jj]])
                for ch in range(4):
                    w = Wc[it % 2]
                    it += 1
                    c0 = TMC + ch * 512
                    for q in range(4):
                        k.dma("pool", w[q][:], W_IN2[q * 1024:(q + 1) * 1024, c0:c0 + 512].rearrange("(k p) n -> p k n", p=128), writes=[w[q]])
                    for sub in range(4):
                        blk = ch * 4 + sub
                        ps = P[blk % 4]
                        for kc in range(32):
                            k.mm(ps[:, :], w[kc // 8][:, kc % 8, sub * 128:(sub + 1) * 128], hp[:, kc, :], kc == 0, kc == 31, [w[kc // 8]] + hpq[(b * 4 + pc) % 2], [ps])
                        ut = uts[blk % 4]
                        evac(ut[:], ps[:, :], [ps], [ut])
                        k.dma("sp", UT[b, blk, :, pc * 512:(pc + 1) * 512], ut[:], reads=[ut], writes=[UT])
        k.pop()

    if "s3" in stages:
        W_UQX = inp("w_uqx", [QR, NH * 256])
        W_UKV = inp("w_ukv", [KVR, NH * 256])
        GMLA = inp("gmla_L", [128, 16])
        k.push()
        gmla = k.sb("gmla", [128, 16], F32)
        k.dma("sp", gmla[:], GMLA[:, :], writes=[gmla])
        rsm = k.sb("rsm", [128, NT], F32)
        ssm_ = k.sb("ssm_", [128, NT], F32)
        for b in range(NB):
            k.push()
            qnT = k.sb("qnT", [128, 6, SEQ], BF16)
            kvnT = k.sb("kvnT", [128, 4, SEQ], BF16)
            krT = k.sb("krT", [64, SEQ], BF16)
            cosT = k.sb("cosT", [64, SEQ], F32)
            sinT = k.sb("sinT", [64, SEQ], F32)
            k.dma("sp", qnT[:], QNT[b].rearrange("k p n -> p k n"), reads=[QNT], writes=[qnT])
            k.dma("sp", kvnT[:], KVNT[b].rearrange("k p n -> p k n"), reads=[KVNT], writes=[kvnT])
            k.dma("sp", krT[:], KRT[b], reads=[KRT], writes=[krT])
            k.dma("sp", cosT[:], COST[b], reads=[COST], writes=[cosT])
            k.dma("sp", sinT[:], SINT[b], reads=[SINT], writes=[sinT])
            Om = [k.sb("Om%d" % t, [128, NH * 128], BF16) for t in range(16)]
            wqs = [k.sb("wq%d" % i, [128, 6, 256], BF16) for i in range(2)]
            wkvs = [k.sb("wkv%d" % i, [128, 4, 256], BF16) for i in range(2)]
            qTn = k.sb("qTn", [128, SEQ], BF16)
            qTr = k.sb("qTr", [64, SEQ], BF16)
            kTn = k.sb("kTn", [128, SEQ], BF16)
            vhs = [k.sb("vh%d" % i, [128, 16, 129], BF16) for i in range(2)]
            for v_ in vhs:
                k.ve("memset", [], [v_], eng="pool", ap=v_[:, :, 128:129], constant=1.0)
            PTs = [k.sb("PT%d" % i, [128, 512], BF16) for i in range(4)]
            rt1 = k.sb("rt1", [64, 512], F32)
            rt2 = k.sb("rt2", [64, 512], F32)
            rinv = k.sb("rinv", [128, 4], F32)
            pti = 0
            for h in range(NH):
                wq, wkv, vh = wqs[h % 2], wkvs[h % 2], vhs[h % 2]
                k.dma("pool", wq[:], W_UQX[:, h * 256:(h + 1) * 256].rearrange("(k p) n -> p k n", p=128), writes=[wq])
                k.dma("pool", wkv[:], W_UKV[:, h * 256:(h + 1) * 256].rearrange("(k p) n -> p k n", p=128), writes=[wkv])
                for pc in range(4):
                    sl = slice(pc * 512, (pc + 1) * 512)
                    for kc in range(6):
                        k.mm(P[0][:, :], wq[:, kc, 0:128], qnT[:, kc, sl], kc == 0, kc == 5, [wq, qnT], [P[0]])
                    for kc in range(6):
                        k.mm(P[1][0:64, :], wq[:, kc, 128:192], qnT[:, kc, sl], kc == 0, kc == 5, [wq, qnT], [P[1]])
                    for kc in range(6):
                        k.mm(P[2][0:64, :], wq[:, kc, 192:256], qnT[:, kc, sl], kc == 0, kc == 5, [wq, qnT], [P[2]])
                    k.op("act", lambda e, o=qTn[:, sl], i=P[0][:, :]: e.copy(out=o, in_=i), [P[0]], [qTn])
                    k.ve("tensor_tensor", [P[1], cosT], [rt1], out=rt1[:], in0=P[1][0:64, :], in1=cosT[:, sl], op=ALU.mult)
                    k.ve("tensor_tensor", [P[2], sinT], [rt2], out=rt2[:], in0=P[2][0:64, :], in1=sinT[:, sl], op=ALU.mult)
                    k.ve("tensor_tensor", [rt1, rt2], [qTr], out=qTr[:, sl], in0=rt1[:], in1=rt2[:], op=ALU.add)
                    for kc in range(4):
                        k.mm(P[3][:, :], wkv[:, kc, 0:128], kvnT[:, kc, sl], kc == 0, kc == 3, [wkv, kvnT], [P[3]])
                    k.op("act", lambda e, o=kTn[:, sl], i=P[3][:, :]: e.copy(out=o, in_=i), [P[3]], [kTn])
                for tg in range(4):
                    ps = P[tg % 2]
                    for j in range(4):
                        t = tg * 4 + j
                        for kc in range(4):
                            k.mm(ps[:, j * 128:(j + 1) * 128], kvnT[:, kc, t * 128:(t + 1) * 128], wkv[:, kc, 128:256], kc == 0, kc == 3, [kvnT, wkv], [ps])
                    evac(vh[:, tg * 4:(tg + 1) * 4, 0:128], ps[:, :].rearrange("p (j d) -> p j d", d=128), [ps], [vh])
                for p4 in range(4):
                    nkc = 4 * p4 + 4
                    O = [P[4 + i] for i in range(4)]
                    for kc in range(nkc):
                        j = kc - 4 * p4
                        jj = max(j, 0)
                        q0 = jj * 128
                        S = P[kc % 2]
                        qs = slice(p4 * 512 + q0, (p4 + 1) * 512)
                        k.mm(S[:, q0:512], kTn[:, kc * 128:(kc + 1) * 128], qTn[:, qs], True, False, [kTn, qTn], [S])
                        k.mm(S[:, q0:512], krT[:, kc * 128:(kc + 1) * 128], qTr[:, qs], False, True, [krT, qTr], [S])
                        PT = PTs[pti % 4]
                        pti += 1
                        k.act(PT[:, q0:512], S[:, q0:512], AF.Exp, [S], [PT], scale=ATT_SCALE)
                        if j >= 0:
                            k.ve("memset", [], [PT], eng="pool", ap=PT[64:128, q0:q0 + 64], constant=0.0)
                        for i in range(jj, 4):
                            k.mm(O[i][:, 0:129], PT[:, i * 128:(i + 1) * 128], vh[:, kc, :], kc == 0, kc == 4 * p4 + i, [PT, vh], [O[i]])
                    for i in range(4):
                        gi = 4 * p4 + i
                        k.ve("reciprocal", [O[i]], [rinv], out=rinv[:, i:i + 1], in_=O[i][:, 128:129])
                        k.ve("tensor_scalar", [O[i], rinv], [Om[gi]], out=Om[gi][:, h * 128:(h + 1) * 128], in0=O[i][:, 0:128], scalar1=rinv[:, i:i + 1], scalar2=None, op0=ALU.mult)
            junk = k.sb("junk3", [128, NH * 128], BF16)
            omT = [k.sb("omT%d" % i, [128, 1024], BF16) for i in range(2)]
            for t in range(16):
                g = b * 16 + t
                k.act(junk[:], Om[t][:], AF.Square, [Om[t]], [junk, ssm_], accum_out=ssm_[:, g:g + 1])
            k.act(rsm[:, b * 16:(b + 1) * 16], ssm_[:, b * 16:(b + 1) * 16], AF.Sqrt, [ssm_, epsT], [rsm], scale=1.0 / 2048, bias=epsT[:])
            k.ve("reciprocal", [rsm], [rsm], out=rsm[:, b * 16:(b + 1) * 16], in_=rsm[:, b * 16:(b + 1) * 16])
            it = 0
            for kc in range(16):
                for tg in range(2):
                    bank = it % 4
                    o_ = omT[it % 2]
                    it += 1
                    for j in range(8):
                        t = tg * 8 + j
                        k.tr(PB[bank][:, j * 128:(j + 1) * 128], Om[t][:, kc * 128:(kc + 1) * 128], identb[:], [Om[t], identb], [P[bank]])
                    k.ve("tensor_scalar", [P[bank], gmla], [o_], out=o_[:], in0=PB[bank][:, 0:1024], scalar1=gmla[:, kc:kc + 1], scalar2=None, op0=ALU.mult)
                    k.dma("sp", OMT[b, kc, :, tg * 1024:(tg + 1) * 1024], o_[:], reads=[o_], writes=[OMT])
            k.pop()
        k.dma("sp", RSM[:, :], rsm[:], reads=[rsm], writes=[RSM])
        k.pop()

    def bc3(ap2, n):
        return ap2.unsqueeze(2).to_broadcast([ap2.shape[0], ap2.shape[1], n])

    if "s4" in stages:
        k.push()
        LRE1 = inp("lamre_T2", [128, NG]); LIM1 = inp("lamim_T2", [128, NG]); LDT1 = inp("logdt_bc", [128, NG])
        BRE2 = inp("bre_L2", [128, 16, NST]); BIM2 = inp("bim_L2", [128, 16, NST])
        LRE2 = inp("lamre_L2", [128, 16, NST]); LIM2 = inp("lamim_L2", [128, 16, NST]); LDT2 = inp("logdt_L2", [128, 16])
        CL3 = inp("c_L3", [128, NG * 16]); DL = inp("d_L", [128, 16])
        MASK8 = inp("mask8", [128, 8]); PSWI = inp("psw", [128, 128]); IOTA = inp("iota_t", [128, SEQ])
        TH = k.sb("TH", [128, NG], F32); RR = k.sb("RR", [128, NG], F32)
        BT = k.sb("BT", [128, 16, 256], BF16)
        CS = k.sb("CS", [128, NG * 16], BF16)
        mask8 = k.sb("mask8", [128, 8], F32); psw = k.sb("psw", [128, 128], F32)
        iot = k.sb("iot", [128, SEQ], F32); dL = k.sb("dL", [128, 16], F32)
        k.dma("sp", mask8[:], MASK8[:, :], writes=[mask8]); k.dma("sp", psw[:], PSWI[:, :], writes=[psw])
        k.dma("sp", iot[:], IOTA[:, :], writes=[iot]); k.dma("sp", dL[:], DL[:, :], writes=[dL])
        k.push()
        a1 = k.sb("a1", [128, NG], F32); a2 = k.sb("a2", [128, NG], F32); a3 = k.sb("a3", [128, NG], F32)
        k.dma("sp", a1[:], LDT1[:, :], writes=[a1]); k.dma("sp", a2[:], LIM1[:, :], writes=[a2]); k.dma("sp", a3[:], LRE1[:, :], writes=[a3])
        k.act(a1[:], a1[:], AF.Exp, [a1], [a1])
        k.ve("tensor_tensor", [a2, a1], [TH], out=TH[:], in0=a2[:], in1=a1[:], op=ALU.mult)
        k.ve("tensor_tensor", [a3, a1], [a3], out=a3[:], in0=a3[:], in1=a1[:], op=ALU.mult)
        k.act(RR[:], a3[:], AF.Exp, [a3], [RR])
        sh3 = [128, 16, NST]
        lre = k.sb("lre", sh3, F32); lim = k.sb("lim", sh3, F32); bre = k.sb("bre", sh3, F32); bim = k.sb("bim", sh3, F32)
        dt2 = k.sb("dt2", [128, 16], F32)
        for t_, src in ((lre, LRE2), (lim, LIM2), (bre, BRE2), (bim, BIM2)):
            k.dma("sp", t_[:], src[:, :, :], writes=[t_])
        k.dma("sp", dt2[:], LDT2[:, :], writes=[dt2])
        k.act(dt2[:], dt2[:], AF.Exp, [dt2], [dt2])
        mag = k.sb("mag", sh3, F32); th = k.sb("th", sh3, F32); ti2 = k.sb("ti2", sh3, I32); tf2 = k.sb("tf2", sh3, F32)
        sn2 = k.sb("sn2", sh3, F32); cs2 = k.sb("cs2", sh3, F32)
        k.ve("tensor_tensor", [lre, dt2], [mag], out=mag[:], in0=lre[:], in1=bc3(dt2[:, :], NST), op=ALU.mult)
        k.act(mag[:], mag[:], AF.Exp, [mag], [mag])
        k.ve("tensor_tensor", [lim, dt2], [th], out=th[:], in0=lim[:], in1=bc3(dt2[:, :], NST), op=ALU.mult)
        range_reduce(th, ti2, tf2)
        k.act(sn2[:], th[:], AF.Sin, [th], [sn2])
        k.act(cs2[:], tf2[:], AF.Sin, [tf2, hpiT], [cs2], scale=-1.0, bias=hpiT[:])
        nr = k.sb("nr", sh3, F32); ni = k.sb("ni", sh3, F32); den = k.sb("den", sh3, F32); u1 = k.sb("u1", sh3, F32)
        fre = k.sb("fre", sh3, F32); fim = k.sb("fim", sh3, F32)
        TT = lambda o, a, b_, op: k.ve("tensor_tensor", [a, b_], [o], out=o[:], in0=a[:], in1=b_[:], op=op)
        TT(nr, mag, cs2, ALU.mult)
        k.ve("tensor_scalar", [nr], [nr], out=nr[:], in0=nr[:], scalar1=-1.0, scalar2=None, op0=ALU.add)
        TT(ni, mag, sn2, ALU.mult)
        TT(den, lre, lre, ALU.mult); TT(u1, lim, lim, ALU.mult); TT(den, den, u1, ALU.add)
        k.ve("reciprocal", [den], [den], out=den[:], in_=den[:])
        TT(fre, nr, lre, ALU.mult); TT(u1, ni, lim, ALU.mult); TT(fre, fre, u1, ALU.add); TT(fre, fre, den, ALU.mult)
        TT(fim, ni, lre, ALU.mult); TT(u1, nr, lim, ALU.mult); TT(fim, fim, u1, ALU.subtract); TT(fim, fim, den, ALU.mult)
        TT(nr, fre, bre, ALU.mult); TT(u1, fim, bim, ALU.mult)
        k.ve("tensor_tensor", [nr, u1], [BT], out=BT[:, :, 0:64], in0=nr[:], in1=u1[:], op=ALU.subtract)
        k.ve("tensor_tensor", [u1, nr], [BT], out=BT[:, :, 192:256], in0=u1[:], in1=nr[:], op=ALU.subtract)
        TT(ni, fre, bim, ALU.mult); TT(u1, fim, bre, ALU.mult)
        k.ve("tensor_tensor", [ni, u1], [BT], out=BT[:, :, 64:128], in0=ni[:], in1=u1[:], op=ALU.add)
        k.ve("tensor_tensor", [ni, u1], [BT], out=BT[:, :, 128:192], in0=ni[:], in1=u1[:], op=ALU.add)
        ctmp = k.sb("ctmp", [128, NG * 16], F32)
        k.dma("sp", ctmp[:], CL3[:, :], writes=[ctmp])
        k.ve("tensor_copy", [ctmp], [CS], out=CS[0:64, :], in_=ctmp[0:64, :])
        k.ve("tensor_scalar", [ctmp], [CS], out=CS[64:128, :], in0=ctmp[64:128, :], scalar1=-1.0, scalar2=None, op0=ALU.mult)
        k.pop()
        LBs = [k.sb("LB%d" % i, [128, 8, 256], BF16) for i in range(2)]
        LCs = [k.sb("LC%d" % i, [128, 8, 128], BF16) for i in range(2)]
        for l_ in LCs:
            k.ve("memset", [], [l_], eng="pool", ap=l_[:], constant=0.0)
        COSs = [k.sb("COS%d" % i, [128, SEQ], F32) for i in range(2)]
        SINs = [k.sb("SIN%d" % i, [128, SEQ], F32) for i in range(2)]
        ang = k.sb("angS", [128, SEQ], F32); tiS = k.sb("tiS", [128, SEQ], I32); tfS = k.sb("tfS", [128, SEQ], F32)
        uTs = [[k.sb("uT%d_%d" % (i, b), [128, SEQ], BF16) for b in range(NB)] for i in range(2)]
        yacc = [k.sb("yacc%d" % b, [128, SEQ], BF16) for b in range(NB)]
        ring = lambda nm, dt_, n=2: [k.sb("%s%d" % (nm, i), [128, 512], dt_) for i in range(n)]
        t1s, t2s, t3s, t4s, hhs = ring("t1_", BF16, 4), ring("t2_", BF16, 4), ring("t3_", BF16, 4), ring("t4_", BF16, 4), ring("hh", F32)
        yts = ring("yt", BF16, 4)
        gy = k.sb("gy", [128, 1024], F32); gy2 = k.sb("gy2", [128, 1024], F32); gin_ = k.sb("gin", [128, 1024], F32)
        gts = [k.sb("gt%d" % i, [128, 1024], BF16) for i in range(2)]
        yaccs = [yacc, [k.sb("yaccB%d" % b, [128, SEQ], BF16) for b in range(NB)]]
        gti = [0]

        def prep_block(gb):
            LB, LC, uT = LBs[gb % 2], LCs[gb % 2], uTs[gb % 2]
            k.ve("tensor_tensor", [BT, mask8], [LB], out=LB[:], in0=BT[:, gb, :].unsqueeze(1).to_broadcast([128, 8, 256]),
                 in1=bc3(mask8[:, :], 256), op=ALU.mult)
            for gl in range(8):
                g = gb * 8 + gl
                k.ve("tensor_copy", [CS], [LC], eng="pool", out=LC[:, gl, gl * 16:(gl + 1) * 16], in_=CS[:, g * 16:(g + 1) * 16])
            for b in range(NB):
                k.dma("sp", uT[b][:], UT[b, gb], reads=[UT], writes=[uT[b]])

        def table_steps(g):
            COS, SIN = COSs[g % 2], SINs[g % 2]
            st = []
            st.append(lambda: k.act(ang[:], iot[:], AF.Copy, [iot, TH], [ang], scale=TH[:, g:g + 1]))
            st.append(lambda: k.ve("tensor_scalar", [ang], [tiS], out=tiS[:], in0=ang[:], scalar1=1.0 / TWO_PI, scalar2=None, op0=ALU.mult))
            st.append(lambda: k.op("act", lambda e: e.copy(out=tfS[:], in_=tiS[:]), [tiS], [tfS]))
            st.append(lambda: k.ve("scalar_tensor_tensor", [tfS, ang], [ang], out=ang[:], in0=tfS[:], scalar=-C1, in1=ang[:], op0=ALU.mult, op1=ALU.add))
            st.append(lambda: k.ve("scalar_tensor_tensor", [tfS, ang], [ang], out=ang[:], in0=tfS[:], scalar=-C2, in1=ang[:], op0=ALU.mult, op1=ALU.add))
            st.append(lambda: k.act(tfS[:], ang[:], AF.Abs, [ang], [tfS]))
            st.append(lambda: k.act(SIN[:], ang[:], AF.Sin, [ang], [SIN], scale=0.999))
            st.append(lambda: k.act(COS[:], tfS[:], AF.Sin, [tfS, hpiT], [COS], scale=-0.999, bias=hpiT[:]))
            return st

        iters = [(gb, gl, b, pc) for gb in range(16) for gl in range(8) for b in range(NB) for pc in range(4)]
        NI = len(iters)

        def ctx_of(n):
            gb, gl, b, pc = iters[n]
            g = gb * 8 + gl
            r_ = n % 4
            return dict(gb=gb, gl=gl, b=b, pc=pc, g=g, r=r_, pa=P[n % 3], pb=P[3], ya_ps=P[4 + pc], sl=slice(pc * 512, (pc + 1) * 512),
                        LB=LBs[gb % 2], LC=LCs[gb % 2], uT=uTs[gb % 2][b], COS=COSs[g % 2], SIN=SINs[g % 2], hh=hhs[pc % 2],
                        yacc=yaccs[gb % 2][b])

        def stA(n):
            c = ctx_of(n)
            t1, t2 = t1s[c["r"]], t2s[c["r"]]
            k.mm(c["pa"][:, :], c["LB"][:, c["gl"], 0:128], c["uT"][:, c["sl"]], True, True, [c["LB"], c["uT"]], [c["pa"]])
            k.mm(c["pb"][:, :], c["LB"][:, c["gl"], 128:256], c["uT"][:, c["sl"]], True, True, [c["LB"], c["uT"]], [c["pb"]])
            k.ve("tensor_tensor", [c["pa"], c["COS"]], [t1], out=t1[:], in0=c["pa"][:, :], in1=c["COS"][:, c["sl"]], op=ALU.mult)
            k.ve("tensor_tensor", [c["pb"], c["SIN"]], [t2], out=t2[:], in0=c["pb"][:, :], in1=c["SIN"][:, c["sl"]], op=ALU.mult)
            k.mm(c["pa"][:, :], identb[:, :], t1[:, :], True, False, [identb, t1], [c["pa"]])
            k.mm(c["pa"][:, :], identb[:, :], t2[:, :], False, True, [identb, t2], [c["pa"]])

        def stB(n):
            c = ctx_of(n)
            t3, hh, pc = t3s[c["r"]], c["hh"], c["pc"]
            init = 0.0 if pc == 0 else hhs[(pc - 1) % 2][:, 511:512]
            rd = [RR, c["pa"]] + ([] if pc == 0 else [hhs[(pc - 1) % 2]])
            k.ve("tensor_tensor_scan", rd, [hh], out=hh[:], data0=RR[:, c["g"]:c["g"] + 1].to_broadcast([128, 512]), data1=c["pa"][:, :],
                 initial=init, op0=ALU.mult, op1=ALU.add)
            k.mm(c["pa"][:, :], psw[:, :], hh[:, :], True, True, [psw, hh], [c["pa"]])
            k.ve("tensor_tensor", [hh, c["COS"]], [t3], eng="pool", out=t3[:], in0=hh[:], in1=c["COS"][:, c["sl"]], op=ALU.mult)

        def stC(n):
            c = ctx_of(n)
            t3, t4 = t3s[c["r"]], t4s[c["r"]]
            k.ve("tensor_tensor", [c["pa"], c["SIN"]], [t4], out=t4[:], in0=c["pa"][:, :], in1=c["SIN"][:, c["sl"]], op=ALU.mult)
            k.mm(c["ya_ps"][:, :], c["LC"][:, c["gl"], :], t3[:, :], c["gl"] == 0, False, [c["LC"], t3], [c["ya_ps"]])
            k.mm(c["ya_ps"][:, :], c["LC"][:, c["gl"], :], t4[:, :], False, c["gl"] == 7, [c["LC"], t4], [c["ya_ps"]])
            if c["gl"] == 7:
                finish_piece(c["gb"], c["b"], c["pc"])

        def finish_piece(gb, b, pc):
            assert NB == 1
            uT = uTs[gb % 2][b]
            sl = slice(pc * 512, (pc + 1) * 512)
            hs = slice(0, 512)
            gt = gts[gti[0] % 2]
            gti[0] += 1
            k.ve("scalar_tensor_tensor", [uT, dL, P[4 + pc]], [gy], out=gy[:, hs], in0=uT[:, sl], scalar=dL[:, gb:gb + 1], in1=P[4 + pc][:, :], op0=ALU.mult, op1=ALU.add)
            k.ve("tensor_tensor", [gy], [gy2], out=gy2[:, hs], in0=gy[:, hs], in1=gy[:, hs], op=ALU.mult)
            k.ve("tensor_scalar", [gy2], [gy2], out=gy2[:, hs], in0=gy2[:, hs], scalar1=0.044715, scalar2=1.0, op0=ALU.mult, op1=ALU.add)
            k.ve("tensor_tensor", [gy2, gy], [gin_], eng="pool", out=gin_[:, hs], in0=gy2[:, hs], in1=gy[:, hs], op=ALU.mult)
            k.act(gin_[:, hs], gin_[:, hs], AF.Sigmoid, [gin_], [gin_], scale=1.5957691216057308)
            k.ve("tensor_tensor", [gin_, gy], [gt], eng="pool", out=gt[:, hs], in0=gin_[:, hs], in1=gy[:, hs], op=ALU.mult)
            k.dma("sp", GT[b, gb, :, sl], gt[:, hs], reads=[gt], writes=[GT])

        prep_block(0)
        for f in table_steps(0):
            f()
        per_grp = NB * 4
        pending = []
        for n in range(NI + 2):
            if n < NI:
                gb, gl, b, pc = iters[n]
                g = gb * 8 + gl
                if b == 0 and pc == 0:
                    if gl == 4 and gb + 1 < 16:
                        prep_block(gb + 1)
                    pending = table_steps(g + 1) if g + 1 < NG else []
                stA(n)
            if 0 <= n - 1 < NI:
                stB(n - 1)
            if 0 <= n - 2 < NI:
                stC(n - 2)
            if n < NI:
                pos = (iters[n][2] * 4 + iters[n][3])
                lo = len(pending) * pos // per_grp if pending else 0
                if pos == 0:
                    cx.tsteps = pending
                K_ = len(cx.tsteps)
                for i_ in range(K_ * pos // per_grp, K_ * (pos + 1) // per_grp):
                    cx.tsteps[i_]()
        k.pop()

    if "s5" in stages:
        W_GLU = inp("w_glu", [SSMW, SSMW])
        BGLU = inp("bglu_L", [128, 16]); GSSM = inp("gssm_L", [128, 16])
        k.push()
        bglu = k.sb("bglu", [128, 16], F32); gssm = k.sb("gssm", [128, 16], F32)
        k.dma("sp", bglu[:], BGLU[:, :], writes=[bglu]); k.dma("sp", gssm[:], GSSM[:, :], writes=[gssm])
        onesb = k.sb("onesb", [128, 1], BF16)
        k.ve("memset", [], [onesb], eng="pool", ap=onesb[:], constant=1.0)
        rss = k.sb("rss", [128, NT], F32)
        gTk = [k.sb("gTk%d" % i, [128, SEQ], BF16) for i in range(16)]
        wgs = [k.sb("wg%d" % i, [128, 16, 128], BF16) for i in range(2)]
        sgs = [k.sb("sg%d" % i, [128, 512], F32) for i in range(2)]
        ors = [k.sb("or%d" % i, [128, 512], F32) for i in range(2)]
        sqs = [k.sb("sq%d" % i, [128, 512], BF16) for i in range(2)]
        osts = [k.sb("ost%d" % i, [128, 512], BF16) for i in range(2)]
        for b in range(NB):
            for kc in range(16):
                k.dma("sp", gTk[kc][:], GT[b, kc], reads=[GT], writes=[gTk[kc]])
            it = 0
            for cb in range(16):
                wg = wgs[cb % 2]
                k.dma("pool", wg[:], W_GLU[:, cb * 128:(cb + 1) * 128].rearrange("(k p) n -> p k n", p=128), writes=[wg])
                for pc in range(4):
                    sl = slice(pc * 512, (pc + 1) * 512)
                    r_ = it % 2
                    it += 1
                    ps = P[r_]
                    for kc in range(16):
                        k.mm(ps[:, :], wg[:, kc, :], gTk[kc][:, sl], kc == 0, kc == 15, [wg, gTk[kc]], [ps])
                    sg, orr, sq, ost = sgs[r_], ors[r_], sqs[r_], osts[r_]
                    k.act(sg[:], ps[:, :], AF.Sigmoid, [ps, bglu], [sg], bias=bglu[:, cb:cb + 1])
                    k.ve("tensor_tensor", [sg, gTk[cb]], [orr], out=orr[:], in0=sg[:], in1=gTk[cb][:, sl], op=ALU.mult)
                    k.act(sq[:], orr[:], AF.Square, [orr], [sq])
                    for j in range(4):
                        t = pc * 4 + j
                        first = (cb == 0 and pc == 0 and j == 0)
                        k.mm(P[7][:, t:t + 1], sq[:, j * 128:(j + 1) * 128], onesb[:, :], first, cb == 15, [sq, onesb], [P[7]], skip_group_check=True)
                    k.ve("tensor_scalar", [orr, gssm], [ost], out=ost[:], in0=orr[:], scalar1=gssm[:, cb:cb + 1], scalar2=None, op0=ALU.mult)
                    k.dma("sp", OST[b, cb, :, sl], ost[:], reads=[ost], writes=[OST])
            k.act(rss[:, b * 16:(b + 1) * 16], P[7][:, 0:16], AF.Sqrt, [P[7], epsT], [rss], scale=1.0 / SSMW, bias=epsT[:])
            k.ve("reciprocal", [rss], [rss], out=rss[:, b * 16:(b + 1) * 16], in_=rss[:, b * 16:(b + 1) * 16])
        k.dma("sp", RSS[:, :], rss[:], reads=[rss], writes=[RSS])
        k.pop()

    if "s6" in stages:
        W_OUT = inp("w_out", [D, D])
        k.push()
        rsm6 = k.sb("rsm6", [128, NT], F32); rss6 = k.sb("rss6", [128, NT], F32)
        k.dma("sp", rsm6[:], RSM[:, :], reads=[RSM], writes=[rsm6]); k.dma("sp", rss6[:], RSS[:, :], reads=[RSS], writes=[rss6])
        GA = k.sb("GA", [128, D], F32)
        oTm = [k.sb("oTm%d" % i, [128, 16, 512], BF16) for i in range(2)]
        oTs = [k.sb("oTs%d" % i, [128, 16, 512], BF16) for i in range(2)]
        Wo = [[k.sb("Wo%d_%d" % (i, q), [128, 8, 512], BF16) for q in range(4)] for i in range(2)]
        xins = [k.sb("xin%d" % i, [128, 512], F32) for i in range(2)]
        tts = [k.sb("tt%d" % i, [128, 512], F32) for i in range(2)]
        it = 0
        wi = 0
        for b in range(NB):
            load_mod_vec(GA, b, 2)
            for tg in range(4):
                om, os_ = oTm[(b * 4 + tg) % 2], oTs[(b * 4 + tg) % 2]
                tsl = slice(tg * 512, (tg + 1) * 512)
                k.dma("sp", om[:], OMT[b, :, :, tsl].rearrange("k p n -> p k n"), reads=[OMT], writes=[om])
                k.dma("sp", os_[:], OST[b, :, :, tsl].rearrange("k p n -> p k n"), reads=[OST], writes=[os_])
                for cc in range(8):
                    w = Wo[wi % 2]
                    wi += 1
                    csl = slice(cc * 512, (cc + 1) * 512)
                    for q in range(4):
                        k.dma("pool", w[q][:], W_OUT[q * 1024:(q + 1) * 1024, csl].rearrange("(k p) n -> p k n", p=128), writes=[w[q]])
                    for j in range(4):
                        tile = tg * 4 + j
                        g = b * 16 + tile
                        r0 = b * SEQ + tile * 128
                        r_ = it % 2
                        it += 1
                        pa, pb_ = P[2 * r_], P[2 * r_ + 1]
                        for kc in range(16):
                            k.mm(pa[:, :], om[:, kc, j * 128:(j + 1) * 128], w[kc // 8][:, kc % 8, :], kc == 0, kc == 15, [om, w[kc // 8]], [pa])
                        for kc in range(16):
                            k.mm(pb_[:, :], os_[:, kc, j * 128:(j + 1) * 128], w[2 + kc // 8][:, kc % 8, :], kc == 0, kc == 15, [os_, w[2 + kc // 8]], [pb_])
                        xin, tt = xins[r_], tts[r_]
                        k.dma("sp", xin[:], X_IN[r0:r0 + 128, csl], writes=[xin])
                        k.ve("tensor_scalar", [pa, rsm6], [tt], out=tt[:], in0=pa[:, :], scalar1=rsm6[:, g:g + 1], scalar2=None, op0=ALU.mult)
                        k.ve("scalar_tensor_tensor", [pb_, rss6, tt], [tt], out=tt[:], in0=pb_[:, :], scalar=rss6[:, g:g + 1], in1=tt[:], op0=ALU.mult, op1=ALU.add)
                        k.ve("tensor_tensor", [tt, GA], [tt], out=tt[:], in0=tt[:], in1=GA[:, csl], op=ALU.mult)
                        k.ve("tensor_tensor", [tt, xin], [tt], out=tt[:], in0=tt[:], in1=xin[:], op=ALU.add)
                        k.dma("sp", X1[r0:r0 + 128, csl], tt[:], reads=[tt], writes=[X1])
        k.pop()

    if "s7" in stages:
        G_FFN = inp("g_ffn", [1, D])
        W_R = inp("w_r", [D, 72]); B_R = inp("b_r", [1, 72])
        TRI = inp("tri", [128, 128]); JB = inp("jb", [128, NBLK]); PIDX = inp("pidx", [128, 1])
        k.push()
        sel12 = k.sb("sel12", [128, NT, 2, 64], BF16)
        gates = k.sb("gates", [128, NT * 2], F32)
        slots = k.sb("slots", [128, NT * 2], I32)
        WR = k.sb("WR", [128, 32, 72], F32)
        k.dma("sp", WR[:], W_R[:, :].rearrange("(k p) n -> p k n", p=128), writes=[WR])
        br = k.sb("br", [1, 72], F32); ones1f = k.sb("ones1f", [1, 128], F32)
        k.dma("sp", br[:], B_R[:, :], writes=[br])
        k.ve("memset", [], [ones1f], eng="pool", ap=ones1f[:], constant=1.0)
        onesB = k.sb("onesB", [128, 128], BF16); trib = k.sb("trib", [128, 128], BF16); trif = k.sb("trif", [128, 128], F32)
        k.ve("memset", [], [onesB], eng="pool", ap=onesB[:], constant=1.0)
        k.dma("sp", trif[:], TRI[:, :], writes=[trif])
        k.ve("tensor_copy", [trif], [trib], out=trib[:], in_=trif[:])
        k.push()
        Af = k.sb("Af", [128, D], F32); SHf = k.sb("SHf", [128, D], F32); gtmp = k.sb("gtmp7", [128, D], F32)
        xts = [k.sb("x1t%d" % i, [128, D], F32) for i in range(2)]
        junk = k.sb("junk7", [128, D], BF16)
        h2bs = [k.sb("h2b%d" % i, [128, D], BF16) for i in range(2)]
        h2T = k.sb("h2T", [128, 32, 128], F32)
        ss = k.sb("ss7", [128, 1], F32); rstd = k.sb("rstd7", [128, 1], F32)
        lg = k.sb("lg", [128, 72], F32); m8 = k.sb("m8", [128, 8], F32); oh = k.sb("oh", [128, 8], F32)
        ngm = k.sb("ngm", [128, 1], F32); ex = k.sb("ex", [128, 8], F32); se = k.sb("se", [128, 1], F32)
        pen = k.sb("pen", [128, 8], F32); em = k.sb("em", [128, 8, 8], F32); t8 = k.sb("t8", [128, 8], F32)
        dl = k.sb("dl", [128, 1], F32); wv = k.sb("wv", [128, 2], F32); ssum = k.sb("ssum", [128, 64], BF16)
        for b in range(NB):
            load_mod_vec(Af, b, 4)
            load_mod_vec(SHf, b, 3)
            make_A(Af, gtmp, G_FFN[0:1, :])
            for t in range(16):
                g = b * 16 + t
                r0 = g * 128
                xt, h2b = xts[g % 2], h2bs[g % 2]
                k.dma("sp", xt[:], X1[r0:r0 + 128, :], reads=[X1], writes=[xt])
                norm_mod_tile(xt, Af, SHf, xt, junk, ss, rstd)
                k.op("act", lambda e, o=h2b[:], i=xt[:]: e.copy(out=o, in_=i), [xt], [h2b])
                k.dma("sp", H2B[r0:r0 + 128, :], h2b[:], reads=[h2b], writes=[H2B])
                for rnd in range(2):
                    for q in range(4):
                        for j in range(4):
                            kc = rnd * 16 + q * 4 + j
                            k.tr(P[q][:, j * 128:(j + 1) * 128], xt[:, kc * 128:(kc + 1) * 128], identf[:], [xt, identf], [P[q]])
                        kc0 = rnd * 16 + q * 4
                        evac(h2T[:, kc0:kc0 + 4, :], P[q][:, :].rearrange("p (j n) -> p j n", n=128), [P[q]], [h2T])
                for kc in range(32):
                    k.mm(P[4][:, 0:72], h2T[:, kc, :], WR[:, kc, :], kc == 0, False, [h2T, WR], [P[4]])
                k.mm(P[4][:, 0:72], ones1f[:, :], br[:, :], False, True, [ones1f, br], [P[4]])
                k.ve("tensor_copy", [P[4]], [lg], out=lg[:], in_=P[4][:, 0:72])
                k.ve("max", [lg], [m8], out=m8[:], in_=lg[:, 0:8])
                k.ve("tensor_scalar", [lg, m8], [oh], out=oh[:], in0=lg[:, 0:8], scalar1=m8[:, 0:1], scalar2=None, op0=ALU.is_equal)
                k.ve("tensor_scalar", [m8], [ngm], out=ngm[:], in0=m8[:, 0:1], scalar1=-1.0, scalar2=None, op0=ALU.mult)
                k.act(ex[:], lg[:, 0:8], AF.Exp, [lg, ngm], [ex, se], bias=ngm[:], accum_out=se[:])
                k.ve("reciprocal", [se], [se], out=se[:], in_=se[:])
                k.ve("tensor_scalar", [oh], [pen], out=pen[:], in0=oh[:], scalar1=1e30, scalar2=-1e30, op0=ALU.mult, op1=ALU.add)
                k.ve("tensor_tensor", [lg, pen], [em], out=em[:], in0=lg[:, 8:72].rearrange("p (a b) -> p a b", b=8), in1=bc3(pen[:, :], 8), op=ALU.add)
                emf = em[:].rearrange("p a b -> p (a b)")
                k.ve("max", [em], [t8], out=t8[:], in_=emf)
                k.ve("tensor_scalar", [em, t8], [sel12], out=sel12[:, g, 0, :], in0=emf, scalar1=t8[:, 0:1], scalar2=None, op0=ALU.is_equal)
                k.ve("tensor_scalar", [em, t8], [sel12], out=sel12[:, g, 1, :], in0=emf, scalar1=t8[:, 1:2], scalar2=None, op0=ALU.is_equal)
                k.ve("tensor_tensor", [t8], [dl], out=dl[:], in0=t8[:, 0:1], in1=t8[:, 1:2], op=ALU.subtract)
                k.act(wv[:, 0:1], dl[:], AF.Sigmoid, [dl], [wv])
                k.act(wv[:, 1:2], dl[:], AF.Sigmoid, [dl, wv], [wv], scale=-1.0)
                k.ve("tensor_scalar", [wv, se], [gates], out=gates[:, 2 * g:2 * g + 2], in0=wv[:], scalar1=se[:, 0:1], scalar2=None, op0=ALU.mult)
                k.ve("tensor_tensor", [sel12], [ssum], out=ssum[:], in0=sel12[:, g, 0, :], in1=sel12[:, g, 1, :], op=ALU.add)
                k.mm(P[7][:, 0:64], onesB[:, :], ssum[:, :], g == 0, g == NT - 1, [onesB, ssum], [P[7]])
        k.pop()
        k.push()
        cnt = k.sb("cnt", [128, 64], F32); pad = k.sb("pad", [128, 64], F32); padi = k.sb("padi", [128, 64], I32)
        pends = k.sb("pends", [128, 64], F32); base = k.sb("base", [128, 64], F32); one64 = k.sb("one64", [128, 64], F32)
        k.ve("memset", [], [one64], eng="pool", ap=one64[:], constant=1.0)
        k.ve("tensor_copy", [P[7]], [cnt], out=cnt[:], in_=P[7][:, 0:64])
        k.ve("tensor_scalar", [cnt], [padi], out=padi[:], in0=cnt[:], scalar1=1.0 / BLK, scalar2=(BLK - 1.0) / BLK - 0.5 + 0.5 / BLK, op0=ALU.mult, op1=ALU.add)
        k.ve("tensor_copy", [padi], [pad], out=pad[:], in_=padi[:])
        k.ve("tensor_scalar", [pad], [pad], out=pad[:], in0=pad[:], scalar1=float(BLK), scalar2=None, op0=ALU.mult)
        k.ve("tensor_tensor_scan", [one64, pad], [pends], out=pends[:], data0=one64[:], data1=pad[:], initial=0.0, op0=ALU.mult, op1=ALU.add)
        k.ve("tensor_tensor", [pends, pad], [base], out=base[:], in0=pends[:], in1=pad[:], op=ALU.subtract)
        jb = k.sb("jb", [128, NBLK], F32); pidx = k.sb("pidx", [128, 1], F32)
        k.dma("sp", jb[:], JB[:, :], writes=[jb]); k.dma("sp", pidx[:], PIDX[:, :], writes=[pidx])
        bexp = k.sb("bexp", [128, NBLK], F32); iw = k.sb("iw", [128, NBLK], I32)
        CH = 48
        cmp_ = k.sb("cmp", [128, CH, 64], F32)
        for c0 in range(0, NBLK, CH):
            n = min(CH, NBLK - c0)
            k.ve("tensor_tensor", [pends, jb], [cmp_], out=cmp_[:, 0:n, :], in0=pends[:, :].unsqueeze(1).to_broadcast([128, n, 64]),
                 in1=bc3(jb[:, c0:c0 + n], 64), op=ALU.is_le)
            k.ve("tensor_reduce", [cmp_], [bexp], out=bexp[:, c0:c0 + n], in_=cmp_[:, 0:n, :], axis=AX.X, op=ALU.add)
        k.ve("tensor_scalar", [bexp], [bexp], out=bexp[:], in0=bexp[:], scalar1=63.0, scalar2=128.0, op0=ALU.min, op1=ALU.mult)
        tl = k.sb("tl", [128, NBLK], F32)
        k.ve("tensor_scalar", [jb, pends], [tl], out=tl[:], in0=jb[:], scalar1=pends[:, 63:64], scalar2=1.0e7, op0=ALU.is_ge, op1=ALU.mult)
        k.ve("tensor_tensor", [bexp, tl], [bexp], out=bexp[:], in0=bexp[:], in1=tl[:], op=ALU.add)
        k.ve("tensor_scalar", [bexp, pidx], [iw], out=iw[:], in0=bexp[:], scalar1=pidx[:, 0:1], scalar2=None, op0=ALU.add)
        k.dma("sp", IWD[:, :], iw[:], reads=[iw], writes=[IWD])
        h2bs = [k.sb("h2c%d" % i, [128, D], BF16) for i in range(2)]
        ssum = k.sb("ssumB", [128, 64], BF16); vv = k.sb("vv", [128, 64], F32); tmp = k.sb("tmpB", [128, 64], F32)
        df = k.sb("df", [128, 2], F32)
        for g in range(NT):
            h2b = h2bs[g % 2]
            r0 = g * 128
            k.dma("sp", h2b[:], H2B[r0:r0 + 128, :], reads=[H2B], writes=[h2b])
            k.ve("tensor_tensor", [sel12], [ssum], out=ssum[:], in0=sel12[:, g, 0, :], in1=sel12[:, g, 1, :], op=ALU.add)
            k.mm(P[5][:, 0:64], trib[:, :], ssum[:, :], True, True, [trib, ssum], [P[5]])
            k.mm(P[6][:, 0:64], onesB[:, :], ssum[:, :], True, True, [onesB, ssum], [P[6]])
            k.ve("tensor_tensor", [P[5], base], [vv], out=vv[:], in0=P[5][:, 0:64], in1=base[:], op=ALU.add)
            k.ve("tensor_tensor", [P[6], base], [base], out=base[:], in0=P[6][:, 0:64], in1=base[:], op=ALU.add)
            for s_ in range(2):
                k.ve("tensor_tensor", [sel12, vv], [tmp], out=tmp[:], in0=sel12[:, g, s_, :], in1=vv[:], op=ALU.mult)
                k.ve("tensor_reduce", [tmp], [df], out=df[:, s_:s_ + 1], in_=tmp[:], axis=AX.X, op=ALU.add)
            k.ve("tensor_copy", [df], [slots], out=slots[:, 2 * g:2 * g + 2], in_=df[:])
            for s_ in range(2):
                k.scatter(XE[:, :], slots[:, 2 * g + s_:2 * g + s_ + 1], h2b[:], reads=[h2b, slots] + (cx.xez if (g == 0 and s_ == 0) else []), writes=[XE])
        k.dma("sp", SLOTS[:, :], slots[:], reads=[slots], writes=[SLOTS])
        k.dma("sp", GATES[:, :], gates[:], reads=[gates], writes=[GATES])
        k.pop()
        k.pop()

    if "s8" in stages:
        W1P = inp("w1p", [NE * 128 * 8, 2048]); W3P = inp("w3p", [NE * 128 * 8, 2048]); W2P = inp("w2p", [NE * 128 * 8, 2048])
        k.push()
        iw0 = k.sb("iw0", [128, NBLK], I32); iwf = k.sb("iwf", [128, NBLK], F32); iwg = k.sb("iwg", [128, NBLK], F32)
        k.dma("sp", iw0[:], IWD[:, :], reads=[IWD], writes=[iw0])
        k.ve("tensor_copy", [iw0], [iwf], out=iwf[:], in_=iw0[:])
        iwc = [k.sb("iwc%d" % c, [128, NBLK], I32) for c in range(8)]
        for c in range(8):
            k.ve("tensor_scalar", [iwf], [iwg], out=iwg[:], in0=iwf[:], scalar1=8.0, scalar2=float(c), op0=ALU.mult, op1=ALU.add)
            k.ve("tensor_copy", [iwg], [iwc[c]], out=iwc[c][:], in_=iwg[:])
        w1c = [[k.sb("w1c%d_%d" % (i, c), [128, 2048], BF16) for c in range(8)] for i in range(2)]
        w3c = [[k.sb("w3c%d_%d" % (i, c), [128, 2048], BF16) for c in range(8)] for i in range(2)]
        w2c = [k.sb("w2c%d" % c, [128, 2048], BF16) for c in range(8)]
        xe = k.sb("xe", [128, D], BF16)
        xT = [k.sb("xT%d" % q, [128, 8, 128], BF16) for q in range(4)]
        a_sb = k.sb("a_sb", [128, 512], F32); act_ = k.sb("act_", [128, 512], BF16); aT = k.sb("aT", [128, 4, 128], BF16)
        yes = [k.sb("ye%d" % i, [128, 2048], BF16) for i in range(2)]
        for j in range(NBLK):
            r_ = j % 2
            for c in range(8):
                k.gather(w1c[r_][c][:], W1P[:, :], iwc[c][:, j:j + 1], reads=[iwc[c], w1c[r_][c]], writes=[w1c[r_][c]], bound=NE * 128 * 8 - 1)
            for c in range(8):
                k.gather(w3c[r_][c][:], W3P[:, :], iwc[c][:, j:j + 1], reads=[iwc[c], w3c[r_][c]], writes=[w3c[r_][c]], bound=NE * 128 * 8 - 1)
            for c in range(8):
                k.gather(w2c[c][:], W2P[:, :], iwc[c][:, j:j + 1], reads=[iwc[c], w2c[c]], writes=[w2c[c]], bound=NE * 128 * 8 - 1)
            for sub in range(BLKT):
                rr0 = (j * BLKT + sub) * 128
                k.dma("sp", xe[:], XE[rr0:rr0 + 128, :], reads=[XE], writes=[xe])
                for q in range(4):
                    for jj in range(8):
                        kc = q * 8 + jj
                        k.tr(PB[q][:, jj * 128:(jj + 1) * 128], xe[:, kc * 128:(kc + 1) * 128], identb[:], [xe, identb], [P[q]])
                    evac(xT[q][:], PB[q][:, 0:1024].rearrange("p (j n) -> p j n", n=128), [P[q]], [xT[q]])
                for kc in range(32):
                    k.mm(P[4][:, :], xT[kc // 8][:, kc % 8, :], w1c[r_][kc // 4][:, (kc % 4) * 512:(kc % 4 + 1) * 512], kc == 0, kc == 31, [xT[kc // 8], w1c[r_][kc // 4]], [P[4]])
                for kc in range(32):
                    k.mm(P[5][:, :], xT[kc // 8][:, kc % 8, :], w3c[r_][kc // 4][:, (kc % 4) * 512:(kc % 4 + 1) * 512], kc == 0, kc == 31, [xT[kc // 8], w3c[r_][kc // 4]], [P[5]])
                k.act(a_sb[:], P[4][:, :], AF.Silu, [P[4]], [a_sb])
                k.ve("tensor_tensor", [a_sb, P[5]], [act_], out=act_[:], in0=a_sb[:], in1=P[5][:, :], op=ALU.mult)
                for mc in range(4):
                    k.tr(PB[6][:, mc * 128:(mc + 1) * 128], act_[:, mc * 128:(mc + 1) * 128], identb[:], [act_, identb], [P[6]])
                evac(aT[:], PB[6][:, 0:512].rearrange("p (j n) -> p j n", n=128), [P[6]], [aT])
                for cc in range(8):
                    ps = P[cc % 4]
                    for kc in range(4):
                        k.mm(ps[:, :], aT[:, kc, :], w2c[kc * 2 + cc // 4][:, (cc % 4) * 512:(cc % 4 + 1) * 512], kc == 0, kc == 3, [aT, w2c[kc * 2 + cc // 4]], [ps])
                    ye = yes[cc // 4]
                    evac(ye[:, (cc % 4) * 512:(cc % 4 + 1) * 512], ps[:, :], [ps], [ye])
                    if cc % 4 == 3:
                        hf = cc // 4
                        k.dma("sp", YE[rr0:rr0 + 128, hf * 2048:(hf + 1) * 2048], ye[:], reads=[ye], writes=[YE])
        k.pop()

    if "s9" in stages:
        G_FIN = inp("g_fin", [1, D])
        k.push()
        slots9 = k.sb("slots9", [128, NT * 2], I32); gates9 = k.sb("gates9", [128, NT * 2], F32)
        k.dma("sp", slots9[:], SLOTS[:, :], reads=[SLOTS], writes=[slots9])
        k.dma("sp", gates9[:], GATES[:, :], reads=[GATES], writes=[gates9])
        GF = k.sb("GF", [128, D], F32); FG = k.sb("FG", [128, D], F32)
        bcast_row(FG, G_FIN[0:1, :])
        y1s = [k.sb("y1_%d" % i, [128, D], BF16) for i in range(2)]
        y2s = [k.sb("y2_%d" % i, [128, D], BF16) for i in range(2)]
        xts = [k.sb("x9_%d" % i, [128, D], F32) for i in range(2)]
        tts = [k.sb("t9_%d" % i, [128, D], F32) for i in range(2)]
        junk = k.sb("junk9", [128, D], BF16)
        ss = k.sb("ss9", [128, 1], F32); rstd = k.sb("rstd9", [128, 1], F32)
        for b in range(NB):
            load_mod_vec(GF, b, 5)
            for t in range(16):
                g = b * 16 + t
                r0 = g * 128
                y1, y2, xt, tt = y1s[g % 2], y2s[g % 2], xts[g % 2], tts[g % 2]
                k.gather(y1[:], YE[:, :], slots9[:, 2 * g:2 * g + 1], reads=[YE, slots9], writes=[y1])
                k.gather(y2[:], YE[:, :], slots9[:, 2 * g + 1:2 * g + 2], reads=[YE, slots9], writes=[y2])
                k.dma("sp", xt[:], X1[r0:r0 + 128, :], reads=[X1], writes=[xt])
                k.ve("tensor_scalar", [y1, gates9], [tt], out=tt[:], in0=y1[:], scalar1=gates9[:, 2 * g:2 * g + 1], scalar2=None, op0=ALU.mult)
                k.ve("scalar_tensor_tensor", [y2, gates9, tt], [tt], out=tt[:], in0=y2[:], scalar=gates9[:, 2 * g + 1:2 * g + 2], in1=tt[:], op0=ALU.mult, op1=ALU.add)
                k.ve("tensor_tensor", [tt, GF], [tt], out=tt[:], in0=tt[:], in1=GF[:], op=ALU.mult)
                k.ve("tensor_tensor", [tt, xt], [xt], out=xt[:], in0=tt[:], in1=xt[:], op=ALU.add)
                k.act(junk[:], xt[:], AF.Square, [xt], [junk, ss], accum_out=ss[:])
                rstd_from_ss(rstd, ss, D)
                k.ve("scalar_tensor_tensor", [xt, rstd, FG], [tt], out=tt[:], in0=xt[:], scalar=rstd[:, 0:1], in1=FG[:], op0=ALU.mult, op1=ALU.mult)
                k.dma("sp", OUT[r0:r0 + 128, :], tt[:], reads=[tt], writes=[OUT])
        k.pop()

    k.emit()
    cx.nc = nc
    return cx


def host_layouts(I, NB=NB_FULL):
    f = np.float32
    NT = NB * 16
    o = {}
    o["x"] = np.ascontiguousarray(I["x"][:NB].reshape(NB * SEQ, D))
    o["c"] = np.ascontiguousarray(I["c"][:NB])
    o["posT"] = np.ascontiguousarray(I["positions"][:NB].reshape(NT, 128).T.astype(np.int32))
    o["w_ada"] = I["w_ada"][0]
    o["b_ada"] = I["b_ada"][0].reshape(1, -1)
    o["g_mix"] = I["norm_mix_gain"][0].reshape(1, -1)
    o["g_ffn"] = I["norm_ffn_gain"][0].reshape(1, -1)
    o["g_fin"] = I["final_gain"].reshape(1, -1)
    o["w_in"] = I["w_in"][0]
    o["g_q"] = I["q_lat_gain"][0].reshape(1, -1)
    o["g_kv"] = I["kv_lat_gain"][0].reshape(1, -1)
    wq = I["w_uq"][0].reshape(QR, NH, 192)
    o["w_uqx"] = np.ascontiguousarray(np.concatenate(
        [wq[:, :, 0:192], wq[:, :, 160:192], wq[:, :, 128:160]], axis=2).reshape(QR, NH * 256))
    o["w_ukv"] = I["w_ukv"][0]
    lre, lim, ldt = I["ssm_lam_re"][0], I["ssm_lam_im"][0], I["ssm_log_dt"][0]
    o["lamre_T2"] = np.ascontiguousarray(np.concatenate([lre.T, lre.T], 0))
    o["lamim_T2"] = np.ascontiguousarray(np.concatenate([lim.T, lim.T], 0))
    o["logdt_bc"] = np.ascontiguousarray(np.broadcast_to(ldt[None, :], (128, NG)))

    def L2(a):
        return np.ascontiguousarray(a.reshape(16, 8, NST, 16).transpose(1, 3, 0, 2).reshape(128, 16, NST))
    o["bre_L2"] = L2(I["ssm_b_re"][0])
    o["bim_L2"] = L2(I["ssm_b_im"][0])
    o["lamre_L2"] = L2(np.broadcast_to(lre[:, :, None], (NG, NST, 16)))
    o["lamim_L2"] = L2(np.broadcast_to(lim[:, :, None], (NG, NST, 16)))
    o["logdt_L2"] = np.ascontiguousarray(np.broadcast_to(ldt.reshape(16, 8)[:, :, None], (16, 8, 16)).transpose(1, 2, 0).reshape(128, 16))
    cre, cim = I["ssm_c_re"][0], I["ssm_c_im"][0]
    o["c_L3"] = np.ascontiguousarray(np.concatenate(
        [cre.transpose(2, 0, 1).reshape(NST, NG * 16), cim.transpose(2, 0, 1).reshape(NST, NG * 16)], 0))
    o["d_L"] = np.ascontiguousarray(I["ssm_d"][0].reshape(16, 128).T)
    o["w_glu"] = I["w_glu"][0]
    o["bglu_L"] = np.ascontiguousarray(I["b_glu"][0].reshape(16, 128).T)
    o["gmla_L"] = np.ascontiguousarray(I["mla_out_gain"][0].reshape(16, 128).T)
    o["gssm_L"] = np.ascontiguousarray(I["ssm_out_gain"][0].reshape(16, 128).T)
    o["w_out"] = I["w_out"][0]
    o["w_r"] = np.ascontiguousarray(np.concatenate([I["w_group_router"][0], I["w_expert_router"][0]], 1))
    o["b_r"] = np.concatenate([I["b_group_router"][0], I["b_expert_router"][0]]).reshape(1, -1)
    o["w1p"] = lambda: np.ascontiguousarray(I["w1_experts"][0].reshape(NE, 32, 128, DE).transpose(0, 2, 1, 3)).reshape(NE * 128 * 8, 2048)
    o["w3p"] = lambda: np.ascontiguousarray(I["w3_experts"][0].reshape(NE, 32, 128, DE).transpose(0, 2, 1, 3)).reshape(NE * 128 * 8, 2048)
    o["w2p"] = lambda: np.ascontiguousarray(I["w2_experts"][0].reshape(NE, 4, 128, D).transpose(0, 2, 1, 3)).reshape(NE * 128 * 8, 2048)
    o["ident"] = np.eye(128, dtype=f)
    invf = np.exp(-math.log(10000.0) * np.arange(0, 64, 2, dtype=f) / 64).astype(f)
    o["invf_bc"] = np.ascontiguousarray(np.broadcast_to(invf[None, :], (128, 32)))
    m8 = np.zeros((128, 8), f)
    for gl in range(8):
        m8[gl * 16:(gl + 1) * 16, gl] = 1
    o["mask8"] = m8
    psw = np.zeros((128, 128), f)
    for p in range(64):
        psw[p + 64, p] = -1.0
        psw[p, p + 64] = 1.0
    o["psw"] = psw
    o["tri"] = np.triu(np.ones((128, 128), f), 1)
    NBLK = (NB * SEQ * 2 + NE * (BLK - 1)) // BLK
    o["jb"] = np.ascontiguousarray(np.broadcast_to((np.arange(NBLK, dtype=f) * BLK)[None, :], (128, NBLK)))
    o["pidx"] = np.arange(128, dtype=f).reshape(128, 1)
    o["iota_t"] = np.ascontiguousarray(np.broadcast_to(np.arange(SEQ, dtype=f)[None, :], (128, SEQ)))
    return o


_NPDT = {F32: np.float32, BF16: ml_dtypes.bfloat16, I32: np.int32}


N_CORES = 4


def kernel(**inputs):
    I = {k_: np.asarray(v) for k_, v in inputs.items()}
    cx = build(NB=1)
    shared = None
    in_maps = []
    for b in range(N_CORES):
        Ib = dict(I)
        Ib["x"] = I["x"][b:b + 1]
        Ib["c"] = I["c"][b:b + 1]
        Ib["positions"] = I["positions"][b:b + 1]
        if shared is None:
            H = host_layouts(Ib, 1)
            shared = {}
            for name, (shape, dt) in cx.inputs.items():
                if name in ("x", "c", "posT"):
                    continue
                v = H[name]() if callable(H[name]) else H[name]
                shared[name] = np.ascontiguousarray(np.asarray(v).astype(_NPDT[dt], copy=False)).reshape(shape)
        m = dict(shared)
        m["x"] = np.ascontiguousarray(Ib["x"].reshape(SEQ, D))
        m["c"] = np.ascontiguousarray(Ib["c"])
        m["posT"] = np.ascontiguousarray(Ib["positions"].reshape(16, 128).T.astype(np.int32))
        in_maps.append(m)
    res = run_bass_kernel_spmd(cx.nc, in_maps, core_ids=list(range(N_CORES)))
    out = np.stack([np.asarray(res.results[b]["out"], dtype=np.float32).reshape(SEQ, D) for b in range(N_CORES)], 0)
    return out
```

```python
import math
import numpy as np
import ml_dtypes
import concourse.bass as bass
import concourse.mybir as mybir
from contextlib import ExitStack
from concourse.bass_utils import run_bass_kernel_spmd

F32 = mybir.dt.float32
BF16 = mybir.dt.bfloat16
I32 = mybir.dt.int32
ALU = mybir.AluOpType
AF = mybir.ActivationFunctionType
AX = mybir.AxisListType

ENGS = ("pe", "dve", "act", "pool", "sp")
NDMASEM = 16
SB_BYTES = 200 * 1024


class T:
    def __init__(self, k, apv, name):
        self.k = k
        self.v = apv
        self.name = name
        self.writer = None
        self.readers = []
        self.birth = list(k.birth)

    def __getitem__(self, key):
        return self.v[key]


class Op:
    __slots__ = ("eng", "fn", "deps", "need_inc", "sem", "val", "is_dma")

    def __init__(self, eng, fn, is_dma=False):
        self.eng = eng
        self.fn = fn
        self.deps = []
        self.need_inc = False
        self.sem = None
        self.val = None
        self.is_dma = is_dma


class K:
    def __init__(self, nc):
        self.nc = nc
        self.es = ExitStack()
        self.ops = {e: [] for e in ENGS}
        self.birth = []
        self.last = {e: None for e in ENGS}
        self.dma_last = {}
        self.dma_ctr = {e: 0 for e in ENGS}
        self.big = self.es.enter_context(nc.sbuf_tensor("sbig", [128, SB_BYTES // 4], F32))
        self.bump = 0
        self.scopes = []
        self.nops = 0
        self.regcache = {}

    def sb(self, name, shape, dtype, parts=None):
        shape = list(shape)
        p = shape[0]
        n = int(np.prod(shape[1:]))
        esz = {F32: 4, BF16: 2, I32: 4}[dtype]
        nbytes = (n * esz + 31) // 32 * 32
        off = self.bump
        self.bump += nbytes
        assert self.bump <= SB_BYTES, "SBUF overflow %s %d" % (name, self.bump)
        v = self.big[0:p, off // 4:(off + nbytes) // 4]
        if dtype != F32:
            v = v.bitcast(dtype)
        v = v[:, 0:n]
        if len(shape) == 3:
            v = v.rearrange("p (a b) -> p a b", a=shape[1], b=shape[2])
        elif len(shape) == 4:
            v = v.rearrange("p (a b c) -> p a b c", a=shape[1], b=shape[2], c=shape[3])
        return T(self, v, name)

    def push(self):
        self.scopes.append(self.bump)

    def pop(self):
        self.bump = self.scopes.pop()
        self.barrier()

    def psum(self, name):
        h = self.es.enter_context(self.nc.psum_tensor(name, [128, 512], F32))
        return T(self, h[:], name)

    def dram(self, name, shape, dtype, kind=None):
        if kind is None:
            h = self.nc.dram_tensor(name, list(shape), dtype)
        else:
            h = self.nc.dram_tensor(name, list(shape), dtype, kind=kind)
        return T(self, h.ap(), name)

    def _add(self, op, reads, writes):
        deps = []
        for t in list(reads) + list(writes):
            deps.extend(t.birth)
        for t in reads:
            if t.writer is not None:
                deps.append(t.writer)
        for t in writes:
            if t.writer is not None:
                deps.append(t.writer)
            deps.extend(t.readers)
        seen = set(id(d) for d in op.deps)
        for d in deps:
            if d is op or id(d) in seen:
                continue
            seen.add(id(d))
            if d.eng == "pe" and op.eng == "pe" and not d.is_dma and not op.is_dma:
                continue
            op.deps.append(d)
            d.need_inc = True
        for t in reads:
            if not op.is_dma:
                t.readers = [r for r in t.readers if r.is_dma or r.eng != op.eng]
            t.readers.append(op)
        for t in writes:
            t.writer = op
            t.readers = []
        self.ops[op.eng].append(op)
        if not op.is_dma:
            self.last[op.eng] = op
        self.nops += 1
        return op

    def op(self, eng, fn, reads=(), writes=()):
        return self._add(Op(eng, fn), reads, writes)

    def _dma_op(self, eng, fn, reads, writes):
        op = Op(eng, fn, is_dma=True)
        slot = (eng, self.dma_ctr[eng] % NDMASEM)
        self.dma_ctr[eng] += 1
        prev = self.dma_last.get(slot)
        if prev is not None:
            op.deps.append(prev)
        op.need_inc = True
        op.sem = slot
        self.dma_last[slot] = op
        return self._add(op, reads, writes)

    def dma(self, eng, out, in_, reads=(), writes=(), **kw):
        return self._dma_op(eng, lambda e: e.dma_start(out=out, in_=in_, **kw), reads, writes)

    def scatter(self, out, idx_ap, in_, reads=(), writes=()):
        return self._dma_op("pool", lambda e: e.indirect_dma_start(
            out=out, out_offset=bass.IndirectOffsetOnAxis(ap=idx_ap, axis=0), in_=in_, in_offset=None), reads, writes)

    def gather(self, out, in_, idx_ap, reads=(), writes=(), bound=None):
        def fn(e):
            kw = {}
            if bound is not None:
                if bound not in self.regcache:
                    self.regcache[bound] = e.to_reg(bound)
                kw = dict(bounds_check=self.regcache[bound], oob_is_err=False)
            return e.indirect_dma_start(out=out, out_offset=None, in_=in_,
                                        in_offset=bass.IndirectOffsetOnAxis(ap=idx_ap, axis=0), **kw)
        return self._dma_op("pool", fn, reads, writes)

    def barrier(self):
        b = [o for o in self.last.values() if o is not None]
        b += list(self.dma_last.values())
        for o in b:
            o.need_inc = True
        self.birth = b

    def ve(self, name, reads, writes, eng="dve", **kw):
        return self.op(eng, lambda e: getattr(e, name)(**kw), reads, writes)

    def mm(self, out, lhsT, rhs, start, stop, reads, writes, **kw):
        return self.op("pe", lambda e: e.matmul(out, lhsT, rhs, start=start, stop=stop, **kw), reads, writes)

    def tr(self, out, in_, ident, reads, writes):
        return self.op("pe", lambda e: e.transpose(out, in_, ident), reads, writes)

    def act(self, out, in_, func, reads, writes, **kw):
        return self.op("act", lambda e: e.activation(out=out, in_=in_, func=func, **kw), reads, writes)

    def emit(self):
        nc = self.nc
        fin = Op("sp", None)
        fin.deps = list(self.dma_last.values()) + [o for o in self.last.values() if o is not None]
        for o in fin.deps:
            o.need_inc = True
        self.ops["sp"].append(fin)
        sems = {}
        for e in ENGS:
            sems[e] = self.es.enter_context(nc.semaphore("s_" + e))
            for i in range(NDMASEM):
                sems[(e, i)] = self.es.enter_context(nc.semaphore("d_%s%d" % (e, i)))
        cnt = {}
        for e in ENGS:
            for op in self.ops[e]:
                if op.is_dma:
                    cnt[op.sem] = cnt.get(op.sem, 0) + 16
                    op.val = cnt[op.sem]
                elif op.need_inc:
                    op.sem = e
                    cnt[e] = cnt.get(e, 0) + 1
                    op.val = cnt[e]
        self.sem_max = dict(cnt)
        engobj = {"pe": "tensor", "dve": "vector", "act": "scalar", "pool": "gpsimd", "sp": "sync"}
        block = self.es.enter_context(nc.Block())

        def make(e):
            def body(eng):
                waited = {}
                for op in self.ops[e]:
                    for d in op.deps:
                        if waited.get(d.sem, 0) < d.val:
                            eng.wait_ge(sems[d.sem], d.val)
                            waited[d.sem] = d.val
                    if op.fn is None:
                        continue
                    ins = op.fn(eng)
                    if op.is_dma:
                        ins.then_inc(sems[op.sem], 16)
                    elif op.need_inc:
                        ins.then_inc(sems[op.sem], 1)
            return body

        for e in ENGS:
            getattr(block, engobj[e])(make(e))
        self.es.close()


D = 4096
SEQ = 2048
NB_FULL = 4
QR, KVR, ROPE = 768, 512, 64
NH = 16
SSMW = 2048
NG = 128
NST = 64
IN_COLS = QR + KVR + ROPE + SSMW
TMC = QR + KVR + ROPE
NE = 64
DE = 512
EPS = 1e-6
BLKT = 2
BLK = BLKT * 128
TWO_PI = 2.0 * math.pi
C1 = 6.28125
C2 = float(TWO_PI - 6.28125)
ATT_SCALE = float((128 + 64) ** -0.5)
PI_LO = 3.1415925


class Cx:
    pass


def build(NB=NB_FULL, stages=None, inject=(), dump=()):
    allst = ["mod", "s1", "s2a", "s2b", "s3", "s4", "s5", "s6", "s7", "s8", "s9"]
    stages = set(allst if stages is None else stages)
    NT = NB * 16
    NTOK = NB * SEQ
    NBLK = (NTOK * 2 + NE * (BLK - 1)) // BLK
    nc = bass.Bass("TRN2", target_bir_lowering=False)
    k = K(nc)
    cx = Cx()
    cx.k, cx.NB, cx.NT = k, NB, NT
    cx.inputs = {}

    def inp(name, shape, dtype=F32):
        t = k.dram(name, shape, dtype, kind="ExternalInput")
        cx.inputs[name] = (tuple(shape), dtype)
        return t

    def scratch(name, shape, dtype):
        if name in inject:
            return inp(name, shape, dtype)
        if name in dump:
            return k.dram(name, shape, dtype, kind="ExternalOutput")
        return k.dram(name, shape, dtype)

    MOD = scratch("MOD", [NB, 6 * D], F32)
    HT = scratch("HT", [NB, 16, 128, 32 * 128], BF16)
    QNT = scratch("QNT", [NB, 6, 128, SEQ], BF16)
    KVNT = scratch("KVNT", [NB, 4, 128, SEQ], BF16)
    KRT = scratch("KRT", [NB, 64, SEQ], BF16)
    COST = scratch("COST", [NB, 64, SEQ], F32)
    SINT = scratch("SINT", [NB, 64, SEQ], F32)
    UT = scratch("UT", [NB, 16, 128, SEQ], BF16)
    OMT = scratch("OMT", [NB, 16, 128, SEQ], BF16)
    GT = scratch("GT", [NB, 16, 128, SEQ], BF16)
    RSM = scratch("RSM", [128, NT], F32)
    RSS = scratch("RSS", [128, NT], F32)
    OST = scratch("OST", [NB, 16, 128, SEQ], BF16)
    X1 = scratch("X1", [NTOK, D], F32)
    XE = scratch("XE", [NBLK * BLK, D], BF16)
    YE = scratch("YE", [NBLK * BLK, D], BF16)
    SLOTS = scratch("SLOTS", [128, NT * 2], I32)
    GATES = scratch("GATES", [128, NT * 2], F32)
    H2B = scratch("H2B", [NTOK, D], BF16)
    IWD = scratch("IWD", [128, NBLK], I32)
    OUT = k.dram("out", [NTOK, D], F32, kind="ExternalOutput")

    P = [k.psum("ps%d" % i) for i in range(8)]
    PB = [p_[:].bitcast(BF16) for p_ in P]

    identf = k.sb("identf", [128, 128], F32)
    identb = k.sb("identb", [128, 128], BF16)
    epsT = k.sb("epsT", [128, 1], F32)
    hpiT = k.sb("hpiT", [128, 1], F32)
    IDENT = inp("ident", [128, 128])
    k.dma("sp", identf[:], IDENT[:, :], writes=[identf])
    k.ve("tensor_copy", [identf], [identb], out=identb[:], in_=identf[:])
    k.ve("memset", [], [epsT], eng="pool", ap=epsT[:], constant=EPS)
    k.ve("memset", [], [hpiT], eng="pool", ap=hpiT[:], constant=math.pi / 2)
    cx.alt = 0
    cx.zf_done = False
    def emit_zero_fill():
        zt = k.sb("zt", [128, 2048], BF16)
        k.ve("memset", [], [zt], eng="pool", ap=zt[:], constant=0.0)
        cx.xez = []
        cx.zf_done = True
        for j in range(NBLK * BLKT):
            for hf in range(2):
                tz = T(k, XE.v, "xez")
                cx.xez.append(tz)
                k.dma("act", XE[j * 128:(j + 1) * 128, hf * 2048:(hf + 1) * 2048], zt[:], reads=[zt], writes=[tz])


    def evac(out, in_, reads, writes):
        cx.alt ^= 1
        if cx.alt:
            k.op("act", lambda e: e.copy(out=out, in_=in_), reads, writes)
        else:
            k.ve("tensor_copy", reads, writes, out=out, in_=in_)

    def rstd_from_ss(rstd, ss, n, reads_extra=()):
        k.act(rstd[:], ss[:], AF.Sqrt, [ss, epsT], [rstd], scale=1.0 / n, bias=epsT[:])
        k.ve("reciprocal", [rstd], [rstd], out=rstd[:], in_=rstd[:])

    def range_reduce(ang, ti, tf, act_cvt=False):
        k.ve("tensor_scalar", [ang], [ti], out=ti[:], in0=ang[:], scalar1=1.0 / TWO_PI, scalar2=None, op0=ALU.mult)
        if act_cvt:
            k.op("act", lambda e: e.copy(out=tf[:], in_=ti[:]), [ti], [tf])
        else:
            k.ve("tensor_copy", [ti], [tf], out=tf[:], in_=ti[:])
        k.ve("scalar_tensor_tensor", [tf, ang], [ang], out=ang[:], in0=tf[:], scalar=-C1, in1=ang[:], op0=ALU.mult, op1=ALU.add)
        k.ve("scalar_tensor_tensor", [tf, ang], [ang], out=ang[:], in0=tf[:], scalar=-C2, in1=ang[:], op0=ALU.mult, op1=ALU.add)
        k.ve("tensor_scalar", [ang], [ang], out=ang[:], in0=ang[:], scalar1=PI_LO, scalar2=-PI_LO, op0=ALU.min, op1=ALU.max)
        k.ve("scalar_tensor_tensor", [ang], [tf], out=tf[:], in0=ang[:], scalar=-1.0, in1=ang[:], op0=ALU.mult, op1=ALU.max)

    def bcast_row(dst, src_row_ap, eng="sp"):
        k.dma(eng, dst[:], src_row_ap.to_broadcast([128, src_row_ap.shape[-1]]), writes=[dst])

    if "mod" in stages:
        C_IN = inp("c", [NB, D])
        W_ADA = inp("w_ada", [D, 6 * D])
        B_ADA = inp("b_ada", [1, 6 * D])
        k.push()
        c4 = k.sb("c4", [NB, D], F32)
        k.dma("sp", c4[:], C_IN[:, :], writes=[c4])
        k.act(c4[:], c4[:], AF.Silu, [c4], [c4])
        for kc in range(32):
            k.tr(P[0][:, kc * NB:(kc + 1) * NB], c4[:, kc * 128:(kc + 1) * 128], identf[0:NB, 0:NB], [c4, identf], [P[0]])
        cT = k.sb("cT", [128, 32, NB], BF16)
        k.ve("tensor_copy", [P[0]], [cT], out=cT[:], in_=P[0][:, 0:32 * NB].rearrange("p (k b) -> p k b", b=NB))
        if "s7" in stages:
            emit_zero_fill()
        ones1 = k.sb("ones1", [1, NB], F32)
        k.ve("memset", [], [ones1], eng="pool", ap=ones1[:], constant=1.0)
        wq = [[k.sb("wa%d_%d" % (i, q), [128, 8, 512], BF16) for q in range(4)] for i in range(2)]
        bch = [k.sb("bch%d" % i, [1, 512], F32) for i in range(2)]
        mo = [k.sb("mo%d" % i, [NB, 512], F32) for i in range(2)]
        for ch in range(48):
            w = wq[ch % 2]
            for q in range(4):
                k.dma("pool", w[q][:], W_ADA[q * 1024:(q + 1) * 1024, ch * 512:(ch + 1) * 512].rearrange("(k p) n -> p k n", p=128), writes=[w[q]])
            bb = bch[ch % 2]
            k.dma("sp", bb[:], B_ADA[0:1, ch * 512:(ch + 1) * 512], writes=[bb])
            ps = P[1 + ch % 2]
            for kc in range(32):
                k.mm(ps[0:NB, :], cT[:, kc, :], w[kc // 8][:, kc % 8, :], kc == 0, False, [cT, w[kc // 8]], [ps])
            k.mm(ps[0:NB, :], ones1[:, :], bb[:, :], False, True, [ones1, bb], [ps])
            m = mo[ch % 2]
            k.ve("tensor_copy", [ps], [m], out=m[:], in_=ps[0:NB, :])
            k.dma("sp", MOD[:, ch * 512:(ch + 1) * 512], m[:], reads=[m], writes=[MOD])
        k.pop()

    if "s7" in stages and not cx.zf_done:
        emit_zero_fill()
    X_IN = inp("x", [NTOK, D])

    def load_mod_vec(dst, b, idx, eng="sp"):
        bcast_row(dst, MOD[b:b + 1, idx * D:(idx + 1) * D], eng)

    def norm_mod_tile(xt, A, SH, out_t, junk, ss, rstd):
        k.act(junk[:], xt[:], AF.Square, [xt], [junk, ss], accum_out=ss[:])
        rstd_from_ss(rstd, ss, D)
        k.ve("scalar_tensor_tensor", [xt, rstd, A], [xt], out=xt[:], in0=xt[:], scalar=rstd[:, 0:1], in1=A[:], op0=ALU.mult, op1=ALU.mult)
        k.ve("tensor_tensor", [xt, SH], [out_t], out=out_t[:], in0=xt[:], in1=SH[:], op=ALU.add)

    def make_A(A, gtmp, grow_ap):
        bcast_row(gtmp, grow_ap)
        k.ve("scalar_tensor_tensor", [A, gtmp], [A], out=A[:], in0=A[:], scalar=1.0, in1=gtmp[:], op0=ALU.add, op1=ALU.mult)

    if "s1" in stages:
        G_MIX = inp("g_mix", [1, D])
        for b in range(NB):
            k.push()
            Aa = k.sb("Aa", [128, D], F32)
            SHa = k.sb("SHa", [128, D], F32)
            gtmp = k.sb("gtmp", [128, D], F32)
            load_mod_vec(Aa, b, 1)
            load_mod_vec(SHa, b, 0)
            make_A(Aa, gtmp, G_MIX[0:1, :])
            xts = [k.sb("xt%d" % i, [128, D], F32) for i in range(2)]
            hbs = [k.sb("hb%d" % i, [128, D], BF16) for i in range(2)]
            junk = k.sb("junk", [128, D], BF16)
            ss = k.sb("ss", [128, 1], F32)
            rstd = k.sb("rstd", [128, 1], F32)
            hTq = [[k.sb("hT%d_%d" % (i, q), [128, 8, 128], BF16) for q in range(4)] for i in range(2)]
            for t in range(16):
                xt, hb, hT = xts[t % 2], hbs[t % 2], hTq[t % 2]
                r0 = b * SEQ + t * 128
                k.dma("sp", xt[:], X_IN[r0:r0 + 128, :], writes=[xt])
                norm_mod_tile(xt, Aa, SHa, hb, junk, ss, rstd)
                for q in range(4):
                    for j in range(8):
                        kc = q * 8 + j
                        k.tr(PB[q][:, j * 128:(j + 1) * 128], hb[:, kc * 128:(kc + 1) * 128], identb[:], [hb, identb], [P[q]])
                    evac(hT[q][:], PB[q][:, 0:1024].rearrange("p (j n) -> p j n", n=128), [P[q]], [hT[q]])
                    k.dma("sp", HT[b, t, :, q * 1024:(q + 1) * 1024], hT[q][:].rearrange("p k n -> p (k n)"), reads=[hT[q]], writes=[HT])
            k.pop()

    if "s2a" in stages:
        W_IN = inp("w_in", [D, IN_COLS])
        G_Q = inp("g_q", [1, QR])
        G_KV = inp("g_kv", [1, KVR])
        INVF = inp("invf_bc", [128, 32])
        POST = inp("posT", [128, NT], I32)
        k.push()
        Wtm = [k.sb("Wtm%d" % q, [128, 8, TMC], BF16) for q in range(4)]
        for q in range(4):
            k.dma("pool", Wtm[q][:], W_IN[q * 1024:(q + 1) * 1024, 0:TMC].rearrange("(k p) n -> p k n", p=128), writes=[Wtm[q]])
        gq = k.sb("gq", [128, QR], F32)
        gkv = k.sb("gkv", [128, KVR], F32)
        bcast_row(gq, G_Q[0:1, :])
        bcast_row(gkv, G_KV[0:1, :])
        invf = k.sb("invf", [128, 32], F32)
        k.dma("sp", invf[:], INVF[:, :], writes=[invf])
        posi = k.sb("posi", [128, NT], I32)
        posf = k.sb("posf", [128, NT], F32)
        k.dma("sp", posi[:], POST[:, :], writes=[posi])
        k.ve("tensor_copy", [posi], [posf], out=posf[:], in_=posi[:])
        hTs = [k.sb("hTt%d" % i, [128, 32, 128], BF16) for i in range(2)]
        zs = [k.sb("z%d" % i, [128, TMC], F32) for i in range(2)]
        junk = k.sb("junk2", [128, QR], BF16)
        ss = k.sb("ss2", [128, 2], F32)
        rs = k.sb("rs2", [128, 2], F32)
        qn = k.sb("qn", [128, QR], BF16)
        kvn = k.sb("kvn", [128, KVR], BF16)
        qnT = [k.sb("qnT%d" % i, [128, 6, 128], BF16) for i in range(2)]
        kvnT = [k.sb("kvnT%d" % i, [128, 4, 128], BF16) for i in range(2)]
        ang = k.sb("ang", [128, 32], F32)
        ti = k.sb("ti", [128, 32], I32)
        tf = k.sb("tf", [128, 32], F32)
        sn = k.sb("sn", [128, 32], F32)
        cs = k.sb("cs", [128, 32], F32)
        t1 = k.sb("t1", [128, 32], F32)
        t2 = k.sb("t2", [128, 32], F32)
        kr = k.sb("kr", [128, 64], BF16)
        krT = [k.sb("krT%d" % i, [64, 128], BF16) for i in range(2)]
        tabc = k.sb("tabc", [128, 64], F32)
        tabs = k.sb("tabs", [128, 64], F32)
        tabT = [k.sb("tabT%d" % i, [64, 256], F32) for i in range(2)]
        ssq, ssk = ss[:, 0:1], ss[:, 1:2]
        for b in range(NB):
            for t in range(16):
                g = b * 16 + t
                hT, z = hTs[t % 2], zs[t % 2]
                k.dma("sp", hT[:], HT[b, t].rearrange("p (k n) -> p k n", n=128), reads=[HT], writes=[hT])
                for bank, c0, c1 in ((0, 0, 512), (1, 512, 1024), (2, 1024, TMC)):
                    for kc in range(32):
                        k.mm(P[bank][:, 0:c1 - c0], hT[:, kc, :], Wtm[kc // 8][:, kc % 8, c0:c1], kc == 0, kc == 31, [hT, Wtm[kc // 8]], [P[bank]])
                    evac(z[:, c0:c1], P[bank][:, 0:c1 - c0], [P[bank]], [z])
                k.act(junk[:], z[:, 0:QR], AF.Square, [z], [junk, ss], accum_out=ssq)
                k.act(junk[:, 0:KVR], z[:, QR:QR + KVR], AF.Square, [z, junk], [junk, ss], accum_out=ssk)
                k.act(rs[:, 0:1], ssq, AF.Sqrt, [ss, epsT], [rs], scale=1.0 / QR, bias=epsT[:])
                k.act(rs[:, 1:2], ssk, AF.Sqrt, [ss, epsT, rs], [rs], scale=1.0 / KVR, bias=epsT[:])
                k.ve("reciprocal", [rs], [rs], out=rs[:], in_=rs[:])
                k.ve("scalar_tensor_tensor", [z, rs, gq], [qn], out=qn[:], in0=z[:, 0:QR], scalar=rs[:, 0:1], in1=gq[:], op0=ALU.mult, op1=ALU.mult)
                k.ve("scalar_tensor_tensor", [z, rs, gkv], [kvn], out=kvn[:], in0=z[:, QR:QR + KVR], scalar=rs[:, 1:2], in1=gkv[:], op0=ALU.mult, op1=ALU.mult)
                qT, kvT = qnT[t % 2], kvnT[t % 2]
                for j in range(6):
                    k.tr(PB[3][:, j * 128:(j + 1) * 128], qn[:, j * 128:(j + 1) * 128], identb[:], [qn, identb], [P[3]])
                evac(qT[:], PB[3][:, 0:768].rearrange("p (j n) -> p j n", n=128), [P[3]], [qT])
                k.dma("sp", QNT[b, :, :, t * 128:(t + 1) * 128].rearrange("k p n -> p k n"), qT[:], reads=[qT], writes=[QNT])
                for j in range(4):
                    k.tr(PB[4][:, j * 128:(j + 1) * 128], kvn[:, j * 128:(j + 1) * 128], identb[:], [kvn, identb], [P[4]])
                evac(kvT[:], PB[4][:, 0:512].rearrange("p (j n) -> p j n", n=128), [P[4]], [kvT])
                k.dma("sp", KVNT[b, :, :, t * 128:(t + 1) * 128].rearrange("k p n -> p k n"), kvT[:], reads=[kvT], writes=[KVNT])
                k.ve("tensor_scalar", [invf, posf], [ang], out=ang[:], in0=invf[:], scalar1=posf[:, g:g + 1], scalar2=None, op0=ALU.mult)
                range_reduce(ang, ti, tf)
                k.act(sn[:], ang[:], AF.Sin, [ang], [sn])
                k.act(cs[:], tf[:], AF.Sin, [tf, hpiT], [cs], scale=-1.0, bias=hpiT[:])
                x1, x2 = z[:, QR + KVR:QR + KVR + 32], z[:, QR + KVR + 32:TMC]
                k.ve("tensor_tensor", [z, cs], [t1], out=t1[:], in0=x1, in1=cs[:], op=ALU.mult)
                k.ve("tensor_tensor", [z, sn], [t2], out=t2[:], in0=x2, in1=sn[:], op=ALU.mult)
                k.ve("tensor_tensor", [t1, t2], [kr], out=kr[:, 0:32], in0=t1[:], in1=t2[:], op=ALU.subtract)
                k.ve("tensor_tensor", [z, cs], [t1], out=t1[:], in0=x2, in1=cs[:], op=ALU.mult)
                k.ve("tensor_tensor", [z, sn], [t2], out=t2[:], in0=x1, in1=sn[:], op=ALU.mult)
                k.ve("tensor_tensor", [t1, t2], [kr], out=kr[:, 32:64], in0=t1[:], in1=t2[:], op=ALU.add)
                kT = krT[t % 2]
                k.tr(PB[5][0:64, 0:128], kr[:, :], identb[:], [kr, identb], [P[5]])
                evac(kT[:], PB[5][0:64, 0:128], [P[5]], [kT])
                k.dma("sp", KRT[b, :, t * 128:(t + 1) * 128], kT[:], reads=[kT], writes=[KRT])
                k.ve("tensor_copy", [cs], [tabc], out=tabc[:, 0:32], in_=cs[:])
                k.ve("tensor_copy", [cs], [tabc], out=tabc[:, 32:64], in_=cs[:])
                k.ve("tensor_scalar", [sn], [tabs], out=tabs[:, 0:32], in0=sn[:], scalar1=-1.0, scalar2=None, op0=ALU.mult)
                k.ve("tensor_copy", [sn], [tabs], out=tabs[:, 32:64], in_=sn[:])
                tT = tabT[t % 2]
                k.tr(P[6][0:64, 0:128], tabc[:, :], identf[:], [tabc, identf], [P[6]])
                k.tr(P[6][0:64, 128:256], tabs[:, :], identf[:], [tabs, identf], [P[6]])
                evac(tT[:], P[6][0:64, 0:256], [P[6]], [tT])
                k.dma("sp", COST[b, :, t * 128:(t + 1) * 128], tT[:, 0:128], reads=[tT], writes=[COST])
                k.dma("sp", SINT[b, :, t * 128:(t + 1) * 128], tT[:, 128:256], reads=[tT], writes=[SINT])
        k.pop()

    if "s2b" in stages:
        W_IN2 = inp("w_in", [D, IN_COLS]) if "w_in" not in cx.inputs else W_IN
        k.push()
        hTp = [k.sb("hTp%d" % i, [128, 32, 512], BF16) for i in range(2)]
        hpq = [[T(k, hTp[i][:, :, jj * 128:(jj + 1) * 128], "hpq") for jj in range(4)] for i in range(2)]
        Wc = [[k.sb("Wc%d_%d" % (i, q), [128, 8, 512], BF16) for q in range(4)] for i in range(2)]
        uts = [k.sb("ut%d" % i, [128, 512], BF16) for i in range(4)]
        it = 0
        for b in range(NB):
            for pc in range(4):
                hp = hTp[(b * 4 + pc) % 2]
                for jj in range(4):
                    k.dma("sp", hp[:, :, jj * 128:(jj + 1) * 128], HT[b, pc * 4 + jj].rearrange("p (k n) -> p k n", n=128), reads=[HT], writes=[hpq[(b * 4 + pc) % 2][jj]])
                for ch in range(4):
                    w = Wc[it % 2]
                    it += 1
                    c0 = TMC + ch * 512
                    for q in range(4):
                        k.dma("pool", w[q][:], W_IN2[q * 1024:(q + 1) * 1024, c0:c0 + 512].rearrange("(k p) n -> p k n", p=128), writes=[w[q]])
                    for sub in range(4):
                        blk = ch * 4 + sub
                        ps = P[blk % 4]
                        for kc in range(32):
                            k.mm(ps[:, :], w[kc // 8][:, kc % 8, sub * 128:(sub + 1) * 128], hp[:, kc, :], kc == 0, kc == 31, [w[kc // 8]] + hpq[(b * 4 + pc) % 2], [ps])
                        ut = uts[blk % 4]
                        evac(ut[:], ps[:, :], [ps], [ut])
                        k.dma("sp", UT[b, blk, :, pc * 512:(pc + 1) * 512], ut[:], reads=[ut], writes=[UT])
        k.pop()

    if "s3" in stages:
        W_UQX = inp("w_uqx", [QR, NH * 256])
        W_UKV = inp("w_ukv", [KVR, NH * 256])
        GMLA = inp("gmla_L", [128, 16])
        k.push()
        gmla = k.sb("gmla", [128, 16], F32)
        k.dma("sp", gmla[:], GMLA[:, :], writes=[gmla])
        rsm = k.sb("rsm", [128, NT], F32)
        ssm_ = k.sb("ssm_", [128, NT], F32)
        for b in range(NB):
            k.push()
            qnT = k.sb("qnT", [128, 6, SEQ], BF16)
            kvnT = k.sb("kvnT", [128, 4, SEQ], BF16)
            krT = k.sb("krT", [64, SEQ], BF16)
            cosT = k.sb("cosT", [64, SEQ], F32)
            sinT = k.sb("sinT", [64, SEQ], F32)
            k.dma("sp", qnT[:], QNT[b].rearrange("k p n -> p k n"), reads=[QNT], writes=[qnT])
            k.dma("sp", kvnT[:], KVNT[b].rearrange("k p n -> p k n"), reads=[KVNT], writes=[kvnT])
            k.dma("sp", krT[:], KRT[b], reads=[KRT], writes=[krT])
            k.dma("sp", cosT[:], COST[b], reads=[COST], writes=[cosT])
            k.dma("sp", sinT[:], SINT[b], reads=[SINT], writes=[sinT])
            Om = [k.sb("Om%d" % t, [128, NH * 128], BF16) for t in range(16)]
            wqs = [k.sb("wq%d" % i, [128, 6, 256], BF16) for i in range(2)]
            wkvs = [k.sb("wkv%d" % i, [128, 4, 256], BF16) for i in range(2)]
            qTn = k.sb("qTn", [128, SEQ], BF16)
            qTr = k.sb("qTr", [64, SEQ], BF16)
            kTn = k.sb("kTn", [128, SEQ], BF16)
            vhs = [k.sb("vh%d" % i, [128, 16, 129], BF16) for i in range(2)]
            for v_ in vhs:
                k.ve("memset", [], [v_], eng="pool", ap=v_[:, :, 128:129], constant=1.0)
            PTs = [k.sb("PT%d" % i, [128, 512], BF16) for i in range(4)]
            rt1 = k.sb("rt1", [64, 512], F32)
            rt2 = k.sb("rt2", [64, 512], F32)
            rinv = k.sb("rinv", [128, 4], F32)
            pti = 0
            for h in range(NH):
                wq, wkv, vh = wqs[h % 2], wkvs[h % 2], vhs[h % 2]
                k.dma("pool", wq[:], W_UQX[:, h * 256:(h + 1) * 256].rearrange("(k p) n -> p k n", p=128), writes=[wq])
                k.dma("pool", wkv[:], W_UKV[:, h * 256:(h + 1) * 256].rearrange("(k p) n -> p k n", p=128), writes=[wkv])
                for pc in range(4):
                    sl = slice(pc * 512, (pc + 1) * 512)
                    for kc in range(6):
                        k.mm(P[0][:, :], wq[:, kc, 0:128], qnT[:, kc, sl], kc == 0, kc == 5, [wq, qnT], [P[0]])
                    for kc in range(6):
                        k.mm(P[1][0:64, :], wq[:, kc, 128:192], qnT[:, kc, sl], kc == 0, kc == 5, [wq, qnT], [P[1]])
                    for kc in range(6):
                        k.mm(P[2][0:64, :], wq[:, kc, 192:256], qnT[:, kc, sl], kc == 0, kc == 5, [wq, qnT], [P[2]])
                    k.op("act", lambda e, o=qTn[:, sl], i=P[0][:, :]: e.copy(out=o, in_=i), [P[0]], [qTn])
                    k.ve("tensor_tensor", [P[1], cosT], [rt1], out=rt1[:], in0=P[1][0:64, :], in1=cosT[:, sl], op=ALU.mult)
                    k.ve("tensor_tensor", [P[2], sinT], [rt2], out=rt2[:], in0=P[2][0:64, :], in1=sinT[:, sl], op=ALU.mult)
                    k.ve("tensor_tensor", [rt1, rt2], [qTr], out=qTr[:, sl], in0=rt1[:], in1=rt2[:], op=ALU.add)
                    for kc in range(4):
                        k.mm(P[3][:, :], wkv[:, kc, 0:128], kvnT[:, kc, sl], kc == 0, kc == 3, [wkv, kvnT], [P[3]])
                    k.op("act", lambda e, o=kTn[:, sl], i=P[3][:, :]: e.copy(out=o, in_=i), [P[3]], [kTn])
                for tg in range(4):
                    ps = P[tg % 2]
                    for j in range(4):
                        t = tg * 4 + j
                        for kc in range(4):
                            k.mm(ps[:, j * 128:(j + 1) * 128], kvnT[:, kc, t * 128:(t + 1) * 128], wkv[:, kc, 128:256], kc == 0, kc == 3, [kvnT, wkv], [ps])
                    evac(vh[:, tg * 4:(tg + 1) * 4, 0:128], ps[:, :].rearrange("p (j d) -> p j d", d=128), [ps], [vh])
                for p4 in range(4):
                    nkc = 4 * p4 + 4
                    O = [P[4 + i] for i in range(4)]
                    for kc in range(nkc):
                        j = kc - 4 * p4
                        jj = max(j, 0)
                        q0 = jj * 128
                        S = P[kc % 2]
                        qs = slice(p4 * 512 + q0, (p4 + 1) * 512)
                        k.mm(S[:, q0:512], kTn[:, kc * 128:(kc + 1) * 128], qTn[:, qs], True, False, [kTn, qTn], [S])
                        k.mm(S[:, q0:512], krT[:, kc * 128:(kc + 1) * 128], qTr[:, qs], False, True, [krT, qTr], [S])
                        PT = PTs[pti % 4]
                        pti += 1
                        k.act(PT[:, q0:512], S[:, q0:512], AF.Exp, [S], [PT], scale=ATT_SCALE)
                        if j >= 0:
                            k.ve("memset", [], [PT], eng="pool", ap=PT[64:128, q0:q0 + 64], constant=0.0)
                        for i in range(jj, 4):
                            k.mm(O[i][:, 0:129], PT[:, i * 128:(i + 1) * 128], vh[:, kc, :], kc == 0, kc == 4 * p4 + i, [PT, vh], [O[i]])
                    for i in range(4):
                        gi = 4 * p4 + i
                        k.ve("reciprocal", [O[i]], [rinv], out=rinv[:, i:i + 1], in_=O[i][:, 128:129])
                        k.ve("tensor_scalar", [O[i], rinv], [Om[gi]], out=Om[gi][:, h * 128:(h + 1) * 128], in0=O[i][:, 0:128], scalar1=rinv[:, i:i + 1], scalar2=None, op0=ALU.mult)
            junk = k.sb("junk3", [128, NH * 128], BF16)
            omT = [k.sb("omT%d" % i, [128, 1024], BF16) for i in range(2)]
            for t in range(16):
                g = b * 16 + t
                k.act(junk[:], Om[t][:], AF.Square, [Om[t]], [junk, ssm_], accum_out=ssm_[:, g:g + 1])
            k.act(rsm[:, b * 16:(b + 1) * 16], ssm_[:, b * 16:(b + 1) * 16], AF.Sqrt, [ssm_, epsT], [rsm], scale=1.0 / 2048, bias=epsT[:])
            k.ve("reciprocal", [rsm], [rsm], out=rsm[:, b * 16:(b + 1) * 16], in_=rsm[:, b * 16:(b + 1) * 16])
            it = 0
            for kc in range(16):
                for tg in range(2):
                    bank = it % 4
                    o_ = omT[it % 2]
                    it += 1
                    for j in range(8):
                        t = tg * 8 + j
                        k.tr(PB[bank][:, j * 128:(j + 1) * 128], Om[t][:, kc * 128:(kc + 1) * 128], identb[:], [Om[t], identb], [P[bank]])
                    k.ve("tensor_scalar", [P[bank], gmla], [o_], out=o_[:], in0=PB[bank][:, 0:1024], scalar1=gmla[:, kc:kc + 1], scalar2=None, op0=ALU.mult)
                    k.dma("sp", OMT[b, kc, :, tg * 1024:(tg + 1) * 1024], o_[:], reads=[o_], writes=[OMT])
            k.pop()
        k.dma("sp", RSM[:, :], rsm[:], reads=[rsm], writes=[RSM])
        k.pop()

    def bc3(ap2, n):
        return ap2.unsqueeze(2).to_broadcast([ap2.shape[0], ap2.shape[1], n])

    if "s4" in stages:
        k.push()
        LRE1 = inp("lamre_T2", [128, NG]); LIM1 = inp("lamim_T2", [128, NG]); LDT1 = inp("logdt_bc", [128, NG])
        BRE2 = inp("bre_L2", [128, 16, NST]); BIM2 = inp("bim_L2", [128, 16, NST])
        LRE2 = inp("lamre_L2", [128, 16, NST]); LIM2 = inp("lamim_L2", [128, 16, NST]); LDT2 = inp("logdt_L2", [128, 16])
        CL3 = inp("c_L3", [128, NG * 16]); DL = inp("d_L", [128, 16])
        MASK8 = inp("mask8", [128, 8]); PSWI = inp("psw", [128, 128]); IOTA = inp("iota_t", [128, SEQ])
        TH = k.sb("TH", [128, NG], F32); RR = k.sb("RR", [128, NG], F32)
        BT = k.sb("BT", [128, 16, 256], BF16)
        CS = k.sb("CS", [128, NG * 16], BF16)
        mask8 = k.sb("mask8", [128, 8], F32); psw = k.sb("psw", [128, 128], F32)
        iot = k.sb("iot", [128, SEQ], F32); dL = k.sb("dL", [128, 16], F32)
        k.dma("sp", mask8[:], MASK8[:, :], writes=[mask8]); k.dma("sp", psw[:], PSWI[:, :], writes=[psw])
        k.dma("sp", iot[:], IOTA[:, :], writes=[iot]); k.dma("sp", dL[:], DL[:, :], writes=[dL])
        k.push()
        a1 = k.sb("a1", [128, NG], F32); a2 = k.sb("a2", [128, NG], F32); a3 = k.sb("a3", [128, NG], F32)
        k.dma("sp", a1[:], LDT1[:, :], writes=[a1]); k.dma("sp", a2[:], LIM1[:, :], writes=[a2]); k.dma("sp", a3[:], LRE1[:, :], writes=[a3])
        k.act(a1[:], a1[:], AF.Exp, [a1], [a1])
        k.ve("tensor_tensor", [a2, a1], [TH], out=TH[:], in0=a2[:], in1=a1[:], op=ALU.mult)
        k.ve("tensor_tensor", [a3, a1], [a3], out=a3[:], in0=a3[:], in1=a1[:], op=ALU.mult)
        k.act(RR[:], a3[:], AF.Exp, [a3], [RR])
        sh3 = [128, 16, NST]
        lre = k.sb("lre", sh3, F32); lim = k.sb("lim", sh3, F32); bre = k.sb("bre", sh3, F32); bim = k.sb("bim", sh3, F32)
        dt2 = k.sb("dt2", [128, 16], F32)
        for t_, src in ((lre, LRE2), (lim, LIM2), (bre, BRE2), (bim, BIM2)):
            k.dma("sp", t_[:], src[:, :, :], writes=[t_])
        k.dma("sp", dt2[:], LDT2[:, :], writes=[dt2])
        k.act(dt2[:], dt2[:], AF.Exp, [dt2], [dt2])
        mag = k.sb("mag", sh3, F32); th = k.sb("th", sh3, F32); ti2 = k.sb("ti2", sh3, I32); tf2 = k.sb("tf2", sh3, F32)
        sn2 = k.sb("sn2", sh3, F32); cs2 = k.sb("cs2", sh3, F32)
        k.ve("tensor_tensor", [lre, dt2], [mag], out=mag[:], in0=lre[:], in1=bc3(dt2[:, :], NST), op=ALU.mult)
        k.act(mag[:], mag[:], AF.Exp, [mag], [mag])
        k.ve("tensor_tensor", [lim, dt2], [th], out=th[:], in0=lim[:], in1=bc3(dt2[:, :], NST), op=ALU.mult)
        range_reduce(th, ti2, tf2)
        k.act(sn2[:], th[:], AF.Sin, [th], [sn2])
        k.act(cs2[:], tf2[:], AF.Sin, [tf2, hpiT], [cs2], scale=-1.0, bias=hpiT[:])
        nr = k.sb("nr", sh3, F32); ni = k.sb("ni", sh3, F32); den = k.sb("den", sh3, F32); u1 = k.sb("u1", sh3, F32)
        fre = k.sb("fre", sh3, F32); fim = k.sb("fim", sh3, F32)
        TT = lambda o, a, b_, op: k.ve("tensor_tensor", [a, b_], [o], out=o[:], in0=a[:], in1=b_[:], op=op)
        TT(nr, mag, cs2, ALU.mult)
        k.ve("tensor_scalar", [nr], [nr], out=nr[:], in0=nr[:], scalar1=-1.0, scalar2=None, op0=ALU.add)
        TT(ni, mag, sn2, ALU.mult)
        TT(den, lre, lre, ALU.mult); TT(u1, lim, lim, ALU.mult); TT(den, den, u1, ALU.add)
        k.ve("reciprocal", [den], [den], out=den[:], in_=den[:])
        TT(fre, nr, lre, ALU.mult); TT(u1, ni, lim, ALU.mult); TT(fre, fre, u1, ALU.add); TT(fre, fre, den, ALU.mult)
        TT(fim, ni, lre, ALU.mult); TT(u1, nr, lim, ALU.mult); TT(fim, fim, u1, ALU.subtract); TT(fim, fim, den, ALU.mult)
        TT(nr, fre, bre, ALU.mult); TT(u1, fim, bim, ALU.mult)
        k.ve("tensor_tensor", [nr, u1], [BT], out=BT[:, :, 0:64], in0=nr[:], in1=u1[:], op=ALU.subtract)
        k.ve("tensor_tensor", [u1, nr], [BT], out=BT[:, :, 192:256], in0=u1[:], in1=nr[:], op=ALU.subtract)
        TT(ni, fre, bim, ALU.mult); TT(u1, fim, bre, ALU.mult)
        k.ve("tensor_tensor", [ni, u1], [BT], out=BT[:, :, 64:128], in0=ni[:], in1=u1[:], op=ALU.add)
        k.ve("tensor_tensor", [ni, u1], [BT], out=BT[:, :, 128:192], in0=ni[:], in1=u1[:], op=ALU.add)
        ctmp = k.sb("ctmp", [128, NG * 16], F32)
        k.dma("sp", ctmp[:], CL3[:, :], writes=[ctmp])
        k.ve("tensor_copy", [ctmp], [CS], out=CS[0:64, :], in_=ctmp[0:64, :])
        k.ve("tensor_scalar", [ctmp], [CS], out=CS[64:128, :], in0=ctmp[64:128, :], scalar1=-1.0, scalar2=None, op0=ALU.mult)
        k.pop()
        LBs = [k.sb("LB%d" % i, [128, 8, 256], BF16) for i in range(2)]
        LCs = [k.sb("LC%d" % i, [128, 8, 128], BF16) for i in range(2)]
        for l_ in LCs:
            k.ve("memset", [], [l_], eng="pool", ap=l_[:], constant=0.0)
        COSs = [k.sb("COS%d" % i, [128, SEQ], F32) for i in range(2)]
        SINs = [k.sb("SIN%d" % i, [128, SEQ], F32) for i in range(2)]
        ang = k.sb("angS", [128, SEQ], F32); tiS = k.sb("tiS", [128, SEQ], I32); tfS = k.sb("tfS", [128, SEQ], F32)
        uTs = [[k.sb("uT%d_%d" % (i, b), [128, SEQ], BF16) for b in range(NB)] for i in range(2)]
        yacc = [k.sb("yacc%d" % b, [128, SEQ], BF16) for b in range(NB)]
        ring = lambda nm, dt_, n=2: [k.sb("%s%d" % (nm, i), [128, 512], dt_) for i in range(n)]
        t1s, t2s, t3s, t4s, hhs = ring("t1_", BF16, 4), ring("t2_", BF16, 4), ring("t3_", BF16, 4), ring("t4_", BF16, 4), ring("hh", F32)
        yts = ring("yt", BF16, 4)
        gy = k.sb("gy", [128, 1024], F32); gy2 = k.sb("gy2", [128, 1024], F32); gin_ = k.sb("gin", [128, 1024], F32)
        gts = [k.sb("gt%d" % i, [128, 1024], BF16) for i in range(2)]
        yaccs = [yacc, [k.sb("yaccB%d" % b, [128, SEQ], BF16) for b in range(NB)]]
        gti = [0]

        def prep_block(gb):
            LB, LC, uT = LBs[gb % 2], LCs[gb % 2], uTs[gb % 2]
            k.ve("tensor_tensor", [BT, mask8], [LB], out=LB[:], in0=BT[:, gb, :].unsqueeze(1).to_broadcast([128, 8, 256]),
                 in1=bc3(mask8[:, :], 256), op=ALU.mult)
            for gl in range(8):
                g = gb * 8 + gl
                k.ve("tensor_copy", [CS], [LC], eng="pool", out=LC[:, gl, gl * 16:(gl + 1) * 16], in_=CS[:, g * 16:(g + 1) * 16])
            for b in range(NB):
                k.dma("sp", uT[b][:], UT[b, gb], reads=[UT], writes=[uT[b]])

        def table_steps(g):
            COS, SIN = COSs[g % 2], SINs[g % 2]
            st = []
            st.append(lambda: k.act(ang[:], iot[:], AF.Copy, [iot, TH], [ang], scale=TH[:, g:g + 1]))
            st.append(lambda: k.ve("tensor_scalar", [ang], [tiS], out=tiS[:], in0=ang[:], scalar1=1.0 / TWO_PI, scalar2=None, op0=ALU.mult))
            st.append(lambda: k.op("act", lambda e: e.copy(out=tfS[:], in_=tiS[:]), [tiS], [tfS]))
            st.append(lambda: k.ve("scalar_tensor_tensor", [tfS, ang], [ang], out=ang[:], in0=tfS[:], scalar=-C1, in1=ang[:], op0=ALU.mult, op1=ALU.add))
            st.append(lambda: k.ve("scalar_tensor_tensor", [tfS, ang], [ang], out=ang[:], in0=tfS[:], scalar=-C2, in1=ang[:], op0=ALU.mult, op1=ALU.add))
            st.append(lambda: k.act(tfS[:], ang[:], AF.Abs, [ang], [tfS]))
            st.append(lambda: k.act(SIN[:], ang[:], AF.Sin, [ang], [SIN], scale=0.999))
            st.append(lambda: k.act(COS[:], tfS[:], AF.Sin, [tfS, hpiT], [COS], scale=-0.999, bias=hpiT[:]))
            return st

        iters = [(gb, gl, b, pc) for gb in range(16) for gl in range(8) for b in range(NB) for pc in range(4)]
        NI = len(iters)

        def ctx_of(n):
            gb, gl, b, pc = iters[n]
            g = gb * 8 + gl
            r_ = n % 4
            return dict(gb=gb, gl=gl, b=b, pc=pc, g=g, r=r_, pa=P[n % 3], pb=P[3], ya_ps=P[4 + pc], sl=slice(pc * 512, (pc + 1) * 512),
                        LB=LBs[gb % 2], LC=LCs[gb % 2], uT=uTs[gb % 2][b], COS=COSs[g % 2], SIN=SINs[g % 2], hh=hhs[pc % 2],
                        yacc=yaccs[gb % 2][b])

        def stA(n):
            c = ctx_of(n)
            t1, t2 = t1s[c["r"]], t2s[c["r"]]
            k.mm(c["pa"][:, :], c["LB"][:, c["gl"], 0:128], c["uT"][:, c["sl"]], True, True, [c["LB"], c["uT"]], [c["pa"]])
            k.mm(c["pb"][:, :], c["LB"][:, c["gl"], 128:256], c["uT"][:, c["sl"]], True, True, [c["LB"], c["uT"]], [c["pb"]])
            k.ve("tensor_tensor", [c["pa"], c["COS"]], [t1], out=t1[:], in0=c["pa"][:, :], in1=c["COS"][:, c["sl"]], op=ALU.mult)
            k.ve("tensor_tensor", [c["pb"], c["SIN"]], [t2], out=t2[:], in0=c["pb"][:, :], in1=c["SIN"][:, c["sl"]], op=ALU.mult)
            k.mm(c["pa"][:, :], identb[:, :], t1[:, :], True, False, [identb, t1], [c["pa"]])
            k.mm(c["pa"][:, :], identb[:, :], t2[:, :], False, True, [identb, t2], [c["pa"]])

        def stB(n):
            c = ctx_of(n)
            t3, hh, pc = t3s[c["r"]], c["hh"], c["pc"]
            init = 0.0 if pc == 0 else hhs[(pc - 1) % 2][:, 511:512]
            rd = [RR, c["pa"]] + ([] if pc == 0 else [hhs[(pc - 1) % 2]])
            k.ve("tensor_tensor_scan", rd, [hh], out=hh[:], data0=RR[:, c["g"]:c["g"] + 1].to_broadcast([128, 512]), data1=c["pa"][:, :],
                 initial=init, op0=ALU.mult, op1=ALU.add)
            k.mm(c["pa"][:, :], psw[:, :], hh[:, :], True, True, [psw, hh], [c["pa"]])
            k.ve("tensor_tensor", [hh, c["COS"]], [t3], eng="pool", out=t3[:], in0=hh[:], in1=c["COS"][:, c["sl"]], op=ALU.mult)

        def stC(n):
            c = ctx_of(n)
            t3, t4 = t3s[c["r"]], t4s[c["r"]]
            k.ve("tensor_tensor", [c["pa"], c["SIN"]], [t4], out=t4[:], in0=c["pa"][:, :], in1=c["SIN"][:, c["sl"]], op=ALU.mult)
            k.mm(c["ya_ps"][:, :], c["LC"][:, c["gl"], :], t3[:, :], c["gl"] == 0, False, [c["LC"], t3], [c["ya_ps"]])
            k.mm(c["ya_ps"][:, :], c["LC"][:, c["gl"], :], t4[:, :], False, c["gl"] == 7, [c["LC"], t4], [c["ya_ps"]])
            if c["gl"] == 7:
                finish_piece(c["gb"], c["b"], c["pc"])

        def finish_piece(gb, b, pc):
            assert NB == 1
            uT = uTs[gb % 2][b]
            sl = slice(pc * 512, (pc + 1) * 512)
            hs = slice(0, 512)
            gt = gts[gti[0] % 2]
            gti[0] += 1
            k.ve("scalar_tensor_tensor", [uT, dL, P[4 + pc]], [gy], out=gy[:, hs], in0=uT[:, sl], scalar=dL[:, gb:gb + 1], in1=P[4 + pc][:, :], op0=ALU.mult, op1=ALU.add)
            k.ve("tensor_tensor", [gy], [gy2], out=gy2[:, hs], in0=gy[:, hs], in1=gy[:, hs], op=ALU.mult)
            k.ve("tensor_scalar", [gy2], [gy2], out=gy2[:, hs], in0=gy2[:, hs], scalar1=0.044715, scalar2=1.0, op0=ALU.mult, op1=ALU.add)
            k.ve("tensor_tensor", [gy2, gy], [gin_], eng="pool", out=gin_[:, hs], in0=gy2[:, hs], in1=gy[:, hs], op=ALU.mult)
            k.act(gin_[:, hs], gin_[:, hs], AF.Sigmoid, [gin_], [gin_], scale=1.5957691216057308)
            k.ve("tensor_tensor", [gin_, gy], [gt], eng="pool", out=gt[:, hs], in0=gin_[:, hs], in1=gy[:, hs], op=ALU.mult)
            k.dma("sp", GT[b, gb, :, sl], gt[:, hs], reads=[gt], writes=[GT])

        prep_block(0)
        for f in table_steps(0):
            f()
        per_grp = NB * 4
        pending = []
        for n in range(NI + 2):
            if n < NI:
                gb, gl, b, pc = iters[n]
                g = gb * 8 + gl
                if b == 0 and pc == 0:
                    if gl == 4 and gb + 1 < 16:
                        prep_block(gb + 1)
                    pending = table_steps(g + 1) if g + 1 < NG else []
                stA(n)
            if 0 <= n - 1 < NI:
                stB(n - 1)
            if 0 <= n - 2 < NI:
                stC(n - 2)
            if n < NI:
                pos = (iters[n][2] * 4 + iters[n][3])
                lo = len(pending) * pos // per_grp if pending else 0
                if pos == 0:
                    cx.tsteps = pending
                K_ = len(cx.tsteps)
                for i_ in range(K_ * pos // per_grp, K_ * (pos + 1) // per_grp):
                    cx.tsteps[i_]()
        k.pop()

    if "s5" in stages:
        W_GLU = inp("w_glu", [SSMW, SSMW])
        BGLU = inp("bglu_L", [128, 16]); GSSM = inp("gssm_L", [128, 16])
        k.push()
        bglu = k.sb("bglu", [128, 16], F32); gssm = k.sb("gssm", [128, 16], F32)
        k.dma("sp", bglu[:], BGLU[:, :], writes=[bglu]); k.dma("sp", gssm[:], GSSM[:, :], writes=[gssm])
        onesb = k.sb("onesb", [128, 1], BF16)
        k.ve("memset", [], [onesb], eng="pool", ap=onesb[:], constant=1.0)
        rss = k.sb("rss", [128, NT], F32)
        gTk = [k.sb("gTk%d" % i, [128, SEQ], BF16) for i in range(16)]
        wgs = [k.sb("wg%d" % i, [128, 16, 128], BF16) for i in range(2)]
        sgs = [k.sb("sg%d" % i, [128, 512], F32) for i in range(2)]
        ors = [k.sb("or%d" % i, [128, 512], F32) for i in range(2)]
        sqs = [k.sb("sq%d" % i, [128, 512], BF16) for i in range(2)]
        osts = [k.sb("ost%d" % i, [128, 512], BF16) for i in range(2)]
        for b in range(NB):
            for kc in range(16):
                k.dma("sp", gTk[kc][:], GT[b, kc], reads=[GT], writes=[gTk[kc]])
            it = 0
            for cb in range(16):
                wg = wgs[cb % 2]
                k.dma("pool", wg[:], W_GLU[:, cb * 128:(cb + 1) * 128].rearrange("(k p) n -> p k n", p=128), writes=[wg])
                for pc in range(4):
                    sl = slice(pc * 512, (pc + 1) * 512)
                    r_ = it % 2
                    it += 1
                    ps = P[r_]
                    for kc in range(16):
                        k.mm(ps[:, :], wg[:, kc, :], gTk[kc][:, sl], kc == 0, kc == 15, [wg, gTk[kc]], [ps])
                    sg, orr, sq, ost = sgs[r_], ors[r_], sqs[r_], osts[r_]
                    k.act(sg[:], ps[:, :], AF.Sigmoid, [ps, bglu], [sg], bias=bglu[:, cb:cb + 1])
                    k.ve("tensor_tensor", [sg, gTk[cb]], [orr], out=orr[:], in0=sg[:], in1=gTk[cb][:, sl], op=ALU.mult)
                    k.act(sq[:], orr[:], AF.Square, [orr], [sq])
                    for j in range(4):
                        t = pc * 4 + j
                        first = (cb == 0 and pc == 0 and j == 0)
                        k.mm(P[7][:, t:t + 1], sq[:, j * 128:(j + 1) * 128], onesb[:, :], first, cb == 15, [sq, onesb], [P[7]], skip_group_check=True)
                    k.ve("tensor_scalar", [orr, gssm], [ost], out=ost[:], in0=orr[:], scalar1=gssm[:, cb:cb + 1], scalar2=None, op0=ALU.mult)
                    k.dma("sp", OST[b, cb, :, sl], ost[:], reads=[ost], writes=[OST])
            k.act(rss[:, b * 16:(b + 1) * 16], P[7][:, 0:16], AF.Sqrt, [P[7], epsT], [rss], scale=1.0 / SSMW, bias=epsT[:])
            k.ve("reciprocal", [rss], [rss], out=rss[:, b * 16:(b + 1) * 16], in_=rss[:, b * 16:(b + 1) * 16])
        k.dma("sp", RSS[:, :], rss[:], reads=[rss], writes=[RSS])
        k.pop()

    if "s6" in stages:
        W_OUT = inp("w_out", [D, D])
        k.push()
        rsm6 = k.sb("rsm6", [128, NT], F32); rss6 = k.sb("rss6", [128, NT], F32)
        k.dma("sp", rsm6[:], RSM[:, :], reads=[RSM], writes=[rsm6]); k.dma("sp", rss6[:], RSS[:, :], reads=[RSS], writes=[rss6])
        GA = k.sb("GA", [128, D], F32)
        oTm = [k.sb("oTm%d" % i, [128, 16, 512], BF16) for i in range(2)]
        oTs = [k.sb("oTs%d" % i, [128, 16, 512], BF16) for i in range(2)]
        Wo = [[k.sb("Wo%d_%d" % (i, q), [128, 8, 512], BF16) for q in range(4)] for i in range(2)]
        xins = [k.sb("xin%d" % i, [128, 512], F32) for i in range(2)]
        tts = [k.sb("tt%d" % i, [128, 512], F32) for i in range(2)]
        it = 0
        wi = 0
        for b in range(NB):
            load_mod_vec(GA, b, 2)
            for tg in range(4):
                om, os_ = oTm[(b * 4 + tg) % 2], oTs[(b * 4 + tg) % 2]
                tsl = slice(tg * 512, (tg + 1) * 512)
                k.dma("sp", om[:], OMT[b, :, :, tsl].rearrange("k p n -> p k n"), reads=[OMT], writes=[om])
                k.dma("sp", os_[:], OST[b, :, :, tsl].rearrange("k p n -> p k n"), reads=[OST], writes=[os_])
                for cc in range(8):
                    w = Wo[wi % 2]
                    wi += 1
                    csl = slice(cc * 512, (cc + 1) * 512)
                    for q in range(4):
                        k.dma("pool", w[q][:], W_OUT[q * 1024:(q + 1) * 1024, csl].rearrange("(k p) n -> p k n", p=128), writes=[w[q]])
                    for j in range(4):
                        tile = tg * 4 + j
                        g = b * 16 + tile
                        r0 = b * SEQ + tile * 128
                        r_ = it % 2
                        it += 1
                        pa, pb_ = P[2 * r_], P[2 * r_ + 1]
                        for kc in range(16):
                            k.mm(pa[:, :], om[:, kc, j * 128:(j + 1) * 128], w[kc // 8][:, kc % 8, :], kc == 0, kc == 15, [om, w[kc // 8]], [pa])
                        for kc in range(16):
                            k.mm(pb_[:, :], os_[:, kc, j * 128:(j + 1) * 128], w[2 + kc // 8][:, kc % 8, :], kc == 0, kc == 15, [os_, w[2 + kc // 8]], [pb_])
                        xin, tt = xins[r_], tts[r_]
                        k.dma("sp", xin[:], X_IN[r0:r0 + 128, csl], writes=[xin])
                        k.ve("tensor_scalar", [pa, rsm6], [tt], out=tt[:], in0=pa[:, :], scalar1=rsm6[:, g:g + 1], scalar2=None, op0=ALU.mult)
                        k.ve("scalar_tensor_tensor", [pb_, rss6, tt], [tt], out=tt[:], in0=pb_[:, :], scalar=rss6[:, g:g + 1], in1=tt[:], op0=ALU.mult, op1=ALU.add)
                        k.ve("tensor_tensor", [tt, GA], [tt], out=tt[:], in0=tt[:], in1=GA[:, csl], op=ALU.mult)
                        k.ve("tensor_tensor", [tt, xin], [tt], out=tt[:], in0=tt[:], in1=xin[:], op=ALU.add)
                        k.dma("sp", X1[r0:r0 + 128, csl], tt[:], reads=[tt], writes=[X1])
        k.pop()

    if "s7" in stages:
        G_FFN = inp("g_ffn", [1, D])
        W_R = inp("w_r", [D, 72]); B_R = inp("b_r", [1, 72])
        TRI = inp("tri", [128, 128]); JB = inp("jb", [128, NBLK]); PIDX = inp("pidx", [128, 1])
        k.push()
        sel12 = k.sb("sel12", [128, NT, 2, 64], BF16)
        gates = k.sb("gates", [128, NT * 2], F32)
        slots = k.sb("slots", [128, NT * 2], I32)
        WR = k.sb("WR", [128, 32, 72], F32)
        k.dma("sp", WR[:], W_R[:, :].rearrange("(k p) n -> p k n", p=128), writes=[WR])
        br = k.sb("br", [1, 72], F32); ones1f = k.sb("ones1f", [1, 128], F32)
        k.dma("sp", br[:], B_R[:, :], writes=[br])
        k.ve("memset", [], [ones1f], eng="pool", ap=ones1f[:], constant=1.0)
        onesB = k.sb("onesB", [128, 128], BF16); trib = k.sb("trib", [128, 128], BF16); trif = k.sb("trif", [128, 128], F32)
        k.ve("memset", [], [onesB], eng="pool", ap=onesB[:], constant=1.0)
        k.dma("sp", trif[:], TRI[:, :], writes=[trif])
        k.ve("tensor_copy", [trif], [trib], out=trib[:], in_=trif[:])
        k.push()
        Af = k.sb("Af", [128, D], F32); SHf = k.sb("SHf", [128, D], F32); gtmp = k.sb("gtmp7", [128, D], F32)
        xts = [k.sb("x1t%d" % i, [128, D], F32) for i in range(2)]
        junk = k.sb("junk7", [128, D], BF16)
        h2bs = [k.sb("h2b%d" % i, [128, D], BF16) for i in range(2)]
        h2T = k.sb("h2T", [128, 32, 128], F32)
        ss = k.sb("ss7", [128, 1], F32); rstd = k.sb("rstd7", [128, 1], F32)
        lg = k.sb("lg", [128, 72], F32); m8 = k.sb("m8", [128, 8], F32); oh = k.sb("oh", [128, 8], F32)
        ngm = k.sb("ngm", [128, 1], F32); ex = k.sb("ex", [128, 8], F32); se = k.sb("se", [128, 1], F32)
        pen = k.sb("pen", [128, 8], F32); em = k.sb("em", [128, 8, 8], F32); t8 = k.sb("t8", [128, 8], F32)
        dl = k.sb("dl", [128, 1], F32); wv = k.sb("wv", [128, 2], F32); ssum = k.sb("ssum", [128, 64], BF16)
        for b in range(NB):
            load_mod_vec(Af, b, 4)
            load_mod_vec(SHf, b, 3)
            make_A(Af, gtmp, G_FFN[0:1, :])
            for t in range(16):
                g = b * 16 + t
                r0 = g * 128
                xt, h2b = xts[g % 2], h2bs[g % 2]
                k.dma("sp", xt[:], X1[r0:r0 + 128, :], reads=[X1], writes=[xt])
                norm_mod_tile(xt, Af, SHf, xt, junk, ss, rstd)
                k.op("act", lambda e, o=h2b[:], i=xt[:]: e.copy(out=o, in_=i), [xt], [h2b])
                k.dma("sp", H2B[r0:r0 + 128, :], h2b[:], reads=[h2b], writes=[H2B])
                for rnd in range(2):
                    for q in range(4):
                        for j in range(4):
                            kc = rnd * 16 + q * 4 + j
                            k.tr(P[q][:, j * 128:(j + 1) * 128], xt[:, kc * 128:(kc + 1) * 128], identf[:], [xt, identf], [P[q]])
                        kc0 = rnd * 16 + q * 4
                        evac(h2T[:, kc0:kc0 + 4, :], P[q][:, :].rearrange("p (j n) -> p j n", n=128), [P[q]], [h2T])
                for kc in range(32):
                    k.mm(P[4][:, 0:72], h2T[:, kc, :], WR[:, kc, :], kc == 0, False, [h2T, WR], [P[4]])
                k.mm(P[4][:, 0:72], ones1f[:, :], br[:, :], False, True, [ones1f, br], [P[4]])
                k.ve("tensor_copy", [P[4]], [lg], out=lg[:], in_=P[4][:, 0:72])
                k.ve("max", [lg], [m8], out=m8[:], in_=lg[:, 0:8])
                k.ve("tensor_scalar", [lg, m8], [oh], out=oh[:], in0=lg[:, 0:8], scalar1=m8[:, 0:1], scalar2=None, op0=ALU.is_equal)
                k.ve("tensor_scalar", [m8], [ngm], out=ngm[:], in0=m8[:, 0:1], scalar1=-1.0, scalar2=None, op0=ALU.mult)
                k.act(ex[:], lg[:, 0:8], AF.Exp, [lg, ngm], [ex, se], bias=ngm[:], accum_out=se[:])
                k.ve("reciprocal", [se], [se], out=se[:], in_=se[:])
                k.ve("tensor_scalar", [oh], [pen], out=pen[:], in0=oh[:], scalar1=1e30, scalar2=-1e30, op0=ALU.mult, op1=ALU.add)
                k.ve("tensor_tensor", [lg, pen], [em], out=em[:], in0=lg[:, 8:72].rearrange("p (a b) -> p a b", b=8), in1=bc3(pen[:, :], 8), op=ALU.add)
                emf = em[:].rearrange("p a b -> p (a b)")
                k.ve("max", [em], [t8], out=t8[:], in_=emf)
                k.ve("tensor_scalar", [em, t8], [sel12], out=sel12[:, g, 0, :], in0=emf, scalar1=t8[:, 0:1], scalar2=None, op0=ALU.is_equal)
                k.ve("tensor_scalar", [em, t8], [sel12], out=sel12[:, g, 1, :], in0=emf, scalar1=t8[:, 1:2], scalar2=None, op0=ALU.is_equal)
                k.ve("tensor_tensor", [t8], [dl], out=dl[:], in0=t8[:, 0:1], in1=t8[:, 1:2], op=ALU.subtract)
                k.act(wv[:, 0:1], dl[:], AF.Sigmoid, [dl], [wv])
                k.act(wv[:, 1:2], dl[:], AF.Sigmoid, [dl, wv], [wv], scale=-1.0)
                k.ve("tensor_scalar", [wv, se], [gates], out=gates[:, 2 * g:2 * g + 2], in0=wv[:], scalar1=se[:, 0:1], scalar2=None, op0=ALU.mult)
                k.ve("tensor_tensor", [sel12], [ssum], out=ssum[:], in0=sel12[:, g, 0, :], in1=sel12[:, g, 1, :], op=ALU.add)
                k.mm(P[7][:, 0:64], onesB[:, :], ssum[:, :], g == 0, g == NT - 1, [onesB, ssum], [P[7]])
        k.pop()
        k.push()
        cnt = k.sb("cnt", [128, 64], F32); pad = k.sb("pad", [128, 64], F32); padi = k.sb("padi", [128, 64], I32)
        pends = k.sb("pends", [128, 64], F32); base = k.sb("base", [128, 64], F32); one64 = k.sb("one64", [128, 64], F32)
        k.ve("memset", [], [one64], eng="pool", ap=one64[:], constant=1.0)
        k.ve("tensor_copy", [P[7]], [cnt], out=cnt[:], in_=P[7][:, 0:64])
        k.ve("tensor_scalar", [cnt], [padi], out=padi[:], in0=cnt[:], scalar1=1.0 / BLK, scalar2=(BLK - 1.0) / BLK - 0.5 + 0.5 / BLK, op0=ALU.mult, op1=ALU.add)
        k.ve("tensor_copy", [padi], [pad], out=pad[:], in_=padi[:])
        k.ve("tensor_scalar", [pad], [pad], out=pad[:], in0=pad[:], scalar1=float(BLK), scalar2=None, op0=ALU.mult)
        k.ve("tensor_tensor_scan", [one64, pad], [pends], out=pends[:], data0=one64[:], data1=pad[:], initial=0.0, op0=ALU.mult, op1=ALU.add)
        k.ve("tensor_tensor", [pends, pad], [base], out=base[:], in0=pends[:], in1=pad[:], op=ALU.subtract)
        jb = k.sb("jb", [128, NBLK], F32); pidx = k.sb("pidx", [128, 1], F32)
        k.dma("sp", jb[:], JB[:, :], writes=[jb]); k.dma("sp", pidx[:], PIDX[:, :], writes=[pidx])
        bexp = k.sb("bexp", [128, NBLK], F32); iw = k.sb("iw", [128, NBLK], I32)
        CH = 48
        cmp_ = k.sb("cmp", [128, CH, 64], F32)
        for c0 in range(0, NBLK, CH):
            n = min(CH, NBLK - c0)
            k.ve("tensor_tensor", [pends, jb], [cmp_], out=cmp_[:, 0:n, :], in0=pends[:, :].unsqueeze(1).to_broadcast([128, n, 64]),
                 in1=bc3(jb[:, c0:c0 + n], 64), op=ALU.is_le)
            k.ve("tensor_reduce", [cmp_], [bexp], out=bexp[:, c0:c0 + n], in_=cmp_[:, 0:n, :], axis=AX.X, op=ALU.add)
        k.ve("tensor_scalar", [bexp], [bexp], out=bexp[:], in0=bexp[:], scalar1=63.0, scalar2=128.0, op0=ALU.min, op1=ALU.mult)
        tl = k.sb("tl", [128, NBLK], F32)
        k.ve("tensor_scalar", [jb, pends], [tl], out=tl[:], in0=jb[:], scalar1=pends[:, 63:64], scalar2=1.0e7, op0=ALU.is_ge, op1=ALU.mult)
        k.ve("tensor_tensor", [bexp, tl], [bexp], out=bexp[:], in0=bexp[:], in1=tl[:], op=ALU.add)
        k.ve("tensor_scalar", [bexp, pidx], [iw], out=iw[:], in0=bexp[:], scalar1=pidx[:, 0:1], scalar2=None, op0=ALU.add)
        k.dma("sp", IWD[:, :], iw[:], reads=[iw], writes=[IWD])
        h2bs = [k.sb("h2c%d" % i, [128, D], BF16) for i in range(2)]
        ssum = k.sb("ssumB", [128, 64], BF16); vv = k.sb("vv", [128, 64], F32); tmp = k.sb("tmpB", [128, 64], F32)
        df = k.sb("df", [128, 2], F32)
        for g in range(NT):
            h2b = h2bs[g % 2]
            r0 = g * 128
            k.dma("sp", h2b[:], H2B[r0:r0 + 128, :], reads=[H2B], writes=[h2b])
            k.ve("tensor_tensor", [sel12], [ssum], out=ssum[:], in0=sel12[:, g, 0, :], in1=sel12[:, g, 1, :], op=ALU.add)
            k.mm(P[5][:, 0:64], trib[:, :], ssum[:, :], True, True, [trib, ssum], [P[5]])
            k.mm(P[6][:, 0:64], onesB[:, :], ssum[:, :], True, True, [onesB, ssum], [P[6]])
            k.ve("tensor_tensor", [P[5], base], [vv], out=vv[:], in0=P[5][:, 0:64], in1=base[:], op=ALU.add)
            k.ve("tensor_tensor", [P[6], base], [base], out=base[:], in0=P[6][:, 0:64], in1=base[:], op=ALU.add)
            for s_ in range(2):
                k.ve("tensor_tensor", [sel12, vv], [tmp], out=tmp[:], in0=sel12[:, g, s_, :], in1=vv[:], op=ALU.mult)
                k.ve("tensor_reduce", [tmp], [df], out=df[:, s_:s_ + 1], in_=tmp[:], axis=AX.X, op=ALU.add)
            k.ve("tensor_copy", [df], [slots], out=slots[:, 2 * g:2 * g + 2], in_=df[:])
            for s_ in range(2):
                k.scatter(XE[:, :], slots[:, 2 * g + s_:2 * g + s_ + 1], h2b[:], reads=[h2b, slots] + (cx.xez if (g == 0 and s_ == 0) else []), writes=[XE])
        k.dma("sp", SLOTS[:, :], slots[:], reads=[slots], writes=[SLOTS])
        k.dma("sp", GATES[:, :], gates[:], reads=[gates], writes=[GATES])
        k.pop()
        k.pop()

    if "s8" in stages:
        W1P = inp("w1p", [NE * 128 * 8, 2048]); W3P = inp("w3p", [NE * 128 * 8, 2048]); W2P = inp("w2p", [NE * 128 * 8, 2048])
        k.push()
        iw0 = k.sb("iw0", [128, NBLK], I32); iwf = k.sb("iwf", [128, NBLK], F32); iwg = k.sb("iwg", [128, NBLK], F32)
        k.dma("sp", iw0[:], IWD[:, :], reads=[IWD], writes=[iw0])
        k.ve("tensor_copy", [iw0], [iwf], out=iwf[:], in_=iw0[:])
        iwc = [k.sb("iwc%d" % c, [128, NBLK], I32) for c in range(8)]
        for c in range(8):
            k.ve("tensor_scalar", [iwf], [iwg], out=iwg[:], in0=iwf[:], scalar1=8.0, scalar2=float(c), op0=ALU.mult, op1=ALU.add)
            k.ve("tensor_copy", [iwg], [iwc[c]], out=iwc[c][:], in_=iwg[:])
        w1c = [[k.sb("w1c%d_%d" % (i, c), [128, 2048], BF16) for c in range(8)] for i in range(2)]
        w3c = [[k.sb("w3c%d_%d" % (i, c), [128, 2048], BF16) for c in range(8)] for i in range(2)]
        w2c = [k.sb("w2c%d" % c, [128, 2048], BF16) for c in range(8)]
        xe = k.sb("xe", [128, D], BF16)
        xT = [k.sb("xT%d" % q, [128, 8, 128], BF16) for q in range(4)]
        a_sb = k.sb("a_sb", [128, 512], F32); act_ = k.sb("act_", [128, 512], BF16); aT = k.sb("aT", [128, 4, 128], BF16)
        yes = [k.sb("ye%d" % i, [128, 2048], BF16) for i in range(2)]
        for j in range(NBLK):
            r_ = j % 2
            for c in range(8):
                k.gather(w1c[r_][c][:], W1P[:, :], iwc[c][:, j:j + 1], reads=[iwc[c], w1c[r_][c]], writes=[w1c[r_][c]], bound=NE * 128 * 8 - 1)
            for c in range(8):
                k.gather(w3c[r_][c][:], W3P[:, :], iwc[c][:, j:j + 1], reads=[iwc[c], w3c[r_][c]], writes=[w3c[r_][c]], bound=NE * 128 * 8 - 1)
            for c in range(8):
                k.gather(w2c[c][:], W2P[:, :], iwc[c][:, j:j + 1], reads=[iwc[c], w2c[c]], writes=[w2c[c]], bound=NE * 128 * 8 - 1)
            for sub in range(BLKT):
                rr0 = (j * BLKT + sub) * 128
                k.dma("sp", xe[:], XE[rr0:rr0 + 128, :], reads=[XE], writes=[xe])
                for q in range(4):
                    for jj in range(8):
                        kc = q * 8 + jj
                        k.tr(PB[q][:, jj * 128:(jj + 1) * 128], xe[:, kc * 128:(kc + 1) * 128], identb[:], [xe, identb], [P[q]])
                    evac(xT[q][:], PB[q][:, 0:1024].rearrange("p (j n) -> p j n", n=128), [P[q]], [xT[q]])
                for kc in range(32):
                    k.mm(P[4][:, :], xT[kc // 8][:, kc % 8, :], w1c[r_][kc // 4][:, (kc % 4) * 512:(kc % 4 + 1) * 512], kc == 0, kc == 31, [xT[kc // 8], w1c[r_][kc // 4]], [P[4]])
                for kc in range(32):
                    k.mm(P[5][:, :], xT[kc // 8][:, kc % 8, :], w3c[r_][kc // 4][:, (kc % 4) * 512:(kc % 4 + 1) * 512], kc == 0, kc == 31, [xT[kc // 8], w3c[r_][kc // 4]], [P[5]])
                k.act(a_sb[:], P[4][:, :], AF.Silu, [P[4]], [a_sb])
                k.ve("tensor_tensor", [a_sb, P[5]], [act_], out=act_[:], in0=a_sb[:], in1=P[5][:, :], op=ALU.mult)
                for mc in range(4):
                    k.tr(PB[6][:, mc * 128:(mc + 1) * 128], act_[:, mc * 128:(mc + 1) * 128], identb[:], [act_, identb], [P[6]])
                evac(aT[:], PB[6][:, 0:512].rearrange("p (j n) -> p j n", n=128), [P[6]], [aT])
                for cc in range(8):
                    ps = P[cc % 4]
                    for kc in range(4):
                        k.mm(ps[:, :], aT[:, kc, :], w2c[kc * 2 + cc // 4][:, (cc % 4) * 512:(cc % 4 + 1) * 512], kc == 0, kc == 3, [aT, w2c[kc * 2 + cc // 4]], [ps])
                    ye = yes[cc // 4]
                    evac(ye[:, (cc % 4) * 512:(cc % 4 + 1) * 512], ps[:, :], [ps], [ye])
                    if cc % 4 == 3:
                        hf = cc // 4
                        k.dma("sp", YE[rr0:rr0 + 128, hf * 2048:(hf + 1) * 2048], ye[:], reads=[ye], writes=[YE])
        k.pop()

    if "s9" in stages:
        G_FIN = inp("g_fin", [1, D])
        k.push()
        slots9 = k.sb("slots9", [128, NT * 2], I32); gates9 = k.sb("gates9", [128, NT * 2], F32)
        k.dma("sp", slots9[:], SLOTS[:, :], reads=[SLOTS], writes=[slots9])
        k.dma("sp", gates9[:], GATES[:, :], reads=[GATES], writes=[gates9])
        GF = k.sb("GF", [128, D], F32); FG = k.sb("FG", [128, D], F32)
        bcast_row(FG, G_FIN[0:1, :])
        y1s = [k.sb("y1_%d" % i, [128, D], BF16) for i in range(2)]
        y2s = [k.sb("y2_%d" % i, [128, D], BF16) for i in range(2)]
        xts = [k.sb("x9_%d" % i, [128, D], F32) for i in range(2)]
        tts = [k.sb("t9_%d" % i, [128, D], F32) for i in range(2)]
        junk = k.sb("junk9", [128, D], BF16)
        ss = k.sb("ss9", [128, 1], F32); rstd = k.sb("rstd9", [128, 1], F32)
        for b in range(NB):
            load_mod_vec(GF, b, 5)
            for t in range(16):
                g = b * 16 + t
                r0 = g * 128
                y1, y2, xt, tt = y1s[g % 2], y2s[g % 2], xts[g % 2], tts[g % 2]
                k.gather(y1[:], YE[:, :], slots9[:, 2 * g:2 * g + 1], reads=[YE, slots9], writes=[y1])
                k.gather(y2[:], YE[:, :], slots9[:, 2 * g + 1:2 * g + 2], reads=[YE, slots9], writes=[y2])
                k.dma("sp", xt[:], X1[r0:r0 + 128, :], reads=[X1], writes=[xt])
                k.ve("tensor_scalar", [y1, gates9], [tt], out=tt[:], in0=y1[:], scalar1=gates9[:, 2 * g:2 * g + 1], scalar2=None, op0=ALU.mult)
                k.ve("scalar_tensor_tensor", [y2, gates9, tt], [tt], out=tt[:], in0=y2[:], scalar=gates9[:, 2 * g + 1:2 * g + 2], in1=tt[:], op0=ALU.mult, op1=ALU.add)
                k.ve("tensor_tensor", [tt, GF], [tt], out=tt[:], in0=tt[:], in1=GF[:], op=ALU.mult)
                k.ve("tensor_tensor", [tt, xt], [xt], out=xt[:], in0=tt[:], in1=xt[:], op=ALU.add)
                k.act(junk[:], xt[:], AF.Square, [xt], [junk, ss], accum_out=ss[:])
                rstd_from_ss(rstd, ss, D)
                k.ve("scalar_tensor_tensor", [xt, rstd, FG], [tt], out=tt[:], in0=xt[:], scalar=rstd[:, 0:1], in1=FG[:], op0=ALU.mult, op1=ALU.mult)
                k.dma("sp", OUT[r0:r0 + 128, :], tt[:], reads=[tt], writes=[OUT])
        k.pop()

    k.emit()
    cx.nc = nc
    return cx


def host_layouts(I, NB=NB_FULL):
    f = np.float32
    NT = NB * 16
    o = {}
    o["x"] = np.ascontiguousarray(I["x"][:NB].reshape(NB * SEQ, D))
    o["c"] = np.ascontiguousarray(I["c"][:NB])
    o["posT"] = np.ascontiguousarray(I["positions"][:NB].reshape(NT, 128).T.astype(np.int32))
    o["w_ada"] = I["w_ada"][0]
    o["b_ada"] = I["b_ada"][0].reshape(1, -1)
    o["g_mix"] = I["norm_mix_gain"][0].reshape(1, -1)
    o["g_ffn"] = I["norm_ffn_gain"][0].reshape(1, -1)
    o["g_fin"] = I["final_gain"].reshape(1, -1)
    o["w_in"] = I["w_in"][0]
    o["g_q"] = I["q_lat_gain"][0].reshape(1, -1)
    o["g_kv"] = I["kv_lat_gain"][0].reshape(1, -1)
    wq = I["w_uq"][0].reshape(QR, NH, 192)
    o["w_uqx"] = np.ascontiguousarray(np.concatenate(
        [wq[:, :, 0:192], wq[:, :, 160:192], wq[:, :, 128:160]], axis=2).reshape(QR, NH * 256))
    o["w_ukv"] = I["w_ukv"][0]
    lre, lim, ldt = I["ssm_lam_re"][0], I["ssm_lam_im"][0], I["ssm_log_dt"][0]
    o["lamre_T2"] = np.ascontiguousarray(np.concatenate([lre.T, lre.T], 0))
    o["lamim_T2"] = np.ascontiguousarray(np.concatenate([lim.T, lim.T], 0))
    o["logdt_bc"] = np.ascontiguousarray(np.broadcast_to(ldt[None, :], (128, NG)))

    def L2(a):
        return np.ascontiguousarray(a.reshape(16, 8, NST, 16).transpose(1, 3, 0, 2).reshape(128, 16, NST))
    o["bre_L2"] = L2(I["ssm_b_re"][0])
    o["bim_L2"] = L2(I["ssm_b_im"][0])
    o["lamre_L2"] = L2(np.broadcast_to(lre[:, :, None], (NG, NST, 16)))
    o["lamim_L2"] = L2(np.broadcast_to(lim[:, :, None], (NG, NST, 16)))
    o["logdt_L2"] = np.ascontiguousarray(np.broadcast_to(ldt.reshape(16, 8)[:, :, None], (16, 8, 16)).transpose(1, 2, 0).reshape(128, 16))
    cre, cim = I["ssm_c_re"][0], I["ssm_c_im"][0]
    o["c_L3"] = np.ascontiguousarray(np.concatenate(
        [cre.transpose(2, 0, 1).reshape(NST, NG * 16), cim.transpose(2, 0, 1).reshape(NST, NG * 16)], 0))
    o["d_L"] = np.ascontiguousarray(I["ssm_d"][0].reshape(16, 128).T)
    o["w_glu"] = I["w_glu"][0]
    o["bglu_L"] = np.ascontiguousarray(I["b_glu"][0].reshape(16, 128).T)
    o["gmla_L"] = np.ascontiguousarray(I["mla_out_gain"][0].reshape(16, 128).T)
    o["gssm_L"] = np.ascontiguousarray(I["ssm_out_gain"][0].reshape(16, 128).T)
    o["w_out"] = I["w_out"][0]
    o["w_r"] = np.ascontiguousarray(np.concatenate([I["w_group_router"][0], I["w_expert_router"][0]], 1))
    o["b_r"] = np.concatenate([I["b_group_router"][0], I["b_expert_router"][0]]).reshape(1, -1)
    o["w1p"] = lambda: np.ascontiguousarray(I["w1_experts"][0].reshape(NE, 32, 128, DE).transpose(0, 2, 1, 3)).reshape(NE * 128 * 8, 2048)
    o["w3p"] = lambda: np.ascontiguousarray(I["w3_experts"][0].reshape(NE, 32, 128, DE).transpose(0, 2, 1, 3)).reshape(NE * 128 * 8, 2048)
    o["w2p"] = lambda: np.ascontiguousarray(I["w2_experts"][0].reshape(NE, 4, 128, D).transpose(0, 2, 1, 3)).reshape(NE * 128 * 8, 2048)
    o["ident"] = np.eye(128, dtype=f)
    invf = np.exp(-math.log(10000.0) * np.arange(0, 64, 2, dtype=f) / 64).astype(f)
    o["invf_bc"] = np.ascontiguousarray(np.broadcast_to(invf[None, :], (128, 32)))
    m8 = np.zeros((128, 8), f)
    for gl in range(8):
        m8[gl * 16:(gl + 1) * 16, gl] = 1
    o["mask8"] = m8
    psw = np.zeros((128, 128), f)
    for p in range(64):
        psw[p + 64, p] = -1.0
        psw[p, p + 64] = 1.0
    o["psw"] = psw
    o["tri"] = np.triu(np.ones((128, 128), f), 1)
    NBLK = (NB * SEQ * 2 + NE * (BLK - 1)) // BLK
    o["jb"] = np.ascontiguousarray(np.broadcast_to((np.arange(NBLK, dtype=f) * BLK)[None, :], (128, NBLK)))
    o["pidx"] = np.arange(128, dtype=f).reshape(128, 1)
    o["iota_t"] = np.ascontiguousarray(np.broadcast_to(np.arange(SEQ, dtype=f)[None, :], (128, SEQ)))
    return o


_NPDT = {F32: np.float32, BF16: ml_dtypes.bfloat16, I32: np.int32}


N_CORES = 4


def kernel(**inputs):
    I = {k_: np.asarray(v) for k_, v in inputs.items()}
    cx = build(NB=1)
    shared = None
    in_maps = []
    for b in range(N_CORES):
        Ib = dict(I)
        Ib["x"] = I["x"][b:b + 1]
        Ib["c"] = I["c"][b:b + 1]
        Ib["positions"] = I["positions"][b:b + 1]
        if shared is None:
            H = host_layouts(Ib, 1)
            shared = {}
            for name, (shape, dt) in cx.inputs.items():
                if name in ("x", "c", "posT"):
                    continue
                v = H[name]() if callable(H[name]) else H[name]
                shared[name] = np.ascontiguousarray(np.asarray(v).astype(_NPDT[dt], copy=False)).reshape(shape)
        m = dict(shared)
        m["x"] = np.ascontiguousarray(Ib["x"].reshape(SEQ, D))
        m["c"] = np.ascontiguousarray(Ib["c"])
        m["posT"] = np.ascontiguousarray(Ib["positions"].reshape(16, 128).T.astype(np.int32))
        in_maps.append(m)
    res = run_bass_kernel_spmd(cx.nc, in_maps, core_ids=list(range(N_CORES)))
    out = np.stack([np.asarray(res.results[b]["out"], dtype=np.float32).reshape(SEQ, D) for b in range(N_CORES)], 0)
    return out
```
